# Optimizing a Trainium2 kernel written in Bass

```python
import math
import jax, jax.numpy as jnp
from jax import lax
import numpy as np

D_MODEL = 1024
BATCH = 8
SEQ = 4096
DEPTH = 4

SB_HEADS = 4
SB_DIM = D_MODEL // (4 * SB_HEADS)
GDN_HEADS = 4
GDN_DK = D_MODEL // (2 * GDN_HEADS)
GDN_DV = D_MODEL // (2 * GDN_HEADS)
DA_HEADS = 4
DA_V = D_MODEL // (4 * DA_HEADS)
DA_QK = DA_V // 2
SB_W = SB_HEADS * SB_DIM
GDN_KW = GDN_HEADS * GDN_DK
GDN_W = GDN_HEADS * GDN_DV
DA_W = DA_HEADS * DA_V
MIX_W = SB_W + GDN_W + DA_W
IN_SPLIT_SIZES = (SB_W, SB_W, SB_W, GDN_KW, GDN_KW, GDN_W, GDN_W, GDN_HEADS, GDN_HEADS, DA_W, DA_W, DA_W)
IN_WIDTH = sum(IN_SPLIT_SIZES)
IN_SPLIT_POINTS = tuple(int(p) for p in np.cumsum(IN_SPLIT_SIZES)[:-1])

Q_BLOCK = 128
GDN_CHUNK = 64
CONV_K = 4
CONV_CH = 2 * GDN_KW + GDN_W
ROPE_THETA = 500000.0
ROT_DIM = DA_QK // 4

N_EXPERTS = 32
TOP_K = 4
D_FF = D_MODEL
SWIGLU_ALPHA = 1.702
SWIGLU_LIMIT = 7.0
MOE_BLOCK = 256

DN_ALPHA = (2 * DEPTH) ** 0.25
DN_BETA = (8 * DEPTH) ** -0.25
LN_EPS = 1e-5
RMS_EPS = 1e-6

kernel_name = 'hybrid_sb_gdn_diff_moe_deepnorm_adaln'


def layer_norm(x, g, b):
    xf = x.astype(jnp.float32)
    mu = jnp.mean(xf, -1, keepdims=True)
    var = jnp.mean(jnp.square(xf - mu), -1, keepdims=True)
    return ((xf - mu) * lax.rsqrt(var + LN_EPS) * g + b).astype(x.dtype)


def rms_norm(x, w):
    xf = x.astype(jnp.float32)
    return (xf * lax.rsqrt(jnp.mean(xf * xf, -1, keepdims=True) + RMS_EPS) * w).astype(x.dtype)


def l2_normalize(x):
    xf = x.astype(jnp.float32)
    return xf * lax.rsqrt(jnp.sum(xf * xf, -1, keepdims=True) + 1e-6)


def split_heads(t, n):
    B, S, _ = t.shape
    return t.reshape(B, S, n, -1).transpose(0, 2, 1, 3)


def merge_heads(t):
    B, H, S, d = t.shape
    return t.transpose(0, 2, 1, 3).reshape(B, S, H * d)


def sweep_query_blocks(fn, q):
    B, H, S = q.shape[:3]
    nb = S // Q_BLOCK
    qb = jnp.moveaxis(q.reshape(B, H, nb, Q_BLOCK, *q.shape[3:]), 2, 0)
    starts = jnp.arange(nb, dtype=jnp.int32) * Q_BLOCK
    out = lax.map(lambda a: fn(a[0], a[1]), (qb, starts))
    return jnp.moveaxis(out, 0, 2).reshape(B, H, S, -1)


def stick_breaking_attention(q, k, v):
    S = q.shape[2]
    scale = q.shape[-1] ** -0.5
    key_pos = jnp.arange(S)

    def block(qb, start):
        z = jnp.einsum('bhqd,bhkd->bhqk', qb, k).astype(jnp.float32) * scale
        q_pos = start + jnp.arange(Q_BLOCK)
        causal = key_pos[None, :] < q_pos[:, None]
        log_keep = jnp.where(causal, jax.nn.log_sigmoid(-z), 0.0)
        between = lax.cumsum(log_keep, axis=3, reverse=True) - log_keep
        w = jnp.where(causal, jnp.exp(jax.nn.log_sigmoid(z) + between), 0.0)
        return jnp.einsum('bhqk,bhkd->bhqd', w.astype(v.dtype), v)

    return sweep_query_blocks(block, q)


def causal_depthwise_conv(x, w):
    K = w.shape[0]
    return lax.conv_general_dilated(x, w[:, None, :], window_strides=(1,), padding=[(K - 1, 0)],
                                    dimension_numbers=('NWC', 'WIO', 'NWC'), feature_group_count=x.shape[-1])


def gated_delta_rule(q, k, v, g, beta):
    f32 = jnp.float32
    B, H, S, dk = k.shape
    dv = v.shape[-1]
    C = GDN_CHUNK
    n = S // C
    q = (q.astype(f32) * dk ** -0.5).reshape(B, H, n, C, dk)
    k = k.astype(f32).reshape(B, H, n, C, dk)
    v = v.astype(f32).reshape(B, H, n, C, dv)
    beta = beta.astype(f32).reshape(B, H, n, C)
    g = jnp.cumsum(g.astype(f32).reshape(B, H, n, C), axis=-1)
    lower = jnp.tril(jnp.ones((C, C), bool))
    strict = jnp.tril(jnp.ones((C, C), bool), -1)
    decay = jnp.where(lower, jnp.exp(jnp.where(lower, g[..., :, None] - g[..., None, :], 0.0)), 0.0)
    k_beta = k * beta[..., None]
    m = jnp.where(strict, jnp.einsum('bhnid,bhnjd->bhnij', k_beta, k) * decay, 0.0)
    rhs = jnp.concatenate([v * beta[..., None], k_beta * jnp.exp(g)[..., None]], -1)
    sol = lax.linalg.triangular_solve(m + jnp.eye(C, dtype=f32), rhs, left_side=True, lower=True,
                                      unit_diagonal=True)
    u = sol[..., :dv]
    w = sol[..., dv:]
    attn_intra = jnp.where(lower, jnp.einsum('bhnid,bhnjd->bhnij', q, k) * decay, 0.0)

    def step(state, inp):
        q_i, k_i, u_i, w_i, g_i, a_i = inp
        v_new = u_i - jnp.einsum('bhcd,bhde->bhce', w_i, state)
        o = (jnp.einsum('bhcd,bhde->bhce', q_i * jnp.exp(g_i)[..., None], state)
             + jnp.einsum('bhij,bhje->bhie', a_i, v_new))
        g_last = g_i[..., -1]
        state = (state * jnp.exp(g_last)[..., None, None]
                 + jnp.einsum('bhcd,bhce->bhde', k_i * jnp.exp(g_last[..., None] - g_i)[..., None], v_new))
        return state, o

    xs = tuple(jnp.moveaxis(t, 2, 0) for t in (q, k, u, w, g, attn_intra))
    _, o = lax.scan(step, jnp.zeros((B, H, dk, dv), f32), xs)
    return jnp.moveaxis(o, 0, 2).reshape(B, H, S, dv)


def partial_rotary(x, cos, sin):
    c = cos[None, :, None, None, :].astype(x.dtype)
    s = sin[None, :, None, None, :].astype(x.dtype)
    half = ROT_DIM // 2
    x1, x2, xp = x[..., :half], x[..., half:ROT_DIM], x[..., ROT_DIM:]
    return jnp.concatenate([x1 * c - x2 * s, x2 * c + x1 * s, xp], -1)


def differential_attention(q, k, v, lam, subln_w, lambda_init):
    S = q.shape[2]
    scale = q.shape[-1] ** -0.5
    key_pos = jnp.arange(S)

    def block(qb, start):
        s = jnp.einsum('bhqcd,bhkcd->bhcqk', qb, k).astype(jnp.float32) * scale
        q_pos = start + jnp.arange(Q_BLOCK)
        causal = key_pos[None, :] <= q_pos[:, None]
        p = jax.nn.softmax(jnp.where(causal, s, -jnp.inf), axis=-1)
        w = p[:, :, 0] - lam * p[:, :, 1]
        return jnp.einsum('bhqk,bhkd->bhqd', w.astype(v.dtype), v)

    o = sweep_query_blocks(block, q)
    return rms_norm(o, subln_w) * (1.0 - lambda_init)


def hybrid_mixer(h, w_in, conv_w, a_log, dt_bias, gdn_norm_w, lam_p, subln_w, w_out, cos, sin, lambda_init):
    B, S, _ = h.shape
    f32 = jnp.float32
    proj = h @ w_in
    sb_q, sb_k, sb_v, gq, gk, gv, gz, ga, gb, dq, dk, dv = jnp.split(proj, IN_SPLIT_POINTS, axis=-1)

    o_sb = merge_heads(stick_breaking_attention(split_heads(sb_q, SB_HEADS), split_heads(sb_k, SB_HEADS),
                                                split_heads(sb_v, SB_HEADS)))

    qkv = jax.nn.silu(causal_depthwise_conv(jnp.concatenate([gq, gk, gv], -1), conv_w))
    gq, gk, gv = jnp.split(qkv, (GDN_KW, 2 * GDN_KW), axis=-1)
    decay = -jnp.exp(a_log.astype(f32))[None, :, None] * jax.nn.softplus(
        ga.astype(f32).transpose(0, 2, 1) + dt_bias.astype(f32)[None, :, None])
    beta = jax.nn.sigmoid(gb.astype(f32)).transpose(0, 2, 1)
    o_g = gated_delta_rule(l2_normalize(split_heads(gq, GDN_HEADS)), l2_normalize(split_heads(gk, GDN_HEADS)),
                           split_heads(gv, GDN_HEADS), decay, beta)
    o_g = rms_norm(o_g, gdn_norm_w) * jax.nn.silu(split_heads(gz, GDN_HEADS).astype(f32))
    o_gdn = merge_heads(o_g).astype(h.dtype)

    dq = partial_rotary(dq.reshape(B, S, DA_HEADS, 2, DA_QK), cos, sin).transpose(0, 2, 1, 3, 4)
    dk = partial_rotary(dk.reshape(B, S, DA_HEADS, 2, DA_QK), cos, sin).transpose(0, 2, 1, 3, 4)
    lp = lam_p.astype(f32)
    lam = jnp.exp(jnp.sum(lp[0] * lp[1])) - jnp.exp(jnp.sum(lp[2] * lp[3])) + lambda_init
    o_da = merge_heads(differential_attention(dq, dk, split_heads(dv, DA_HEADS), lam, subln_w, lambda_init))

    mixed = jnp.concatenate([o_sb, o_gdn, o_da.astype(h.dtype)], -1)
    return mixed @ w_out


def clamped_swiglu(hgu):
    x_glu = jnp.minimum(hgu[..., ::2], SWIGLU_LIMIT)
    x_lin = jnp.clip(hgu[..., 1::2], -SWIGLU_LIMIT, SWIGLU_LIMIT)
    return x_glu * jax.nn.sigmoid(SWIGLU_ALPHA * x_glu) * (x_lin + 1.0)


def moe_ffn(h, w_router, b_router, w_gu, b_gu, w_down, b_down):
    B, S, D = h.shape
    N = B * S
    xf = h.reshape(N, D)
    logits = (xf @ w_router).astype(jnp.float32) + b_router.astype(jnp.float32)
    top_val, top_idx = lax.top_k(logits, TOP_K)
    gates = jax.nn.softmax(top_val, axis=-1)
    e_flat = top_idx.reshape(-1)
    tok_flat = jnp.repeat(jnp.arange(N, dtype=jnp.int32), TOP_K)
    gate_flat = gates.reshape(-1)
    order = jnp.argsort(e_flat)
    e_sorted = e_flat[order]
    counts = jnp.zeros((N_EXPERTS,), jnp.int32).at[e_flat].add(1)
    starts = jnp.cumsum(counts) - counts
    padded = (counts + MOE_BLOCK - 1) // MOE_BLOCK * MOE_BLOCK
    padded_ends = jnp.cumsum(padded)
    padded_starts = padded_ends - padded
    rank = jnp.arange(N * TOP_K, dtype=jnp.int32)
    dest = padded_starts[e_sorted] + rank - starts[e_sorted]
    n_rows = -(-(N * TOP_K) // MOE_BLOCK) * MOE_BLOCK + N_EXPERTS * MOE_BLOCK
    n_blocks = n_rows // MOE_BLOCK
    row_tok = jnp.full((n_rows,), N, jnp.int32).at[dest].set(tok_flat[order])
    row_gate = jnp.zeros((n_rows,), jnp.float32).at[dest].set(gate_flat[order])
    block_expert = jnp.minimum(jnp.searchsorted(padded_ends, jnp.arange(n_blocks, dtype=jnp.int32) * MOE_BLOCK,
                                                side='right'), N_EXPERTS - 1)
    x_rows = jnp.concatenate([xf, jnp.zeros((1, D), xf.dtype)], 0)[row_tok].reshape(n_blocks, MOE_BLOCK, D)

    def expert_block(args):
        xb, e = args
        return clamped_swiglu(xb @ w_gu[e] + b_gu[e]) @ w_down[e] + b_down[e]

    y_rows = lax.map(expert_block, (x_rows, block_expert)).reshape(n_rows, D)
    y = jax.ops.segment_sum(y_rows * row_gate[:, None].astype(y_rows.dtype), row_tok, num_segments=N + 1)[:N]
    return y.reshape(B, S, D)


def setup_inputs(seed: int = 0) -> dict:
    key = jax.random.key(seed)
    ks = jax.random.split(key, 24)
    f32 = jnp.float32
    D = D_MODEL

    def nrm(k, shape, scale):
        return jax.random.normal(k, shape, f32) * scale

    dt = jnp.exp(jax.random.uniform(ks[7], (DEPTH, GDN_HEADS), f32, math.log(1e-3), math.log(1e-1)))
    return {
        'x': nrm(ks[0], (BATCH, SEQ, D), 1.0),
        'c': nrm(ks[1], (BATCH, D), 1.0),
        'w_ada': nrm(ks[2], (DEPTH, D, 6 * D), 0.1 * D ** -0.5),
        'b_ada': nrm(ks[3], (DEPTH, 6 * D), 0.02),
        'w_in': nrm(ks[4], (DEPTH, D, IN_WIDTH), D ** -0.5),
        'conv_w': nrm(ks[5], (DEPTH, CONV_K, CONV_CH), CONV_K ** -0.5),
        'gdn_a_log': jnp.log(jax.random.uniform(ks[6], (DEPTH, GDN_HEADS), f32, 1.0, 16.0)),
        'gdn_dt_bias': dt + jnp.log(-jnp.expm1(-dt)),
        'gdn_norm_w': 1.0 + nrm(ks[8], (DEPTH, GDN_DV), 0.02),
        'da_lambda': nrm(ks[9], (DEPTH, 4, DA_QK), 0.1),
        'da_subln_w': 1.0 + nrm(ks[10], (DEPTH, DA_V), 0.02),
        'w_out': nrm(ks[11], (DEPTH, MIX_W, D), DN_BETA * MIX_W ** -0.5),
        'ln1_g': 1.0 + nrm(ks[12], (DEPTH, D), 0.02),
        'ln1_b': nrm(ks[13], (DEPTH, D), 0.02),
        'w_router': nrm(ks[14], (DEPTH, D, N_EXPERTS), D ** -0.5),
        'b_router': nrm(ks[15], (DEPTH, N_EXPERTS), 0.01),
        'w_gu': nrm(ks[16], (DEPTH, N_EXPERTS, D, 2 * D_FF), D ** -0.5),
        'b_gu': nrm(ks[17], (DEPTH, N_EXPERTS, 2 * D_FF), 0.02),
        'w_down': nrm(ks[18], (DEPTH, N_EXPERTS, D_FF, D), DN_BETA * D_FF ** -0.5),
        'b_down': nrm(ks[19], (DEPTH, N_EXPERTS, D), 0.02),
        'ln2_g': 1.0 + nrm(ks[20], (DEPTH, D), 0.02),
        'ln2_b': nrm(ks[21], (DEPTH, D), 0.02),
    }


def reference(x, c, w_ada, b_ada, w_in, conv_w, gdn_a_log, gdn_dt_bias, gdn_norm_w, da_lambda, da_subln_w,
              w_out, ln1_g, ln1_b, w_router, b_router, w_gu, b_gu, w_down, b_down, ln2_g, ln2_b):
    S = x.shape[1]
    pos = jnp.arange(S, dtype=jnp.float32)
    inv_freq = ROPE_THETA ** (-jnp.arange(0, ROT_DIM, 2, dtype=jnp.float32) / ROT_DIM)
    ang = pos[:, None] * inv_freq[None, :]
    cos, sin = jnp.cos(ang), jnp.sin(ang)
    cond = jax.nn.silu(c)
    for l in range(DEPTH):
        mod = cond @ w_ada[l] + b_ada[l]
        sh1, sc1, g1, sh2, sc2, g2 = [m[:, None, :] for m in jnp.split(mod, 6, axis=-1)]
        lambda_init = 0.8 - 0.6 * math.exp(-0.3 * l)
        h = x * (1.0 + sc1) + sh1
        mix = hybrid_mixer(h, w_in[l], conv_w[l], gdn_a_log[l], gdn_dt_bias[l], gdn_norm_w[l], da_lambda[l],
                           da_subln_w[l], w_out[l], cos, sin, lambda_init)
        x = layer_norm(DN_ALPHA * x + (1.0 + g1) * mix, ln1_g[l], ln1_b[l])
        h = x * (1.0 + sc2) + sh2
        ffn = moe_ffn(h, w_router[l], b_router[l], w_gu[l], b_gu[l], w_down[l], b_down[l])
        x = layer_norm(DN_ALPHA * x + (1.0 + g2) * ffn, ln2_g[l], ln2_b[l])
    return x
```

```python
import contextlib
import math
import numpy as np
import concourse.bass as bass
import concourse.mybir as mybir
from concourse.bass_utils import run_bass_kernel_spmd

F32 = mybir.dt.float32
BF = mybir.dt.bfloat16
AF = mybir.ActivationFunctionType
ALU = mybir.AluOpType
AX = mybir.AxisListType

D = 1024
DEPTH = 4
SEQ = 4096
NE = 32
IN_W = 3592
DN_ALPHA = (2 * DEPTH) ** 0.25
C_SBQ, C_SBK, C_SBV, C_GQ, C_GK, C_GV, C_GZ, C_GA, C_GB, C_DQ, C_DK, C_DV = (
    0, 256, 512, 768, 1280, 1792, 2304, 2816, 2820, 2824, 3080, 3336)


class T:
    __slots__ = ("h", "w", "r")

    def __init__(self, h):
        self.h = h
        self.w = None
        self.r = {}

    def __getitem__(self, k):
        return self.h[k]


class TV:
    __slots__ = ("h", "p")

    def __init__(self, parent, ap):
        self.h = ap
        self.p = parent

    def __getitem__(self, k):
        return self.h[k]


def _roots(bs):
    return [getattr(b, "p", b) for b in bs]


class KB:
    def __init__(self, nc, es):
        self.nc = nc
        self.es = es
        self.E = {}
        for name, e in (("pe", nc.tensor), ("dve", nc.vector), ("act", nc.scalar), ("pool", nc.gpsimd),
                        ("sp", nc.sync)):
            sem = es.enter_context(nc.semaphore("c_" + name))
            self.E[name] = {"eng": e, "sem": sem, "cnt": 0, "known": {}}
        self.rings = {}
        for q in ("sp", "pool", "act"):
            self.rings[q] = {"i": 0, "slots": [[es.enter_context(nc.semaphore(f"d_{q}{i}")), 0] for i in range(12)]}
        self.all_dma = {}
        self.ninst = 0

    def _waits(self, en, r, w, extra=()):
        E = self.E[en]
        deps = {}

        def add(t):
            k = id(t[0])
            if k not in deps or deps[k][1] < t[1]:
                deps[k] = t
        for b in r:
            if b.w:
                add(b.w)
        for b in w:
            if b.w:
                add(b.w)
            for d in b.r.values():
                add(d)
        for t in extra:
            add(t)
        for k, (sem, val) in deps.items():
            if en == "pe" and sem is E["sem"]:
                continue
            if E["known"].get(k, 0) < val:
                E["eng"].wait_ge(sem, val)
                E["known"][k] = val
                self.ninst += 1

    def op(self, en, fn, r=(), w=()):
        E = self.E[en]
        r, w = _roots(r), _roots(w)
        self._waits(en, r, w)
        inst = fn(E["eng"])
        E["cnt"] += 1
        inst.then_inc(E["sem"], 1)
        self.ninst += 1
        tok = (E["sem"], E["cnt"])
        k = id(E["sem"])
        for b in r:
            b.r[k] = tok
        for b in w:
            b.w = tok
            b.r = {}

    def dma(self, q, out, in_, r=(), w=(), **kw):
        E = self.E[q]
        r, w = _roots(r), _roots(w)
        ring = self.rings[q]
        slot = ring["slots"][ring["i"]]
        ring["i"] = (ring["i"] + 1) % len(ring["slots"])
        extra = [(slot[0], slot[1])] if slot[1] else []
        self._waits(q, r, w, extra)
        inst = E["eng"].dma_start(out=out, in_=in_, **kw)
        slot[1] += 16
        inst.then_inc(slot[0], 16)
        self.ninst += 1
        tok = (slot[0], slot[1])
        k = id(slot[0])
        for b in r:
            b.r[k] = tok
        for b in w:
            b.w = tok
            b.r = {}
        self.all_dma[k] = tok

    def barrier(self):
        toks = [(E["sem"], E["cnt"]) for E in self.E.values() if E["cnt"]]
        toks += list(self.all_dma.values())
        for en, E in self.E.items():
            for sem, val in toks:
                if E["known"].get(id(sem), 0) < val:
                    E["eng"].wait_ge(sem, val)
                    E["known"][id(sem)] = val
                    self.ninst += 1

    def sb(self, es, name, shape, dt=F32):
        self.uid = getattr(self, "uid", 0) + 1
        return T(es.enter_context(self.nc.sbuf_tensor(f"{name}_{self.uid}", list(shape), dt)))

    def ps(self, es, name, shape, dt=F32):
        self.uid = getattr(self, "uid", 0) + 1
        return T(es.enter_context(self.nc.psum_tensor(f"{name}_{self.uid}", list(shape), dt)))


def _bcast_rows(ap_row, n=128):
    return ap_row.partition_broadcast(n)


def build_program(S=SEQ, depth=DEPTH, debug=False, phases=None):
    nc = bass.Bass("TRN2", target_bir_lowering=False)
    NT = S // 128
    NG = S // 512
    dbg_kind = "ExternalOutput" if debug else "Internal"

    def din(name, shape, dt=F32):
        return nc.dram_tensor(name, list(shape), dt, kind="ExternalInput").ap()

    def dscr(name, shape, dt=F32):
        return nc.dram_tensor(name, list(shape), dt, kind=dbg_kind).ap()

    I = {}
    I["x"] = din("x", [S, D])
    I["c"] = din("c", [8, 128])
    I["w_ada"] = din("w_ada", [depth, D, 6 * D])
    I["b_ada"] = din("b_ada", [depth, 6 * D])
    I["w_in"] = din("w_in", [depth, D, IN_W])
    I["conv_w"] = din("conv_w", [depth, 4, 1536])
    I["gdn_a_log"] = din("gdn_a_log", [depth, 4])
    I["gdn_dt_bias"] = din("gdn_dt_bias", [depth, 4])
    I["gdn_norm_w"] = din("gdn_norm_w", [depth, 128])
    I["da_lambda"] = din("da_lambda", [depth, 128])
    I["da_subln_w"] = din("da_subln_w", [depth, 64])
    I["w_out"] = din("w_out", [depth, D, D])
    for n in ("ln1_g", "ln1_b", "ln2_g", "ln2_b"):
        I[n] = din(n, [depth, D])
    I["w_router"] = din("w_router", [depth, D, NE])
    I["b_router"] = din("b_router", [depth, NE])
    I["w_gu"] = din("w_gu", [depth, NE, D, 2 * D])
    I["b_gu"] = din("b_gu", [depth, NE, 2 * D])
    I["w_down"] = din("w_down", [depth, NE, D, D])
    I["b_down"] = din("b_down", [depth, NE, D])
    I["k_ident"] = din("k_ident", [128, 128])
    I["k_tri"] = din("k_tri", [128, 3 * 128])
    I["k_msb"] = din("k_msb", [128, 4 * 512])
    I["k_mda"] = din("k_mda", [128, 4 * 512])
    I["k_cos"] = din("k_cos", [S, 512])
    I["k_sin"] = din("k_sin", [S, 512])
    out = nc.dram_tensor("out", [S, D], F32, kind="ExternalOutput").ap()

    X = [I["x"]] + [dscr(f"xs{l}", [S, D]) for l in range(depth - 1)] + [out]
    x1 = dscr("x1", [S, D])
    sbqT = dscr("sbqT", [256, S], BF)
    sbkT = dscr("sbkT", [256, S], BF)
    sbv = dscr("sbv", [S, 256], BF)
    gpre = dscr("gpre", [1536, S])
    gzd = dscr("gzd", [S, 512])
    gabd = dscr("gabd", [S, 8])
    dqT = dscr("dqT", [256, S], BF)
    dkT = dscr("dkT", [256, S], BF)
    dav = dscr("dav", [S, 256], BF)
    mixT = dscr("mixT", [D, S], BF)

    with contextlib.ExitStack() as es:
        K = KB(nc, es)
        identF = K.sb(es, "identF", [128, 128])
        identB = K.sb(es, "identB", [128, 128], BF)
        PB = K.sb(es, "PB", [128, 6, D])
        modT = K.sb(es, "modT", [128, 4, 8])
        K.dma("sp", identF[:], I["k_ident"][:, :], w=[identF])
        K.op("dve", lambda e: e.tensor_copy(out=identB[:], in_=identF[:]), r=[identF], w=[identB])

        for l in range(depth):
            lam_init = 0.8 - 0.6 * math.exp(-0.3 * l)
            xin, xout = X[l], X[l + 1]
            if phases is None or "A" in phases:
                phase_adaln(K, nc, I, l, identF, PB, modT)
                K.barrier()
            if phases is None or "B" in phases:
                phase_inproj(K, nc, I, l, S, xin, identF, identB, modT,
                             sbqT, sbkT, sbv, gpre, gzd, gabd, dqT, dkT, dav)
                K.barrier()
            if phases is None or "C" in phases:
                phase_sb(K, nc, I, S, sbqT, sbkT, sbv, mixT)
                K.barrier()
            if phases is None or "D" in phases:
                phase_da(K, nc, I, l, S, lam_init, dqT, dkT, dav, mixT)
                K.barrier()
            if phases is None or "E" in phases:
                phase_gdn(K, nc, I, l, S, identF, gpre, gzd, gabd, mixT)
                K.barrier()
            if phases is None or "F" in phases:
                phase_outproj(K, nc, I, l, S, xin, x1, mixT, PB)
                K.barrier()
            if phases is None or "G" in phases:
                phase_moe(K, nc, I, l, S, x1, xout, identF, PB, modT)
                K.barrier()
        K.barrier()
    return nc


def phase_adaln(K, nc, I, l, identF, PB, modT):
    with contextlib.ExitStack() as es:
        c8 = K.sb(es, "a_c8", [8, 128])
        cT = K.sb(es, "a_cT", [128, 8])
        condB = K.sb(es, "a_condB", [128, 8, 128])
        onesF = K.sb(es, "a_ones", [128, 128])
        MODB = K.sb(es, "a_modb", [128, 6 * D])
        BAB = K.sb(es, "a_bab", [128, 6 * D])
        tmp = K.sb(es, "a_tmp", [128, 8, 128])
        wst = [K.sb(es, f"a_w{i}", [128, 3072]) for i in range(2)]
        pT = K.ps(es, "a_pT", [128, 8])
        acc = [K.ps(es, f"a_acc{i}", [128, 512]) for i in range(6)]

        K.dma("sp", c8[:], I["c"][:, :], w=[c8])
        K.dma("sp", BAB[:], _bcast_rows(I["b_ada"][l:l + 1, :]), w=[BAB])
        for j, n in enumerate(("ln1_g", "ln1_b", "ln2_g", "ln2_b")):
            K.dma("sp", PB[:, 2 + j, :], _bcast_rows(I[n][l:l + 1, :]), w=[PB])
        K.op("pool", lambda e: e.memset(onesF[:], 1.0), w=[onesF])
        K.op("pe", lambda e: e.transpose(out=pT[:], in_=c8[:], identity=identF[0:8, 0:8]), r=[c8, identF], w=[pT])
        K.op("act", lambda e: e.activation(out=cT[:], in_=pT[:], func=AF.Silu), r=[pT], w=[cT])
        for k in range(8):
            K.op("dve", lambda e, k=k: e.tensor_scalar(out=condB[:, k, :], in0=onesF[:], scalar1=cT[:, k:k + 1],
                                                        scalar2=None, op0=ALU.mult), r=[onesF, cT], w=[condB])
        for half in range(2):
            for k in range(8):
                wb = wst[k % 2]
                K.dma("sp", wb[:], I["w_ada"][l, k * 128:(k + 1) * 128, half * 3072:(half + 1) * 3072], w=[wb])
                for n in range(6):
                    K.op("pe", lambda e, n=n, k=k, wb=wb: e.matmul(acc[n][:], lhsT=condB[:, k, :],
                                                                     rhs=wb[:, n * 512:(n + 1) * 512],
                                                                     start=(k == 0), stop=(k == 7)),
                         r=[condB, wb], w=[acc[n]])
            for n in range(6):
                c0 = half * 3072 + n * 512
                K.op("dve", lambda e, n=n, c0=c0: e.tensor_tensor(out=MODB[:, c0:c0 + 512], in0=acc[n][:],
                                                                    in1=BAB[:, c0:c0 + 512], op=ALU.add),
                     r=[acc[n], BAB], w=[MODB])
        for j in (1, 2, 4, 5):
            K.op("dve", lambda e, j=j: e.tensor_scalar(out=MODB[:, j * D:(j + 1) * D], in0=MODB[:, j * D:(j + 1) * D],
                                                        scalar1=1.0, scalar2=None, op0=ALU.add), r=[MODB], w=[MODB])
        K.op("dve", lambda e: e.tensor_copy(out=PB[:, 0, :], in_=MODB[:, 2 * D:3 * D]), r=[MODB], w=[PB])
        K.op("dve", lambda e: e.tensor_copy(out=PB[:, 1, :], in_=MODB[:, 5 * D:6 * D]), r=[MODB], w=[PB])
        for i, j in enumerate((0, 1, 3, 4)):
            for k in range(8):
                K.op("dve", lambda e, j=j, k=k: e.tensor_tensor(out=tmp[:, k, :],
                                                                 in0=MODB[:, j * D + k * 128:j * D + (k + 1) * 128],
                                                                 in1=identF[:], op=ALU.mult),
                     r=[MODB, identF], w=[tmp])
            K.op("dve", lambda e, i=i: e.tensor_reduce(out=modT[:, i, :], in_=tmp[:], axis=AX.X, op=ALU.add),
                 r=[tmp], w=[modT])


def phase_inproj(K, nc, I, l, S, xin, identF, identB, modT, sbqT, sbkT, sbv, gpre, gzd, gabd, dqT, dkT, dav):
    import os
    CUT = int(os.environ.get("KCUT", "99"))
    NG = S // 512
    with contextlib.ExitStack() as es:
        W = K.sb(es, "b_W", [128, 8, IN_W], BF)
        wst = [K.sb(es, f"b_wst{i}", [128, IN_W]) for i in range(2)]
        xt = [K.sb(es, f"b_xt{i}", [128, D]) for i in range(2)]
        hT = [K.sb(es, f"b_hT{i}", [128, 8, 512], BF) for i in range(2)]
        ob = [K.sb(es, f"b_ob{i}", [128, 512], BF) for i in range(2)]
        of = [K.sb(es, f"b_of{i}", [128, 512]) for i in range(2)]
        tv = [K.sb(es, f"b_tv{i}", [128, 512], BF) for i in range(2)]
        tq = [K.sb(es, f"b_tq{i}", [128, 512], BF) for i in range(2)]
        xs = [K.sb(es, f"b_xs{i}", [128, 512]) for i in range(2)]
        xsw = K.sb(es, "b_xsw", [128, 512])
        xsw2 = K.sb(es, "b_xsw2", [128, 512])
        tz = [K.sb(es, f"b_tz{i}", [128, 512]) for i in range(2)]
        tab = [K.sb(es, f"b_tab{i}", [128, 8]) for i in range(2)]
        cs = [K.sb(es, f"b_cs{i}", [128, 2, 512]) for i in range(2)]
        dT = [K.sb(es, f"b_dT{i}", [128, 4, 512], BF) for i in range(2)]
        pX = [K.ps(es, f"b_pX{i}", [128, 512]) for i in range(2)]
        pF = [K.ps(es, f"b_pF{i}", [128, 512]) for i in range(2)]
        pA = K.ps(es, "b_pA", [128, 512])
        pB = K.ps(es, "b_pB", [128, 512])
        pC = K.ps(es, "b_pC", [128, 512])
        pD = K.ps(es, "b_pD", [128, 512], BF)
        pE = pX[0]

        K.op("pool", lambda e: e.memset(xsw[:], 0.0), w=[xsw])
        for k in range(8):
            st = wst[k % 2]
            K.dma("sp", st[:], I["w_in"][l, k * 128:(k + 1) * 128, :], w=[st])
            K.op("act" if k % 2 == 0 else "pool",
                 (lambda e, k=k, st=st: e.activation(out=W[:, k, :], in_=st[:], func=AF.Copy)) if k % 2 == 0 else
                 (lambda e, k=k, st=st: e.tensor_copy(out=W[:, k, :], in_=st[:])), r=[st], w=[W])

        if CUT < 1:
            return
        feat_chunks = ([(C_SBQ + j * 128, sbqT, j * 128, True) for j in range(2)] +
                       [(C_SBK + j * 128, sbkT, j * 128, True) for j in range(2)] +
                       [(C_GQ + j * 128, gpre, j * 128, False) for j in range(12)])
        ev = 0
        for g in range(NG):
            h = hT[g % 2]
            dTg = dT[g % 2]
            for tt in range(4):
                ti = g * 4 + tt
                x_ = xt[ti % 2]
                K.dma("sp", x_[:], xin[ti * 128:(ti + 1) * 128, :], w=[x_])
                for half in range(2):
                    p = pX[half]
                    for kk in range(4):
                        k = half * 4 + kk
                        K.op("pe", lambda e, p=p, kk=kk, k=k, x_=x_: e.transpose(
                            out=p[:, kk * 128:(kk + 1) * 128], in_=x_[:, k * 128:(k + 1) * 128], identity=identF[:]),
                            r=[x_, identF], w=[p])
                    for kk in range(4):
                        k = half * 4 + kk
                        K.op("act", lambda e, p=p, kk=kk, k=k, h=h, tt=tt: e.activation(
                            out=h[:, k, tt * 128:(tt + 1) * 128], in_=p[:, kk * 128:(kk + 1) * 128],
                            func=AF.Identity, bias=modT[:, 0, k:k + 1], scale=modT[:, 1, k:k + 1]),
                            r=[p, modT], w=[h])
            if CUT < 2:
                continue
            for ci, (c0, dst, r0, isbf) in enumerate(feat_chunks):
                p = pF[ci % 2]
                for k in range(8):
                    K.op("pe", lambda e, p=p, k=k, c0=c0, h=h: e.matmul(
                        p[:], lhsT=W[:, k, c0:c0 + 128], rhs=h[:, k, :], start=(k == 0), stop=(k == 7)),
                        r=[W, h], w=[p])
                o = (ob if isbf else of)[ev % 2]
                if ev % 2 == 0:
                    K.op("dve", lambda e, o=o, p=p: e.tensor_copy(out=o[:], in_=p[:]), r=[p], w=[o])
                else:
                    K.op("act", lambda e, o=o, p=p: e.activation(out=o[:], in_=p[:], func=AF.Copy), r=[p], w=[o])
                ev += 1
                K.dma("sp", dst[r0:r0 + 128, g * 512:(g + 1) * 512], o[:], r=[o], w=[])
            for tt in range(4):
                if CUT < 3:
                    continue
                ti = g * 4 + tt
                lt = lambda k: h[:, k, tt * 128:(tt + 1) * 128]
                tv_, tq_, tz_, tab_, cs_, xs_ = tv[ti % 2], tq[ti % 2], tz[ti % 2], tab[ti % 2], cs[ti % 2], xs[ti % 2]
                K.dma("sp", cs_[:, 0, :], I["k_cos"][ti * 128:(ti + 1) * 128, :], w=[cs_])
                K.dma("sp", cs_[:, 1, :], I["k_sin"][ti * 128:(ti + 1) * 128, :], w=[cs_])
                for (pp, o0, c0, n) in ((pA, 0, C_SBV, 256), (pA, 256, C_DV, 256), (pB, 0, C_DQ, 512),
                                        (pC, 0, C_GZ, 512), (pE, 0, C_GA, 8)):
                    for k in range(8):
                        K.op("pe", lambda e, pp=pp, o0=o0, c0=c0, n=n, k=k: e.matmul(
                            pp[:, o0:o0 + n], lhsT=lt(k), rhs=W[:, k, c0:c0 + n], start=(k == 0), stop=(k == 7)),
                            r=[W, h], w=[pp])
                K.op("act", lambda e: e.activation(out=tv_[:], in_=pA[:], func=AF.Copy), r=[pA], w=[tv_])
                K.dma("sp", sbv[ti * 128:(ti + 1) * 128, :], tv_[:, 0:256], r=[tv_])
                K.dma("sp", dav[ti * 128:(ti + 1) * 128, :], tv_[:, 256:512], r=[tv_])
                K.op("act", lambda e: e.activation(out=tz_[:], in_=pC[:], func=AF.Copy), r=[pC], w=[tz_])
                K.dma("sp", gzd[ti * 128:(ti + 1) * 128, :], tz_[:], r=[tz_])
                K.op("dve", lambda e: e.tensor_copy(out=tab_[:], in_=pE[:, 0:8]), r=[pE], w=[tab_])
                K.dma("sp", gabd[ti * 128:(ti + 1) * 128, :], tab_[:], r=[tab_])
                if CUT < 4:
                    continue
                K.op("act", lambda e: e.activation(out=xs_[:], in_=pB[:], func=AF.Copy), r=[pB], w=[xs_])
                xv = xs_[:].rearrange("p (g d) -> p g d", d=32)
                wv = xsw[:].rearrange("p (g d) -> p g d", d=32)
                K.op("pool", lambda e: e.tensor_copy(out=wv[:, :, 0:4], in_=xv[:, :, 4:8]), r=[xs_], w=[xsw])
                K.op("pool", lambda e: e.tensor_copy(out=wv[:, :, 4:8], in_=xv[:, :, 0:4]), r=[xs_], w=[xsw])
                K.op("dve", lambda e: e.tensor_tensor(out=xs_[:], in0=xs_[:], in1=cs_[:, 0, :], op=ALU.mult),
                     r=[xs_, cs_], w=[xs_])
                K.op("pool", lambda e: e.tensor_tensor(out=xsw2[:], in0=xsw[:], in1=cs_[:, 1, :], op=ALU.mult),
                     r=[xsw, cs_], w=[xsw2])
                K.op("dve", lambda e: e.tensor_tensor(out=tq_[:], in0=xs_[:], in1=xsw2[:], op=ALU.add),
                     r=[xs_, xsw2], w=[tq_])
                if CUT < 5:
                    continue
                tqf = tq_[:]
                for j in range(4):
                    K.op("pe", lambda e, j=j: e.transpose(out=pD[:, j * 128:(j + 1) * 128],
                                                          in_=tqf[:, j * 128:(j + 1) * 128], identity=identB[:]),
                         r=[tq_, identB], w=[pD])
                K.op("dve", lambda e: e.tensor_copy(
                    out=dTg[:, :, tt * 128:(tt + 1) * 128], in_=pD[:].rearrange("p (j t) -> p j t", t=128)),
                    r=[pD], w=[dTg])
            for j in range(4):
                dst = dqT if j < 2 else dkT
                K.dma("sp", dst[(j % 2) * 128:(j % 2 + 1) * 128, g * 512:(g + 1) * 512], dTg[:, j, :], r=[dTg])


def phase_sb(K, nc, I, S, sbqT, sbkT, sbv, mixT):
    NCH = S // 512
    NB = S // 128
    scale = 64 ** -0.5
    with contextlib.ExitStack() as es:
        msk = K.sb(es, "c_msk", [128, 4, 512])
        mskB = K.sb(es, "c_mskB", [128, 4, 512], BF)
        tri = K.sb(es, "c_tri", [128, 128])
        UsB = K.sb(es, "c_UsB", [128, 128], BF)
        onesB = K.sb(es, "c_onesB", [128, 128], BF)
        V = K.sb(es, "c_V", [128, NB, 256], BF)
        qT = [K.sb(es, f"c_qT{i}", [64, S], BF) for i in range(2)]
        kT = [K.sb(es, f"c_kT{i}", [64, S], BF) for i in range(2)]
        CS = K.sb(es, "c_CS", [128, 512])
        E1 = [K.sb(es, f"c_E1{i}", [128, 512]) for i in range(2)]
        SP = [K.sb(es, f"c_SP{i}", [128, 512]) for i in range(2)]
        SPb = [K.sb(es, f"c_SPb{i}", [128, 512], BF) for i in range(2)]
        t1 = [K.sb(es, f"c_t1{i}", [128, 512]) for i in range(2)]
        t2 = [K.sb(es, f"c_t2{i}", [128, 512]) for i in range(2)]
        Wt = [K.sb(es, f"c_W{i}", [128, 512], BF) for i in range(2)]
        osb = [K.sb(es, f"c_o{i}", [64, 512], BF) for i in range(2)]
        pZ = [K.ps(es, f"c_pZ{i}", [128, 512]) for i in range(2)]
        pBt = [K.ps(es, f"c_pB{i}", [128, 512]) for i in range(2)]
        pCt = [K.ps(es, f"c_pC{i}", [128, 512]) for i in range(2)]
        pO = K.ps(es, "c_pO", [64, 512])

        K.dma("sp", msk[:].rearrange("p r t -> p (r t)"), I["k_msb"][:, :], w=[msk])
        K.dma("sp", tri[:], I["k_tri"][:, 256:384], w=[tri])
        K.dma("sp", V[:], sbv.rearrange("(n p) d -> p n d", p=128), w=[V])
        K.op("dve", lambda e: e.tensor_copy(out=mskB[:], in_=msk[:]), r=[msk], w=[mskB])
        K.op("dve", lambda e: e.tensor_copy(out=UsB[:], in_=tri[:]), r=[tri], w=[UsB])
        K.op("pool", lambda e: e.memset(onesB[:], 1.0), w=[onesB])
        it = 0
        for h in range(4):
            q_, k_ = qT[h % 2], kT[h % 2]
            K.dma("sp", q_[:], sbqT[h * 64:(h + 1) * 64, :], w=[q_])
            K.dma("sp", k_[:], sbkT[h * 64:(h + 1) * 64, :], w=[k_])
            for c in range(NCH):
                K.op("pool", lambda e: e.memset(CS[:], 0.0), w=[CS])
                nblk = 4 * c + 4
                for bi, sbk in enumerate(range(nblk - 1, -1, -1)):
                    r_ = sbk - 4 * c
                    i2 = it % 2
                    it += 1
                    z, pb, pc = pZ[i2], pBt[i2], pCt[i2]
                    e1, sp, spb, a1, a2, w_ = E1[i2], SP[i2], SPb[i2], t1[i2], t2[i2], Wt[i2]
                    K.op("pe", lambda e: e.matmul(z[:], lhsT=k_[:, sbk * 128:(sbk + 1) * 128],
                                                  rhs=q_[:, c * 512:(c + 1) * 512], start=True, stop=True),
                         r=[k_, q_], w=[z])
                    K.op("act", lambda e: e.activation(out=e1[:], in_=z[:], func=AF.Exp, scale=scale), r=[z], w=[e1])
                    K.op("act", lambda e: e.activation(out=sp[:], in_=e1[:], func=AF.Ln, bias=1.0), r=[e1], w=[sp])
                    if r_ >= 0:
                        K.op("pool", lambda e: e.tensor_tensor(out=sp[:], in0=sp[:], in1=msk[:, r_, :], op=ALU.mult),
                             r=[sp, msk], w=[sp])
                    K.op("pool", lambda e: e.tensor_copy(out=spb[:], in_=sp[:]), r=[sp], w=[spb])
                    K.op("pe", lambda e: e.matmul(pb[:], lhsT=UsB[:], rhs=spb[:], start=True, stop=True),
                         r=[UsB, spb], w=[pb])
                    K.op("pe", lambda e: e.matmul(pc[:], lhsT=onesB[:], rhs=spb[:], start=True, stop=True),
                         r=[onesB, spb], w=[pc])
                    K.op("dve", lambda e: e.scalar_tensor_tensor(out=a1[:], in0=z[:], scalar=scale, in1=sp[:],
                                                                 op0=ALU.mult, op1=ALU.subtract), r=[z, sp], w=[a1])
                    K.op("dve", lambda e: e.tensor_tensor(out=a2[:], in0=a1[:], in1=pb[:], op=ALU.subtract),
                         r=[a1, pb], w=[a2])
                    K.op("pool", lambda e: e.tensor_tensor(out=a2[:], in0=a2[:], in1=CS[:], op=ALU.subtract),
                         r=[a2, CS], w=[a2])
                    K.op("act", lambda e: e.activation(out=w_[:], in_=a2[:], func=AF.Exp), r=[a2], w=[w_])
                    if r_ >= 0:
                        K.op("pool", lambda e: e.tensor_tensor(out=w_[:], in0=w_[:], in1=mskB[:, r_, :], op=ALU.mult),
                             r=[w_, mskB], w=[w_])
                    K.op("pe", lambda e: e.matmul(pO[:], lhsT=V[:, sbk, h * 64:(h + 1) * 64], rhs=w_[:],
                                                  start=(bi == 0), stop=(bi == nblk - 1)), r=[V, w_], w=[pO])
                    if bi < nblk - 1:
                        K.op("dve", lambda e: e.tensor_tensor(out=CS[:], in0=CS[:], in1=pc[:], op=ALU.add),
                             r=[CS, pc], w=[CS])
                o_ = osb[(h * NCH + c) % 2]
                K.op("act", lambda e: e.activation(out=o_[:], in_=pO[:], func=AF.Copy), r=[pO], w=[o_])
                K.dma("sp", mixT[h * 64:(h + 1) * 64, c * 512:(c + 1) * 512], o_[:], r=[o_])


def phase_da(K, nc, I, l, S, lam_init, dqT, dkT, dav, mixT):
    NCH = S // 512
    NB = S // 128
    scale = 32 ** -0.5
    with contextlib.ExitStack() as es:
        mskB = K.sb(es, "d_mskB", [128, 4, 512], BF)
        msk = K.sb(es, "d_msk", [128, 4, 512])
        onesB = K.sb(es, "d_onesB", [128, 64], BF)
        onesF = K.sb(es, "d_onesF", [64, 64])
        V = K.sb(es, "d_V", [128, NB, 256], BF)
        qT = [K.sb(es, f"d_qT{i}", [64, S], BF) for i in range(2)]
        kT = [K.sb(es, f"d_kT{i}", [64, S], BF) for i in range(2)]
        Et = [K.sb(es, f"d_E{i}", [128, 512], BF) for i in range(4)]
        lp = K.sb(es, "d_lp", [128, 128])
        lpp = K.sb(es, "d_lpp", [128, 2, 32])
        ls = K.sb(es, "d_ls", [128, 2])
        le = K.sb(es, "d_le", [128, 2])
        nlam = K.sb(es, "d_nlam", [128, 1])
        wcol = K.sb(es, "d_wcol", [64, 1])
        rr = [K.sb(es, f"d_rr{i}", [64, 512]) for i in range(2)]
        oo = [K.sb(es, f"d_oo{i}", [64, 512]) for i in range(2)]
        od = K.sb(es, "d_od", [64, 512])
        sq = K.sb(es, "d_sq", [64, 512])
        rs = K.sb(es, "d_rs", [64, 512])
        ob = [K.sb(es, f"d_ob{i}", [64, 512], BF) for i in range(2)]
        pS = [K.ps(es, f"d_pS{i}", [128, 512]) for i in range(2)]
        pN = [K.ps(es, f"d_pN{i}", [64, 512]) for i in range(2)]
        pDn = [K.ps(es, f"d_pD{i}", [64, 512]) for i in range(2)]
        pQ = K.ps(es, "d_pQ", [64, 512])

        K.dma("sp", msk[:].rearrange("p r t -> p (r t)"), I["k_mda"][:, :], w=[msk])
        K.dma("sp", V[:], dav.rearrange("(n p) d -> p n d", p=128), w=[V])
        K.dma("sp", lp[:], _bcast_rows(I["da_lambda"][l:l + 1, :]), w=[lp])
        K.dma("sp", wcol[:], I["da_subln_w"][l:l + 1, :].rearrange("o d -> d o"), w=[wcol],
              allow_slow_non_contiguous=True)
        K.op("dve", lambda e: e.tensor_copy(out=mskB[:], in_=msk[:]), r=[msk], w=[mskB])
        K.op("pool", lambda e: e.memset(onesB[:], 1.0), w=[onesB])
        K.op("pool", lambda e: e.memset(onesF[:], 1.0), w=[onesF])
        K.op("dve", lambda e: e.tensor_tensor(out=lpp[:, 0, :], in0=lp[:, 0:32], in1=lp[:, 32:64], op=ALU.mult),
             r=[lp], w=[lpp])
        K.op("dve", lambda e: e.tensor_tensor(out=lpp[:, 1, :], in0=lp[:, 64:96], in1=lp[:, 96:128], op=ALU.mult),
             r=[lp], w=[lpp])
        K.op("dve", lambda e: e.tensor_reduce(out=ls[:], in_=lpp[:], axis=AX.X, op=ALU.add), r=[lpp], w=[ls])
        K.op("act", lambda e: e.activation(out=le[:], in_=ls[:], func=AF.Exp), r=[ls], w=[le])
        K.op("dve", lambda e: e.tensor_tensor(out=nlam[:], in0=le[:, 1:2], in1=le[:, 0:1], op=ALU.subtract),
             r=[le], w=[nlam])
        K.op("dve", lambda e: e.tensor_scalar(out=nlam[:], in0=nlam[:], scalar1=-lam_init, scalar2=None, op0=ALU.add),
             r=[nlam], w=[nlam])
        K.op("dve", lambda e: e.tensor_scalar(out=wcol[:], in0=wcol[:], scalar1=(1.0 - lam_init), scalar2=None,
                                              op0=ALU.mult), r=[wcol], w=[wcol])
        it = 0
        for h in range(4):
            q_, k_ = qT[h % 2], kT[h % 2]
            K.dma("sp", q_[:], dqT[h * 64:(h + 1) * 64, :], w=[q_])
            K.dma("sp", k_[:], dkT[h * 64:(h + 1) * 64, :], w=[k_])
            for c in range(NCH):
                nblk = 4 * c + 4
                for bi, sbk in enumerate(range(nblk - 1, -1, -1)):
                    r_ = sbk - 4 * c
                    for m in range(2):
                        ps_ = pS[it % 2]
                        e_ = Et[it % 4]
                        it += 1
                        K.op("pe", lambda e: e.matmul(ps_[:], lhsT=k_[m * 32:(m + 1) * 32, sbk * 128:(sbk + 1) * 128],
                                                      rhs=q_[m * 32:(m + 1) * 32, c * 512:(c + 1) * 512],
                                                      start=True, stop=True), r=[k_, q_], w=[ps_])
                        K.op("act", lambda e: e.activation(out=e_[:], in_=ps_[:], func=AF.Exp, scale=scale),
                             r=[ps_], w=[e_])
                        if r_ >= 0:
                            K.op("pool" if m else "dve",
                                 lambda e: e.tensor_tensor(out=e_[:], in0=e_[:], in1=mskB[:, r_, :], op=ALU.mult),
                                 r=[e_, mskB], w=[e_])
                        K.op("pe", lambda e: e.matmul(pN[m][:], lhsT=V[:, sbk, h * 64:(h + 1) * 64], rhs=e_[:],
                                                      start=(bi == 0), stop=(bi == nblk - 1)), r=[V, e_], w=[pN[m]])
                        K.op("pe", lambda e: e.matmul(pDn[m][:], lhsT=onesB[:], rhs=e_[:],
                                                      start=(bi == 0), stop=(bi == nblk - 1)), r=[onesB, e_], w=[pDn[m]])
                for m in range(2):
                    K.op("act", lambda e: e.activation(out=rr[m][:], in_=pDn[m][:], func=AF.Ln), r=[pDn[m]], w=[rr[m]])
                    K.op("act", lambda e: e.activation(out=rr[m][:], in_=rr[m][:], func=AF.Exp, scale=-1.0),
                         r=[rr[m]], w=[rr[m]])
                    K.op("dve", lambda e: e.tensor_tensor(out=oo[m][:], in0=pN[m][:], in1=rr[m][:], op=ALU.mult),
                         r=[pN[m], rr[m]], w=[oo[m]])
                K.op("dve", lambda e: e.scalar_tensor_tensor(out=od[:], in0=oo[1][:], scalar=nlam[0:64, :], in1=oo[0][:],
                                                             op0=ALU.mult, op1=ALU.add), r=[oo[0], oo[1], nlam], w=[od])
                K.op("pool", lambda e: e.tensor_tensor(out=sq[:], in0=od[:], in1=od[:], op=ALU.mult), r=[od], w=[sq])
                K.op("pe", lambda e: e.matmul(pQ[:], lhsT=onesF[:], rhs=sq[:], start=True, stop=True),
                     r=[onesF, sq], w=[pQ])
                K.op("act", lambda e: e.activation(out=rs[:], in_=pQ[:], func=AF.Ln, scale=1.0 / 64, bias=1e-6),
                     r=[pQ], w=[rs])
                K.op("act", lambda e: e.activation(out=rs[:], in_=rs[:], func=AF.Exp, scale=-0.5), r=[rs], w=[rs])
                o_ = ob[(h * NCH + c) % 2]
                K.op("dve", lambda e: e.scalar_tensor_tensor(out=o_[:], in0=od[:], scalar=wcol[:], in1=rs[:],
                                                             op0=ALU.mult, op1=ALU.mult), r=[od, wcol, rs], w=[o_])
                K.dma("sp", mixT[768 + h * 64:768 + (h + 1) * 64, c * 512:(c + 1) * 512], o_[:], r=[o_])


def phase_gdn(K, nc, I, l, S, identF, gpre, gzd, gabd, mixT):
    NU = S // 128
    with contextlib.ExitStack() as es:
        sbt = lambda n, shp, dt=F32: K.sb(es, "e_" + n, shp, dt)
        tri = sbt("tri", [128, 384])
        onesF = sbt("ones", [128, 128])
        c48 = sbt("c48", [48, 128])
        cw = sbt("cw", [128, 48])
        alog = sbt("alog", [128, 4])
        dtb = sbt("dtb", [128, 4])
        nea = sbt("nea", [128, 4])
        gnw = sbt("gnw", [128, 128])
        St = [sbt(f"St{h}", [128, 128]) for h in range(4)]
        win = [sbt(f"win{i}", [128, 12, 131]) for i in range(2)]
        gab = [sbt(f"gab{i}", [128, 8]) for i in range(2)]
        gz = [sbt(f"gz{i}", [128, 512]) for i in range(2)]
        sm = {n: [sbt(f"{n}{i}", [128, 4]) for i in range(2)] for n in
              ("xg", "ex", "sp", "a", "e2", "beta", "nbeta", "gcol", "eg", "egl", "ekd", "tmp4")}
        names = ("cq", "ck", "cv", "qn", "kn", "vt", "qnT", "knT", "abc", "dec", "decl", "decs", "X0", "X1",
                 "XT0", "XT1", "A", "AT", "wT", "kdec", "vnew", "o", "zs", "on", "junk")
        bt = {n: [sbt(f"{n}{i}", [128, 128]) for i in range(2)] for n in names}
        Yt = [sbt(f"Y{i}", [128, 256]) for i in range(2)]
        obf = [sbt(f"obf{i}", [128, 128], BF) for i in range(2)]
        ssq = [sbt(f"ssq{i}", [128, 4]) for i in range(2)]
        rn = [sbt(f"rn{i}", [128, 4]) for i in range(2)]
        banks = [K.ps(es, f"e_bank{i}", [128, 512]) for i in range(8)]
        playout = (("pq", "pk", "pv", "pc48"), ("pqT", "pkT", "pwT", "pg"), ("KK", "QK", "Grow", "pOT"),
                   ("pAT", "pXT", "p1", "p4"), ("pX2", "pXT2", "p2", "p3"))
        pt = {}
        for bi_, grp in enumerate(playout):
            for si_, n in enumerate(grp):
                pt[n] = TV(banks[bi_], banks[bi_][:, si_ * 128:(si_ + 1) * 128])
        pY = [TV(banks[5], banks[5][:, 0:256]), TV(banks[6], banks[6][:, 0:256])]
        Lf, mlow, mstr = tri[:, 0:128], tri[:, 128:256], tri[:, 256:384]

        K.dma("sp", tri[:], I["k_tri"][:, :], w=[tri])
        K.dma("sp", c48[:], I["conv_w"][l].rearrange("k (j p) -> (k j) p", p=128), w=[c48])
        K.dma("sp", alog[:], _bcast_rows(I["gdn_a_log"][l:l + 1, :]), w=[alog])
        K.dma("sp", dtb[:], _bcast_rows(I["gdn_dt_bias"][l:l + 1, :]), w=[dtb])
        K.dma("sp", gnw[:], _bcast_rows(I["gdn_norm_w"][l:l + 1, :]), w=[gnw])
        K.op("pool", lambda e: e.memset(onesF[:], 1.0), w=[onesF])
        for h in range(4):
            K.op("pool", lambda e, h=h: e.memset(St[h][:], 0.0), w=[St[h]])
        K.op("pe", lambda e: e.transpose(out=pt["pc48"][:, 0:48], in_=c48[:], identity=identF[0:48, 0:48]),
             r=[c48, identF], w=[pt["pc48"]])
        K.op("dve", lambda e: e.tensor_copy(out=cw[:], in_=pt["pc48"][:, 0:48]), r=[pt["pc48"]], w=[cw])
        K.op("act", lambda e: e.activation(out=nea[:], in_=alog[:], func=AF.Exp), r=[alog], w=[nea])
        K.op("dve", lambda e: e.tensor_scalar(out=nea[:], in0=nea[:], scalar1=-1.0, scalar2=None, op0=ALU.mult),
             r=[nea], w=[nea])

        def tr(dst_ps, src, srcT):
            K.op("pe", lambda e: e.transpose(out=dst_ps[:], in_=src, identity=identF[:]), r=[srcT, identF], w=[dst_ps])

        def mm(dst_ps, lhsT, rhs, deps):
            K.op("pe", lambda e: e.matmul(dst_ps[:], lhsT=lhsT, rhs=rhs, start=True, stop=True), r=deps, w=[dst_ps])

        cpy_i = [0]

        def cpy(dst, dstT, src_ps):
            cpy_i[0] += 1
            cm = os.environ.get("KCPY", "dve")
            if (cpy_i[0] % 2 and cm == "alt") or cm == "dve":
                K.op("dve", lambda e: e.tensor_copy(out=dst, in_=src_ps[:]), r=[src_ps], w=[dstT])
            else:
                K.op("act", lambda e: e.activation(out=dst, in_=src_ps[:], func=AF.Copy), r=[src_ps], w=[dstT])

        import os
        CUT3 = int(os.environ.get("KCUT3", "99"))
        for u in range(NU):
            if CUT3 < 1:
                break
            i2 = u % 2
            w_ = win[i2]
            g_ = gab[i2]
            z_ = gz[i2]
            if u == 0:
                K.op("pool", lambda e: e.memset(w_[:, :, 0:3], 0.0), w=[w_])
                K.dma("sp", w_[:, :, 3:131], gpre.rearrange("(j p) s -> p j s", p=128)[:, :, 0:128], w=[w_])
            else:
                K.dma("sp", w_[:], gpre.rearrange("(j p) s -> p j s", p=128)[:, :, u * 128 - 3:u * 128 + 128], w=[w_])
            K.dma("sp", g_[:], gabd[u * 128:(u + 1) * 128, :], w=[g_])
            K.dma("sp", z_[:], gzd[u * 128:(u + 1) * 128, :], w=[z_])
            s = {n: v[i2] for n, v in sm.items()}
            K.op("dve", lambda e: e.tensor_tensor(out=s["xg"][:], in0=g_[:, 0:4], in1=dtb[:], op=ALU.add),
                 r=[g_, dtb], w=[s["xg"]])
            K.op("act", lambda e: e.activation(out=s["ex"][:], in_=s["xg"][:], func=AF.Exp), r=[s["xg"]], w=[s["ex"]])
            K.op("act", lambda e: e.activation(out=s["sp"][:], in_=s["ex"][:], func=AF.Ln, bias=1.0),
                 r=[s["ex"]], w=[s["sp"]])
            K.op("dve", lambda e: e.tensor_tensor(out=s["a"][:], in0=s["sp"][:], in1=nea[:], op=ALU.mult),
                 r=[s["sp"], nea], w=[s["a"]])
            K.op("act", lambda e: e.activation(out=s["e2"][:], in_=g_[:, 4:8], func=AF.Exp, scale=-1.0),
                 r=[g_], w=[s["e2"]])
            K.op("dve", lambda e: e.tensor_scalar(out=s["e2"][:], in0=s["e2"][:], scalar1=1.0, scalar2=None,
                                                  op0=ALU.add), r=[s["e2"]], w=[s["e2"]])
            K.op("dve", lambda e: e.reciprocal(out=s["beta"][:], in_=s["e2"][:]), r=[s["e2"]], w=[s["beta"]])
            K.op("dve", lambda e: e.tensor_scalar(out=s["nbeta"][:], in0=s["beta"][:], scalar1=-1.0, scalar2=None,
                                                  op0=ALU.mult), r=[s["beta"]], w=[s["nbeta"]])
            pg = pt["pg"]
            K.op("pe", lambda e: e.matmul(pg[:, 0:4], lhsT=Lf, rhs=s["a"][:], start=True, stop=True),
                 r=[tri, s["a"]], w=[pg])
            K.op("pe", lambda e: e.matmul(pg[:, 4:8], lhsT=onesF[:], rhs=s["a"][:], start=True, stop=True),
                 r=[onesF, s["a"]], w=[pg])
            K.op("dve", lambda e: e.tensor_copy(out=s["gcol"][:], in_=pg[:, 0:4]), r=[pg], w=[s["gcol"]])
            K.op("act", lambda e: e.activation(out=s["eg"][:], in_=pg[:, 0:4], func=AF.Exp), r=[pg], w=[s["eg"]])
            K.op("act", lambda e: e.activation(out=s["egl"][:], in_=pg[:, 4:8], func=AF.Exp), r=[pg], w=[s["egl"]])
            K.op("dve", lambda e: e.tensor_tensor(out=s["tmp4"][:], in0=pg[:, 4:8], in1=s["gcol"][:], op=ALU.subtract),
                 r=[pg, s["gcol"]], w=[s["tmp4"]])
            K.op("act", lambda e: e.activation(out=s["ekd"][:], in_=s["tmp4"][:], func=AF.Exp),
                 r=[s["tmp4"]], w=[s["ekd"]])
            for h in range(4):
                if CUT3 < 2:
                    break
                b = {n: v[(u * 4 + h) % 2] for n, v in bt.items()}
                Y = Yt[(u * 4 + h) % 2]
                sq_, rn_ = ssq[(u * 4 + h) % 2], rn[(u * 4 + h) % 2]
                hc = slice(h, h + 1)
                for nm, j in (("cq", h), ("ck", 4 + h), ("cv", 8 + h)):
                    t_ = b[nm]
                    K.op("dve", lambda e: e.tensor_scalar(out=t_[:], in0=w_[:, j, 0:128], scalar1=cw[:, j:j + 1],
                                                          scalar2=None, op0=ALU.mult), r=[w_, cw], w=[t_])
                    for k in range(1, 4):
                        if int(os.environ.get("KCUT4", "99")) < 1:
                            continue
                        K.op("dve", lambda e, k=k: e.scalar_tensor_tensor(
                            out=t_[:], in0=w_[:, j, k:k + 128], scalar=cw[:, k * 12 + j:k * 12 + j + 1], in1=t_[:],
                            op0=ALU.mult, op1=ALU.add), r=[w_, cw, t_], w=[t_])
                    if int(os.environ.get("KCUT4", "99")) < 2:
                        continue
                    K.op("act", lambda e: e.activation(out=t_[:], in_=t_[:], func=AF.Silu), r=[t_], w=[t_])
                if CUT3 < 3:
                    continue
                C5 = int(os.environ.get("KCUT5", "99"))
                tr(pt["pq"], b["cq"][:], b["cq"])
                tr(pt["pk"], b["ck"][:], b["ck"])
                tr(pt["pv"], b["cv"][:], b["cv"])
                if C5 < 1:
                    continue
                K.op("act", lambda e: e.activation(out=b["junk"][:], in_=pt["pq"][:], func=AF.Square,
                                                   accum_out=sq_[:, 0:1]), r=[pt["pq"]], w=[b["junk"], sq_])
                K.op("act", lambda e: e.activation(out=b["junk"][:], in_=pt["pk"][:], func=AF.Square,
                                                   accum_out=sq_[:, 1:2]), r=[pt["pk"]], w=[b["junk"], sq_])
                if C5 < 2:
                    continue
                K.op("act", lambda e: e.activation(out=rn_[:, 0:2], in_=sq_[:, 0:2], func=AF.Ln, bias=1e-6),
                     r=[sq_], w=[rn_])
                K.op("act", lambda e: e.activation(out=rn_[:, 0:2], in_=rn_[:, 0:2], func=AF.Exp, scale=-0.5),
                     r=[rn_], w=[rn_])
                if C5 < 3:
                    continue
                K.op("dve", lambda e: e.tensor_scalar(out=b["qn"][:], in0=pt["pq"][:], scalar1=rn_[:, 0:1],
                                                      scalar2=128 ** -0.5, op0=ALU.mult, op1=ALU.mult),
                     r=[pt["pq"], rn_], w=[b["qn"]])
                if int(os.environ.get("KCUT6", "99")) < 1:
                    continue
                K.op("dve", lambda e: e.tensor_scalar(out=b["kn"][:], in0=pt["pk"][:], scalar1=rn_[:, 1:2],
                                                      scalar2=None, op0=ALU.mult), r=[pt["pk"], rn_], w=[b["kn"]])
                if int(os.environ.get("KCUT6", "99")) < 2:
                    continue
                cpy(b["vt"][:], b["vt"], pt["pv"])
                if C5 < 4:
                    continue
                tr(pt["pqT"], b["qn"][:], b["qn"])
                tr(pt["pkT"], b["kn"][:], b["kn"])
                cpy(b["qnT"][:], b["qnT"], pt["pqT"])
                cpy(b["knT"][:], b["knT"], pt["pkT"])
                if C5 < 5:
                    continue
                mm(pt["KK"], b["knT"][:], b["knT"][:], [b["knT"]])
                mm(pt["QK"], b["qnT"][:], b["knT"][:], [b["qnT"], b["knT"]])
                if CUT3 < 4:
                    continue
                K.op("pool", lambda e: e.tensor_scalar(out=b["abc"][:], in0=onesF[:], scalar1=s["a"][:, hc],
                                                       scalar2=None, op0=ALU.mult), r=[onesF, s["a"]], w=[b["abc"]])
                mm(pt["Grow"], b["abc"][:], Lf, [b["abc"], tri])
                K.op("dve", lambda e: e.tensor_scalar(out=b["dec"][:], in0=pt["Grow"][:], scalar1=s["gcol"][:, hc],
                                                      scalar2=0.0, op0=ALU.subtract, op1=ALU.max),
                     r=[pt["Grow"], s["gcol"]], w=[b["dec"]])
                K.op("act", lambda e: e.activation(out=b["dec"][:], in_=b["dec"][:], func=AF.Exp, scale=-1.0),
                     r=[b["dec"]], w=[b["dec"]])
                K.op("pool", lambda e: e.tensor_tensor(out=b["decl"][:], in0=b["dec"][:], in1=mlow, op=ALU.mult),
                     r=[b["dec"], tri], w=[b["decl"]])
                K.op("pool", lambda e: e.tensor_tensor(out=b["decs"][:], in0=b["dec"][:], in1=mstr, op=ALU.mult),
                     r=[b["dec"], tri], w=[b["decs"]])
                K.op("dve", lambda e: e.scalar_tensor_tensor(out=b["X0"][:], in0=pt["KK"][:], scalar=s["nbeta"][:, hc],
                                                             in1=b["decs"][:], op0=ALU.mult, op1=ALU.mult),
                     r=[pt["KK"], s["nbeta"], b["decs"]], w=[b["X0"]])
                K.op("dve", lambda e: e.tensor_tensor(out=b["A"][:], in0=pt["QK"][:], in1=b["decl"][:], op=ALU.mult),
                     r=[pt["QK"], b["decl"]], w=[b["A"]])
                tr(pt["pAT"], b["A"][:], b["A"])
                cpy(b["AT"][:], b["AT"], pt["pAT"])
                tr(pt["pXT"], b["X0"][:], b["X0"])
                cpy(b["XT0"][:], b["XT0"], pt["pXT"])
                K.op("pool", lambda e: e.tensor_scalar(out=Y[:, 0:128], in0=b["vt"][:], scalar1=s["beta"][:, hc],
                                                       scalar2=None, op0=ALU.mult), r=[b["vt"], s["beta"]], w=[Y])
                K.op("dve", lambda e: e.tensor_scalar(out=Y[:, 128:256], in0=b["kn"][:], scalar1=s["beta"][:, hc],
                                                      scalar2=s["eg"][:, hc], op0=ALU.mult, op1=ALU.mult),
                     r=[b["kn"], s["beta"], s["eg"]], w=[Y])
                if CUT3 < 5:
                    continue
                for k in range(7):
                    Xk, XTk = b[f"X{k % 2}"], b[f"XT{k % 2}"]
                    Xn, XTn = b[f"X{(k + 1) % 2}"], b[f"XT{(k + 1) % 2}"]
                    py = pY[k % 2]
                    K.op("pe", lambda e: e.matmul(py[:], lhsT=XTk[:], rhs=Y[:], start=True, stop=True),
                         r=[XTk, Y], w=[py])
                    if k < 6:
                        mm(pt["pX2"], XTk[:], Xk[:], [XTk, Xk])
                        mm(pt["pXT2"], Xk[:], XTk[:], [XTk, Xk])
                    K.op("dve", lambda e: e.tensor_tensor(out=Y[:], in0=Y[:], in1=py[:], op=ALU.add), r=[Y, py], w=[Y])
                    if k < 6:
                        K.op("act", lambda e: e.activation(out=Xn[:], in_=pt["pX2"][:], func=AF.Copy),
                             r=[pt["pX2"]], w=[Xn])
                        K.op("pool" if False else "act", lambda e: e.activation(out=XTn[:], in_=pt["pXT2"][:],
                                                                                 func=AF.Copy),
                             r=[pt["pXT2"]], w=[XTn])
                tr(pt["pwT"], Y[:, 128:256], Y)
                cpy(b["wT"][:], b["wT"], pt["pwT"])
                K.op("pool", lambda e: e.tensor_scalar(out=b["kdec"][:], in0=b["kn"][:], scalar1=s["ekd"][:, hc],
                                                       scalar2=None, op0=ALU.mult), r=[b["kn"], s["ekd"]], w=[b["kdec"]])
                if CUT3 < 6:
                    continue
                mm(pt["p1"], b["wT"][:], St[h][:], [b["wT"], St[h]])
                K.op("dve", lambda e: e.tensor_tensor(out=b["vnew"][:], in0=Y[:, 0:128], in1=pt["p1"][:],
                                                      op=ALU.subtract), r=[Y, pt["p1"]], w=[b["vnew"]])
                mm(pt["p2"], b["qnT"][:], St[h][:], [b["qnT"], St[h]])
                mm(pt["p3"], b["AT"][:], b["vnew"][:], [b["AT"], b["vnew"]])
                mm(pt["p4"], b["kdec"][:], b["vnew"][:], [b["kdec"], b["vnew"]])
                K.op("dve", lambda e: e.scalar_tensor_tensor(out=St[h][:], in0=St[h][:], scalar=s["egl"][:, hc],
                                                             in1=pt["p4"][:], op0=ALU.mult, op1=ALU.add),
                     r=[St[h], s["egl"], pt["p4"]], w=[St[h]])
                K.op("dve", lambda e: e.tensor_scalar(out=b["o"][:], in0=pt["p2"][:], scalar1=s["eg"][:, hc],
                                                      scalar2=None, op0=ALU.mult), r=[pt["p2"], s["eg"]], w=[b["o"]])
                K.op("dve", lambda e: e.tensor_tensor(out=b["o"][:], in0=b["o"][:], in1=pt["p3"][:], op=ALU.add),
                     r=[b["o"], pt["p3"]], w=[b["o"]])
                K.op("act", lambda e: e.activation(out=b["junk"][:], in_=b["o"][:], func=AF.Square,
                                                   accum_out=sq_[:, 2:3]), r=[b["o"]], w=[b["junk"], sq_])
                K.op("act", lambda e: e.activation(out=rn_[:, 2:3], in_=sq_[:, 2:3], func=AF.Ln, scale=1.0 / 128,
                                                   bias=1e-6), r=[sq_], w=[rn_])
                K.op("act", lambda e: e.activation(out=rn_[:, 2:3], in_=rn_[:, 2:3], func=AF.Exp, scale=-0.5),
                     r=[rn_], w=[rn_])
                K.op("act", lambda e: e.activation(out=b["zs"][:], in_=z_[:, h * 128:(h + 1) * 128], func=AF.Silu),
                     r=[z_], w=[b["zs"]])
                K.op("dve", lambda e: e.scalar_tensor_tensor(out=b["on"][:], in0=b["o"][:], scalar=rn_[:, 2:3],
                                                             in1=gnw[:], op0=ALU.mult, op1=ALU.mult),
                     r=[b["o"], rn_, gnw], w=[b["on"]])
                K.op("pool", lambda e: e.tensor_tensor(out=b["on"][:], in0=b["on"][:], in1=b["zs"][:], op=ALU.mult),
                     r=[b["on"], b["zs"]], w=[b["on"]])
                tr(pt["pOT"], b["on"][:], b["on"])
                ob_ = obf[(u * 4 + h) % 2]
                K.op("dve", lambda e: e.tensor_copy(out=ob_[:], in_=pt["pOT"][:]), r=[pt["pOT"]], w=[ob_])
                K.dma("sp", mixT[256 + h * 128:256 + (h + 1) * 128, u * 128:(u + 1) * 128], ob_[:], r=[ob_])


def _ln_tile(K, z, xo, PB, gi, bi, st, mv, rs, nmr, xn):
    for hh in range(2):
        K.op("dve", lambda e, hh=hh: e.bn_stats(out=st[:, hh, :], in_=z[:, hh * 512:(hh + 1) * 512]), r=[z], w=[st])
    K.op("dve", lambda e: e.bn_aggr(out=mv[:], in_=st[:].rearrange("p a b -> p (a b)")), r=[st], w=[mv])
    K.op("act", lambda e: e.activation(out=rs[:], in_=mv[:, 1:2], func=AF.Ln, bias=1e-5), r=[mv], w=[rs])
    K.op("act", lambda e: e.activation(out=rs[:], in_=rs[:], func=AF.Exp, scale=-0.5), r=[rs], w=[rs])
    K.op("dve", lambda e: e.scalar_tensor_tensor(out=nmr[:], in0=mv[:, 0:1], scalar=-1.0, in1=rs[:],
                                                 op0=ALU.mult, op1=ALU.mult), r=[mv, rs], w=[nmr])
    K.op("act", lambda e: e.activation(out=xn[:], in_=z[:], func=AF.Identity, bias=nmr[:], scale=rs[:]),
         r=[z, nmr, rs], w=[xn])
    K.op("pool", lambda e: e.tensor_tensor(out=xn[:], in0=xn[:], in1=PB[:, gi, :], op=ALU.mult), r=[xn, PB], w=[xn])
    K.op("pool", lambda e: e.tensor_tensor(out=xo[:], in0=xn[:], in1=PB[:, bi, :], op=ALU.add), r=[xn, PB], w=[xo])


def phase_outproj(K, nc, I, l, S, xin, x1, mixT, PB):
    NG = S // 512
    with contextlib.ExitStack() as es:
        W = K.sb(es, "f_W", [128, 8, D], BF)
        wst = [K.sb(es, f"f_wst{i}", [128, D]) for i in range(2)]
        mt = [K.sb(es, f"f_mt{i}", [128, 8, 512], BF) for i in range(2)]
        xt = [K.sb(es, f"f_xt{i}", [128, D]) for i in range(2)]
        z = [K.sb(es, f"f_z{i}", [128, D]) for i in range(2)]
        xn = [K.sb(es, f"f_xn{i}", [128, D]) for i in range(2)]
        xo = [K.sb(es, f"f_xo{i}", [128, D]) for i in range(2)]
        st = [K.sb(es, f"f_st{i}", [128, 2, 6]) for i in range(2)]
        mv = [K.sb(es, f"f_mv{i}", [128, 2]) for i in range(2)]
        rs = [K.sb(es, f"f_rs{i}", [128, 1]) for i in range(2)]
        nmr = [K.sb(es, f"f_nmr{i}", [128, 1]) for i in range(2)]
        ps = [K.ps(es, f"f_ps{i}", [128, 512]) for i in range(4)]
        for k in range(8):
            s_ = wst[k % 2]
            K.dma("sp", s_[:], I["w_out"][l, k * 128:(k + 1) * 128, :], w=[s_])
            K.op("act", lambda e, k=k, s_=s_: e.activation(out=W[:, k, :], in_=s_[:], func=AF.Copy), r=[s_], w=[W])
        for g in range(NG):
            m_ = mt[g % 2]
            K.dma("sp", m_[:], mixT.rearrange("(k p) s -> p k s", p=128)[:, :, g * 512:(g + 1) * 512], w=[m_])
            for tt in range(4):
                ti = g * 4 + tt
                i2 = ti % 2
                K.dma("sp", xt[i2][:], xin[ti * 128:(ti + 1) * 128, :], w=[xt[i2]])
                for hh in range(2):
                    p = ps[i2 * 2 + hh]
                    for k in range(8):
                        K.op("pe", lambda e, p=p, k=k, hh=hh: e.matmul(
                            p[:], lhsT=m_[:, k, tt * 128:(tt + 1) * 128], rhs=W[:, k, hh * 512:(hh + 1) * 512],
                            start=(k == 0), stop=(k == 7)), r=[m_, W], w=[p])
                    K.op("dve", lambda e, p=p, hh=hh: e.tensor_tensor(
                        out=z[i2][:, hh * 512:(hh + 1) * 512], in0=p[:], in1=PB[:, 0, hh * 512:(hh + 1) * 512],
                        op=ALU.mult), r=[p, PB], w=[z[i2]])
                K.op("dve", lambda e: e.scalar_tensor_tensor(out=z[i2][:], in0=xt[i2][:], scalar=DN_ALPHA, in1=z[i2][:],
                                                             op0=ALU.mult, op1=ALU.add), r=[xt[i2], z[i2]], w=[z[i2]])
                _ln_tile(K, z[i2], xo[i2], PB, 2, 3, st[i2], mv[i2], rs[i2], nmr[i2], xn[i2])
                K.dma("sp", x1[ti * 128:(ti + 1) * 128, :], xo[i2][:], r=[xo[i2]])


def phase_moe(K, nc, I, l, S, x1, xout, identF, PB, modT):
    TC = min(1024, S)
    NCH = S // TC
    NTT = TC // 128
    NH = TC // 512
    with contextlib.ExitStack() as es:
        sbt = lambda n, shp, dt=F32: K.sb(es, "g_" + n, shp, dt)
        Wg = sbt("Wg", [128, 8, D], BF)
        Wl = sbt("Wl", [128, 8, D], BF)
        Wd = sbt("Wd", [128, 8, D], BF)
        stg = [sbt(f"stg{i}", [128, 2 * D]) for i in range(2)]
        hT = sbt("hT", [128, 8, TC], BF)
        ysb = sbt("ysb", [128, NTT, D])
        act = sbt("act", [128, 8, TC], BF)
        xg = [sbt(f"xg{i}", [128, 512]) for i in range(2)]
        sg = [sbt(f"sg{i}", [128, 512]) for i in range(2)]
        xl = [sbt(f"xl{i}", [128, 512]) for i in range(2)]
        xt = [sbt(f"xt{i}", [128, D]) for i in range(2)]
        h32 = sbt("h32", [128, 8, 128])
        wr = sbt("wr", [128, 8, NE])
        brB = sbt("brB", [128, NE])
        bd = sbt("bd", [NE, D])
        bgR = sbt("bgR", [128, 2, 256])
        bgT = sbt("bgT", [128, 2, 256])
        lg = sbt("lg", [128, NE])
        t8 = sbt("t8", [128, 8])
        nmx = sbt("nmx", [128, 1])
        msk = sbt("msk", [128, NE])
        exs = sbt("exs", [128, NE])
        ssum = sbt("ssum", [128, 1])
        gates = sbt("gates", [128, NTT, NE])
        gT = sbt("gT", [NE, 128])
        z = sbt("z", [128, D])
        xn = sbt("xn", [128, D])
        xo = [sbt(f"xo{i}", [128, D]) for i in range(2)]
        st = sbt("st", [128, 2, 6])
        mv = sbt("mv", [128, 2])
        rs = sbt("rs", [128, 1])
        nmr = sbt("nmr", [128, 1])
        pG = [K.ps(es, f"g_pG{i}", [128, 512]) for i in range(2)]
        pL = [K.ps(es, f"g_pL{i}", [128, 512]) for i in range(2)]
        pD = [K.ps(es, f"g_pD{i}", [128, 512]) for i in range(2)]

        K.dma("sp", wr[:], I["w_router"][l].rearrange("(k p) e -> p k e", p=128), w=[wr])
        K.dma("sp", brB[:], _bcast_rows(I["b_router"][l:l + 1, :]), w=[brB])
        K.dma("sp", bd[:], I["b_down"][l, :, :], w=[bd])
        K.dma("sp", bgR[:], I["b_gu"][l].rearrange("(q e) (f c) -> (e f) q c", q=2, c=256), w=[bgR])
        for q in range(2):
            for two in range(2):
                K.op("pe", lambda e, q=q, two=two: e.transpose(
                    out=pG[0][:, (q * 2 + two) * 128:(q * 2 + two + 1) * 128],
                    in_=bgR[:, q, two::2], identity=identF[:]), r=[bgR, identF], w=[pG[0]])
        for q in range(2):
            for two in range(2):
                K.op("dve", lambda e, q=q, two=two: e.tensor_copy(
                    out=bgT[:, two, q * 128:(q + 1) * 128],
                    in_=pG[0][:, (q * 2 + two) * 128:(q * 2 + two + 1) * 128]), r=[pG[0]], w=[bgT])

        def load_gu(e):
            for k in range(8):
                s_ = stg[k % 2]
                K.dma("sp", s_[:], I["w_gu"][l, e, k * 128:(k + 1) * 128, :], w=[s_])
                K.op("act", lambda en, k=k, s_=s_: en.activation(out=Wg[:, k, :], in_=s_[:, 0::2], func=AF.Copy),
                     r=[s_], w=[Wg])
                K.op("act", lambda en, k=k, s_=s_: en.activation(out=Wl[:, k, :], in_=s_[:, 1::2], func=AF.Copy),
                     r=[s_], w=[Wl])

        def load_d(e):
            for k2 in range(4):
                s_ = stg[k2 % 2]
                K.dma("sp", s_[:].rearrange("p (a d) -> p a d", a=2),
                      I["w_down"][l, e, k2 * 256:(k2 + 1) * 256, :].rearrange("(a p) d -> p a d", p=128), w=[s_])
                K.op("pool", lambda en, k2=k2, s_=s_: en.tensor_copy(
                    out=Wd[:, 2 * k2:2 * k2 + 2, :], in_=s_[:].rearrange("p (a d) -> p a d", a=2)), r=[s_], w=[Wd])

        for ch in range(NCH):
            t0 = ch * NTT
            for tt in range(NTT):
                ti = t0 + tt
                x_ = xt[ti % 2]
                K.dma("sp", x_[:], x1[ti * 128:(ti + 1) * 128, :], w=[x_])
                for hh in range(2):
                    p = pG[hh]
                    for kk in range(4):
                        k = hh * 4 + kk
                        K.op("pe", lambda e, p=p, kk=kk, k=k: e.transpose(
                            out=p[:, kk * 128:(kk + 1) * 128], in_=x_[:, k * 128:(k + 1) * 128], identity=identF[:]),
                            r=[x_, identF], w=[p])
                    for kk in range(4):
                        k = hh * 4 + kk
                        K.op("act", lambda e, p=p, kk=kk, k=k: e.activation(
                            out=h32[:, k, :], in_=p[:, kk * 128:(kk + 1) * 128], func=AF.Identity,
                            bias=modT[:, 2, k:k + 1], scale=modT[:, 3, k:k + 1]), r=[p, modT], w=[h32])
                K.op("pool", lambda e: e.tensor_copy(out=hT[:, :, tt * 128:(tt + 1) * 128], in_=h32[:]), r=[h32], w=[hT])
                for k in range(8):
                    K.op("pe", lambda e, k=k: e.matmul(pL[0][:, 0:NE], lhsT=h32[:, k, :], rhs=wr[:, k, :],
                                                       start=(k == 0), stop=(k == 7)), r=[h32, wr], w=[pL[0]])
                K.op("dve", lambda e: e.tensor_tensor(out=lg[:], in0=pL[0][:, 0:NE], in1=brB[:], op=ALU.add),
                     r=[pL[0], brB], w=[lg])
                K.op("dve", lambda e: e.max(out=t8[:], in_=lg[:]), r=[lg], w=[t8])
                K.op("dve", lambda e: e.tensor_scalar(out=msk[:], in0=lg[:], scalar1=t8[:, 3:4], scalar2=None,
                                                      op0=ALU.is_ge), r=[lg, t8], w=[msk])
                K.op("dve", lambda e: e.tensor_scalar(out=nmx[:], in0=t8[:, 0:1], scalar1=-1.0, scalar2=None,
                                                      op0=ALU.mult), r=[t8], w=[nmx])
                K.op("act", lambda e: e.activation(out=exs[:], in_=lg[:], func=AF.Exp, bias=nmx[:]), r=[lg, nmx], w=[exs])
                K.op("dve", lambda e: e.tensor_tensor(out=exs[:], in0=exs[:], in1=msk[:], op=ALU.mult),
                     r=[exs, msk], w=[exs])
                K.op("dve", lambda e: e.tensor_reduce(out=ssum[:], in_=exs[:], axis=AX.X, op=ALU.add), r=[exs], w=[ssum])
                K.op("dve", lambda e: e.reciprocal(out=ssum[:], in_=ssum[:]), r=[ssum], w=[ssum])
                K.op("dve", lambda e: e.tensor_scalar(out=gates[:, tt, :], in0=exs[:], scalar1=ssum[:], scalar2=None,
                                                      op0=ALU.mult), r=[exs, ssum], w=[gates])
                K.op("pe", lambda e: e.transpose(out=pL[1][0:NE, 0:128], in_=gates[:, tt, :], identity=identF[:]),
                     r=[gates, identF], w=[pL[1]])
                K.op("dve", lambda e: e.tensor_copy(out=gT[:], in_=pL[1][0:NE, 0:128]), r=[pL[1]], w=[gT])
                for hh in range(2):
                    K.op("pe", lambda e, hh=hh: e.matmul(pD[hh][:], lhsT=gT[:], rhs=bd[:, hh * 512:(hh + 1) * 512],
                                                         start=True, stop=True), r=[gT, bd], w=[pD[hh]])
                    K.op("dve", lambda e, hh=hh: e.tensor_copy(out=ysb[:, tt, hh * 512:(hh + 1) * 512], in_=pD[hh][:]),
                         r=[pD[hh]], w=[ysb])
            load_gu(0)
            it = 0
            for ex in range(NE):
                load_d(ex)
                q, ei = ex // 16, (ex % 16) * 8
                for hf in range(NH):
                    for fc in range(8):
                        i2 = it % 2
                        it += 1
                        for (p, Wm) in ((pG[i2], Wg), (pL[i2], Wl)):
                            for k in range(8):
                                K.op("pe", lambda e, p=p, Wm=Wm, k=k: e.matmul(
                                    p[:], lhsT=Wm[:, k, fc * 128:(fc + 1) * 128], rhs=hT[:, k, hf * 512:(hf + 1) * 512],
                                    start=(k == 0), stop=(k == 7)), r=[Wm, hT], w=[p])
                        bcol = q * 128 + ei + fc
                        K.op("dve", lambda e: e.tensor_scalar(out=xg[i2][:], in0=pG[i2][:],
                                                              scalar1=bgT[:, 0, bcol:bcol + 1], scalar2=7.0,
                                                              op0=ALU.add, op1=ALU.min), r=[pG[i2], bgT], w=[xg[i2]])
                        K.op("act", lambda e: e.activation(out=sg[i2][:], in_=xg[i2][:], func=AF.Sigmoid, scale=1.702),
                             r=[xg[i2]], w=[sg[i2]])
                        K.op("dve", lambda e: e.tensor_scalar(out=xl[i2][:], in0=pL[i2][:],
                                                              scalar1=bgT[:, 1, bcol:bcol + 1], scalar2=7.0,
                                                              op0=ALU.add, op1=ALU.min), r=[pL[i2], bgT], w=[xl[i2]])
                        K.op("pool", lambda e: e.tensor_scalar(out=xl[i2][:], in0=xl[i2][:], scalar1=-7.0, scalar2=1.0,
                                                               op0=ALU.max, op1=ALU.add), r=[xl[i2]], w=[xl[i2]])
                        K.op("pool", lambda e: e.tensor_tensor(out=xg[i2][:], in0=xg[i2][:], in1=sg[i2][:], op=ALU.mult),
                             r=[xg[i2], sg[i2]], w=[xg[i2]])
                        K.op("pool", lambda e: e.tensor_tensor(out=act[:, fc, hf * 512:(hf + 1) * 512], in0=xg[i2][:],
                                                               in1=xl[i2][:], op=ALU.mult),
                             r=[xg[i2], xl[i2]], w=[act])
                if ex + 1 < NE:
                    load_gu(ex + 1)
                for tt in range(NTT):
                    for dh in range(2):
                        p = pD[(tt * 2 + dh) % 2]
                        for fc in range(8):
                            K.op("pe", lambda e, p=p, fc=fc: e.matmul(
                                p[:], lhsT=act[:, fc, tt * 128:(tt + 1) * 128], rhs=Wd[:, fc, dh * 512:(dh + 1) * 512],
                                start=(fc == 0), stop=(fc == 7)), r=[act, Wd], w=[p])
                        K.op("dve", lambda e, p=p: e.scalar_tensor_tensor(
                            out=ysb[:, tt, dh * 512:(dh + 1) * 512], in0=p[:], scalar=gates[:, tt, ex:ex + 1],
                            in1=ysb[:, tt, dh * 512:(dh + 1) * 512], op0=ALU.mult, op1=ALU.add),
                            r=[p, gates, ysb], w=[ysb])
            for tt in range(NTT):
                ti = t0 + tt
                x_ = xt[ti % 2]
                K.dma("sp", x_[:], x1[ti * 128:(ti + 1) * 128, :], w=[x_])
                K.op("dve", lambda e: e.tensor_tensor(out=z[:], in0=ysb[:, tt, :], in1=PB[:, 1, :], op=ALU.mult),
                     r=[ysb, PB], w=[z])
                K.op("dve", lambda e: e.scalar_tensor_tensor(out=z[:], in0=x_[:], scalar=DN_ALPHA, in1=z[:],
                                                             op0=ALU.mult, op1=ALU.add), r=[x_, z], w=[z])
                o_ = xo[ti % 2]
                _ln_tile(K, z, o_, PB, 4, 5, st, mv, rs, nmr, xn)
                K.dma("sp", xout[ti * 128:(ti + 1) * 128, :], o_[:], r=[o_])


def _consts(S):
    k = {}
    k["k_ident"] = np.eye(128, dtype=np.float32)
    r = np.arange(128)
    Lf = (r[:, None] <= r[None, :]).astype(np.float32)
    mlow = (r[:, None] >= r[None, :]).astype(np.float32)
    mstr = (r[:, None] > r[None, :]).astype(np.float32)
    k["k_tri"] = np.ascontiguousarray(np.concatenate([Lf, mlow, mstr], 1))
    t = np.arange(512)
    k["k_msb"] = np.ascontiguousarray(np.concatenate(
        [((128 * rr + r[:, None]) < t[None, :]).astype(np.float32) for rr in range(4)], 1))
    k["k_mda"] = np.ascontiguousarray(np.concatenate(
        [((128 * rr + r[:, None]) <= t[None, :]).astype(np.float32) for rr in range(4)], 1))
    pos = np.arange(S, dtype=np.float32)
    inv = (500000.0 ** (-np.arange(0, 8, 2, dtype=np.float32) / 8)).astype(np.float32)
    ang = pos[:, None] * inv[None, :]
    cf = np.ones((S, 16, 32), np.float32)
    sf = np.zeros((S, 16, 32), np.float32)
    co, si = np.cos(ang).astype(np.float32), np.sin(ang).astype(np.float32)
    cf[:, :, 0:4] = co[:, None, :]
    cf[:, :, 4:8] = co[:, None, :]
    sf[:, :, 0:4] = -si[:, None, :]
    sf[:, :, 4:8] = si[:, None, :]
    k["k_cos"] = np.ascontiguousarray(cf.reshape(S, 512))
    k["k_sin"] = np.ascontiguousarray(sf.reshape(S, 512))
    return k


_NC_CACHE = {}


def kernel(**inputs):
    x = np.asarray(inputs["x"], dtype=np.float32)
    B, S, _ = x.shape
    depth = int(np.asarray(inputs["w_in"]).shape[0])
    key = (S, depth)
    if key not in _NC_CACHE:
        _NC_CACHE[key] = build_program(S=S, depth=depth)
    nc = _NC_CACHE[key]
    shared = {}
    for k_, v in inputs.items():
        if k_ in ("x", "c"):
            continue
        v = np.ascontiguousarray(np.asarray(v, dtype=np.float32))
        if k_ == "da_lambda":
            v = np.ascontiguousarray(v.reshape(v.shape[0], 128))
        shared[k_] = v
    shared.update(_consts(S))
    c = np.asarray(inputs["c"], dtype=np.float32)
    in_maps = []
    for b in range(B):
        m = dict(shared)
        m["x"] = np.ascontiguousarray(x[b])
        m["c"] = np.ascontiguousarray(c[b].reshape(8, 128))
        in_maps.append(m)
    res = run_bass_kernel_spmd(nc, in_maps, core_ids=list(range(B)))
    return np.stack([np.asarray(r["out"], dtype=np.float32) for r in res.results], axis=0)
```

```python
import contextlib
import math
import numpy as np
import concourse.bass as bass
import concourse.mybir as mybir
from concourse.bass_utils import run_bass_kernel_spmd

F32 = mybir.dt.float32
BF = mybir.dt.bfloat16
AF = mybir.ActivationFunctionType
ALU = mybir.AluOpType
AX = mybir.AxisListType

D = 1024
DEPTH = 4
SEQ = 4096
NE = 32
IN_W = 3592
DN_ALPHA = (2 * DEPTH) ** 0.25
C_SBQ, C_SBK, C_SBV, C_GQ, C_GK, C_GV, C_GZ, C_GA, C_GB, C_DQ, C_DK, C_DV = (
    0, 256, 512, 768, 1280, 1792, 2304, 2816, 2820, 2824, 3080, 3336)


class T:
    __slots__ = ("h", "w", "r")

    def __init__(self, h):
        self.h = h
        self.w = None
        self.r = {}

    def __getitem__(self, k):
        return self.h[k]


class TV:
    __slots__ = ("h", "p")

    def __init__(self, parent, ap):
        self.h = ap
        self.p = parent

    def __getitem__(self, k):
        return self.h[k]


def _roots(bs):
    return [getattr(b, "p", b) for b in bs]


class KB:
    def __init__(self, nc, es):
        self.nc = nc
        self.es = es
        self.E = {}
        for name, e in (("pe", nc.tensor), ("dve", nc.vector), ("act", nc.scalar), ("pool", nc.gpsimd),
                        ("sp", nc.sync)):
            sem = es.enter_context(nc.semaphore("c_" + name))
            self.E[name] = {"eng": e, "sem": sem, "cnt": 0, "known": {}}
        self.rings = {}
        for q in ("sp", "pool", "act"):
            self.rings[q] = {"i": 0, "slots": [[es.enter_context(nc.semaphore(f"d_{q}{i}")), 0] for i in range(12)]}
        self.all_dma = {}
        self.ninst = 0

    def _waits(self, en, r, w, extra=()):
        E = self.E[en]
        deps = {}

        def add(t):
            k = id(t[0])
            if k not in deps or deps[k][1] < t[1]:
                deps[k] = t
        for b in r:
            if b.w:
                add(b.w)
        for b in w:
            if b.w:
                add(b.w)
            for d in b.r.values():
                add(d)
        for t in extra:
            add(t)
        for k, (sem, val) in deps.items():
            if en == "pe" and sem is E["sem"]:
                continue
            if E["known"].get(k, 0) < val:
                E["eng"].wait_ge(sem, val)
                E["known"][k] = val
                self.ninst += 1

    def op(self, en, fn, r=(), w=()):
        E = self.E[en]
        r, w = _roots(r), _roots(w)
        self._waits(en, r, w)
        inst = fn(E["eng"])
        E["cnt"] += 1
        inst.then_inc(E["sem"], 1)
        self.ninst += 1
        tok = (E["sem"], E["cnt"])
        k = id(E["sem"])
        for b in r:
            b.r[k] = tok
        for b in w:
            b.w = tok
            b.r = {}

    def dma(self, q, out, in_, r=(), w=(), **kw):
        E = self.E[q]
        r, w = _roots(r), _roots(w)
        ring = self.rings[q]
        slot = ring["slots"][ring["i"]]
        ring["i"] = (ring["i"] + 1) % len(ring["slots"])
        extra = [(slot[0], slot[1])] if slot[1] else []
        self._waits(q, r, w, extra)
        inst = E["eng"].dma_start(out=out, in_=in_, **kw)
        slot[1] += 16
        inst.then_inc(slot[0], 16)
        self.ninst += 1
        tok = (slot[0], slot[1])
        k = id(slot[0])
        for b in r:
            b.r[k] = tok
        for b in w:
            b.w = tok
            b.r = {}
        self.all_dma[k] = tok

    def barrier(self):
        toks = [(E["sem"], E["cnt"]) for E in self.E.values() if E["cnt"]]
        toks += list(self.all_dma.values())
        for en, E in self.E.items():
            for sem, val in toks:
                if E["known"].get(id(sem), 0) < val:
                    E["eng"].wait_ge(sem, val)
                    E["known"][id(sem)] = val
                    self.ninst += 1

    def sb(self, es, name, shape, dt=F32):
        self.uid = getattr(self, "uid", 0) + 1
        return T(es.enter_context(self.nc.sbuf_tensor(f"{name}_{self.uid}", list(shape), dt)))

    def ps(self, es, name, shape, dt=F32):
        self.uid = getattr(self, "uid", 0) + 1
        return T(es.enter_context(self.nc.psum_tensor(f"{name}_{self.uid}", list(shape), dt)))


def _bcast_rows(ap_row, n=128):
    return ap_row.partition_broadcast(n)


def build_program(S=SEQ, depth=DEPTH, debug=False, phases=None):
    nc = bass.Bass("TRN2", target_bir_lowering=False)
    NT = S // 128
    NG = S // 512
    dbg_kind = "ExternalOutput" if debug else "Internal"

    def din(name, shape, dt=F32):
        return nc.dram_tensor(name, list(shape), dt, kind="ExternalInput").ap()

    def dscr(name, shape, dt=F32):
        return nc.dram_tensor(name, list(shape), dt, kind=dbg_kind).ap()

    I = {}
    I["x"] = din("x", [S, D])
    I["c"] = din("c", [8, 128])
    I["w_ada"] = din("w_ada", [depth, D, 6 * D])
    I["b_ada"] = din("b_ada", [depth, 6 * D])
    I["w_in"] = din("w_in", [depth, D, IN_W])
    I["conv_w"] = din("conv_w", [depth, 4, 1536])
    I["gdn_a_log"] = din("gdn_a_log", [depth, 4])
    I["gdn_dt_bias"] = din("gdn_dt_bias", [depth, 4])
    I["gdn_norm_w"] = din("gdn_norm_w", [depth, 128])
    I["da_lambda"] = din("da_lambda", [depth, 128])
    I["da_subln_w"] = din("da_subln_w", [depth, 64])
    I["w_out"] = din("w_out", [depth, D, D])
    for n in ("ln1_g", "ln1_b", "ln2_g", "ln2_b"):
        I[n] = din(n, [depth, D])
    I["w_router"] = din("w_router", [depth, D, NE])
    I["b_router"] = din("b_router", [depth, NE])
    I["w_gu"] = din("w_gu", [depth, NE, D, 2 * D])
    I["b_gu"] = din("b_gu", [depth, NE, 2 * D])
    I["w_down"] = din("w_down", [depth, NE, D, D])
    I["b_down"] = din("b_down", [depth, NE, D])
    I["k_ident"] = din("k_ident", [128, 128])
    I["k_tri"] = din("k_tri", [128, 3 * 128])
    I["k_msb"] = din("k_msb", [128, 4 * 512])
    I["k_mda"] = din("k_mda", [128, 4 * 512])
    I["k_cos"] = din("k_cos", [S, 512])
    I["k_sin"] = din("k_sin", [S, 512])
    out = nc.dram_tensor("out", [S, D], F32, kind="ExternalOutput").ap()

    X = [I["x"]] + [dscr(f"xs{l}", [S, D]) for l in range(depth - 1)] + [out]
    x1 = dscr("x1", [S, D])
    sbqT = dscr("sbqT", [256, S], BF)
    sbkT = dscr("sbkT", [256, S], BF)
    sbv = dscr("sbv", [S, 256], BF)
    gpre = dscr("gpre", [1536, S])
    gzd = dscr("gzd", [S, 512])
    gabd = dscr("gabd", [S, 8])
    dqT = dscr("dqT", [256, S], BF)
    dkT = dscr("dkT", [256, S], BF)
    dav = dscr("dav", [S, 256], BF)
    mixT = dscr("mixT", [D, S], BF)

    with contextlib.ExitStack() as es:
        K = KB(nc, es)
        identF = K.sb(es, "identF", [128, 128])
        identB = K.sb(es, "identB", [128, 128], BF)
        PB = K.sb(es, "PB", [128, 6, D])
        modT = K.sb(es, "modT", [128, 4, 8])
        K.dma("sp", identF[:], I["k_ident"][:, :], w=[identF])
        K.op("dve", lambda e: e.tensor_copy(out=identB[:], in_=identF[:]), r=[identF], w=[identB])

        for l in range(depth):
            lam_init = 0.8 - 0.6 * math.exp(-0.3 * l)
            xin, xout = X[l], X[l + 1]
            if phases is None or "A" in phases:
                phase_adaln(K, nc, I, l, identF, PB, modT)
                K.barrier()
            if phases is None or "B" in phases:
                phase_inproj(K, nc, I, l, S, xin, identF, identB, modT,
                             sbqT, sbkT, sbv, gpre, gzd, gabd, dqT, dkT, dav)
                K.barrier()
            if phases is None or "C" in phases:
                phase_sb(K, nc, I, S, sbqT, sbkT, sbv, mixT)
                K.barrier()
            if phases is None or "D" in phases:
                phase_da(K, nc, I, l, S, lam_init, dqT, dkT, dav, mixT)
                K.barrier()
            if phases is None or "E" in phases:
                phase_gdn(K, nc, I, l, S, identF, gpre, gzd, gabd, mixT)
                K.barrier()
            if phases is None or "F" in phases:
                phase_outproj(K, nc, I, l, S, xin, x1, mixT, PB)
                K.barrier()
            if phases is None or "G" in phases:
                phase_moe(K, nc, I, l, S, x1, xout, identF, PB, modT)
                K.barrier()
        K.barrier()
    return nc


def phase_adaln(K, nc, I, l, identF, PB, modT):
    with contextlib.ExitStack() as es:
        c8 = K.sb(es, "a_c8", [8, 128])
        cT = K.sb(es, "a_cT", [128, 8])
        condB = K.sb(es, "a_condB", [128, 8, 128])
        onesF = K.sb(es, "a_ones", [128, 128])
        MODB = K.sb(es, "a_modb", [128, 6 * D])
        BAB = K.sb(es, "a_bab", [128, 6 * D])
        tmp = K.sb(es, "a_tmp", [128, 8, 128])
        wst = [K.sb(es, f"a_w{i}", [128, 3072]) for i in range(2)]
        pT = K.ps(es, "a_pT", [128, 8])
        acc = [K.ps(es, f"a_acc{i}", [128, 512]) for i in range(6)]

        K.dma("sp", c8[:], I["c"][:, :], w=[c8])
        K.dma("sp", BAB[:], _bcast_rows(I["b_ada"][l:l + 1, :]), w=[BAB])
        for j, n in enumerate(("ln1_g", "ln1_b", "ln2_g", "ln2_b")):
            K.dma("sp", PB[:, 2 + j, :], _bcast_rows(I[n][l:l + 1, :]), w=[PB])
        K.op("pool", lambda e: e.memset(onesF[:], 1.0), w=[onesF])
        K.op("pe", lambda e: e.transpose(out=pT[:], in_=c8[:], identity=identF[0:8, 0:8]), r=[c8, identF], w=[pT])
        K.op("act", lambda e: e.activation(out=cT[:], in_=pT[:], func=AF.Silu), r=[pT], w=[cT])
        for k in range(8):
            K.op("dve", lambda e, k=k: e.tensor_scalar(out=condB[:, k, :], in0=onesF[:], scalar1=cT[:, k:k + 1],
                                                        scalar2=None, op0=ALU.mult), r=[onesF, cT], w=[condB])
        for half in range(2):
            for k in range(8):
                wb = wst[k % 2]
                K.dma("sp", wb[:], I["w_ada"][l, k * 128:(k + 1) * 128, half * 3072:(half + 1) * 3072], w=[wb])
                for n in range(6):
                    K.op("pe", lambda e, n=n, k=k, wb=wb: e.matmul(acc[n][:], lhsT=condB[:, k, :],
                                                                     rhs=wb[:, n * 512:(n + 1) * 512],
                                                                     start=(k == 0), stop=(k == 7)),
                         r=[condB, wb], w=[acc[n]])
            for n in range(6):
                c0 = half * 3072 + n * 512
                K.op("dve", lambda e, n=n, c0=c0: e.tensor_tensor(out=MODB[:, c0:c0 + 512], in0=acc[n][:],
                                                                    in1=BAB[:, c0:c0 + 512], op=ALU.add),
                     r=[acc[n], BAB], w=[MODB])
        for j in (1, 2, 4, 5):
            K.op("dve", lambda e, j=j: e.tensor_scalar(out=MODB[:, j * D:(j + 1) * D], in0=MODB[:, j * D:(j + 1) * D],
                                                        scalar1=1.0, scalar2=None, op0=ALU.add), r=[MODB], w=[MODB])
        K.op("dve", lambda e: e.tensor_copy(out=PB[:, 0, :], in_=MODB[:, 2 * D:3 * D]), r=[MODB], w=[PB])
        K.op("dve", lambda e: e.tensor_copy(out=PB[:, 1, :], in_=MODB[:, 5 * D:6 * D]), r=[MODB], w=[PB])
        for i, j in enumerate((0, 1, 3, 4)):
            for k in range(8):
                K.op("dve", lambda e, j=j, k=k: e.tensor_tensor(out=tmp[:, k, :],
                                                                 in0=MODB[:, j * D + k * 128:j * D + (k + 1) * 128],
                                                                 in1=identF[:], op=ALU.mult),
                     r=[MODB, identF], w=[tmp])
            K.op("dve", lambda e, i=i: e.tensor_reduce(out=modT[:, i, :], in_=tmp[:], axis=AX.X, op=ALU.add),
                 r=[tmp], w=[modT])


def phase_inproj(K, nc, I, l, S, xin, identF, identB, modT, sbqT, sbkT, sbv, gpre, gzd, gabd, dqT, dkT, dav):
    import os
    CUT = int(os.environ.get("KCUT", "99"))
    NG = S // 512
    with contextlib.ExitStack() as es:
        W = K.sb(es, "b_W", [128, 8, IN_W], BF)
        wst = [K.sb(es, f"b_wst{i}", [128, IN_W]) for i in range(2)]
        xt = [K.sb(es, f"b_xt{i}", [128, D]) for i in range(2)]
        hT = [K.sb(es, f"b_hT{i}", [128, 8, 512], BF) for i in range(2)]
        ob = [K.sb(es, f"b_ob{i}", [128, 512], BF) for i in range(2)]
        of = [K.sb(es, f"b_of{i}", [128, 512]) for i in range(2)]
        tv = [K.sb(es, f"b_tv{i}", [128, 512], BF) for i in range(2)]
        tq = [K.sb(es, f"b_tq{i}", [128, 512], BF) for i in range(2)]
        xs = [K.sb(es, f"b_xs{i}", [128, 512]) for i in range(2)]
        xsw = K.sb(es, "b_xsw", [128, 512])
        xsw2 = K.sb(es, "b_xsw2", [128, 512])
        tz = [K.sb(es, f"b_tz{i}", [128, 512]) for i in range(2)]
        tab = [K.sb(es, f"b_tab{i}", [128, 8]) for i in range(2)]
        cs = [K.sb(es, f"b_cs{i}", [128, 2, 512]) for i in range(2)]
        dT = [K.sb(es, f"b_dT{i}", [128, 4, 512], BF) for i in range(2)]
        pX = [K.ps(es, f"b_pX{i}", [128, 512]) for i in range(2)]
        pF = [K.ps(es, f"b_pF{i}", [128, 512]) for i in range(2)]
        pA = K.ps(es, "b_pA", [128, 512])
        pB = K.ps(es, "b_pB", [128, 512])
        pC = K.ps(es, "b_pC", [128, 512])
        pD = K.ps(es, "b_pD", [128, 512], BF)
        pE = pX[0]

        K.op("pool", lambda e: e.memset(xsw[:], 0.0), w=[xsw])
        for k in range(8):
            st = wst[k % 2]
            K.dma("sp", st[:], I["w_in"][l, k * 128:(k + 1) * 128, :], w=[st])
            K.op("act" if k % 2 == 0 else "pool",
                 (lambda e, k=k, st=st: e.activation(out=W[:, k, :], in_=st[:], func=AF.Copy)) if k % 2 == 0 else
                 (lambda e, k=k, st=st: e.tensor_copy(out=W[:, k, :], in_=st[:])), r=[st], w=[W])

        if CUT < 1:
            return
        feat_chunks = ([(C_SBQ + j * 128, sbqT, j * 128, True) for j in range(2)] +
                       [(C_SBK + j * 128, sbkT, j * 128, True) for j in range(2)] +
                       [(C_GQ + j * 128, gpre, j * 128, False) for j in range(12)])
        ev = 0
        for g in range(NG):
            h = hT[g % 2]
            dTg = dT[g % 2]
            for tt in range(4):
                ti = g * 4 + tt
                x_ = xt[ti % 2]
                K.dma("sp", x_[:], xin[ti * 128:(ti + 1) * 128, :], w=[x_])
                for half in range(2):
                    p = pX[half]
                    for kk in range(4):
                        k = half * 4 + kk
                        K.op("pe", lambda e, p=p, kk=kk, k=k, x_=x_: e.transpose(
                            out=p[:, kk * 128:(kk + 1) * 128], in_=x_[:, k * 128:(k + 1) * 128], identity=identF[:]),
                            r=[x_, identF], w=[p])
                    for kk in range(4):
                        k = half * 4 + kk
                        K.op("act", lambda e, p=p, kk=kk, k=k, h=h, tt=tt: e.activation(
                            out=h[:, k, tt * 128:(tt + 1) * 128], in_=p[:, kk * 128:(kk + 1) * 128],
                            func=AF.Identity, bias=modT[:, 0, k:k + 1], scale=modT[:, 1, k:k + 1]),
                            r=[p, modT], w=[h])
            if CUT < 2:
                continue
            for ci, (c0, dst, r0, isbf) in enumerate(feat_chunks):
                p = pF[ci % 2]
                for k in range(8):
                    K.op("pe", lambda e, p=p, k=k, c0=c0, h=h: e.matmul(
                        p[:], lhsT=W[:, k, c0:c0 + 128], rhs=h[:, k, :], start=(k == 0), stop=(k == 7)),
                        r=[W, h], w=[p])
                o = (ob if isbf else of)[ev % 2]
                if ev % 2 == 0:
                    K.op("dve", lambda e, o=o, p=p: e.tensor_copy(out=o[:], in_=p[:]), r=[p], w=[o])
                else:
                    K.op("act", lambda e, o=o, p=p: e.activation(out=o[:], in_=p[:], func=AF.Copy), r=[p], w=[o])
                ev += 1
                K.dma("sp", dst[r0:r0 + 128, g * 512:(g + 1) * 512], o[:], r=[o], w=[])
            for tt in range(4):
                if CUT < 3:
                    continue
                ti = g * 4 + tt
                lt = lambda k: h[:, k, tt * 128:(tt + 1) * 128]
                tv_, tq_, tz_, tab_, cs_, xs_ = tv[ti % 2], tq[ti % 2], tz[ti % 2], tab[ti % 2], cs[ti % 2], xs[ti % 2]
                K.dma("sp", cs_[:, 0, :], I["k_cos"][ti * 128:(ti + 1) * 128, :], w=[cs_])
                K.dma("sp", cs_[:, 1, :], I["k_sin"][ti * 128:(ti + 1) * 128, :], w=[cs_])
                for (pp, o0, c0, n) in ((pA, 0, C_SBV, 256), (pA, 256, C_DV, 256), (pB, 0, C_DQ, 512),
                                        (pC, 0, C_GZ, 512), (pE, 0, C_GA, 8)):
                    for k in range(8):
                        K.op("pe", lambda e, pp=pp, o0=o0, c0=c0, n=n, k=k: e.matmul(
                            pp[:, o0:o0 + n], lhsT=lt(k), rhs=W[:, k, c0:c0 + n], start=(k == 0), stop=(k == 7)),
                            r=[W, h], w=[pp])
                K.op("act", lambda e: e.activation(out=tv_[:], in_=pA[:], func=AF.Copy), r=[pA], w=[tv_])
                K.dma("sp", sbv[ti * 128:(ti + 1) * 128, :], tv_[:, 0:256], r=[tv_])
                K.dma("sp", dav[ti * 128:(ti + 1) * 128, :], tv_[:, 256:512], r=[tv_])
                K.op("act", lambda e: e.activation(out=tz_[:], in_=pC[:], func=AF.Copy), r=[pC], w=[tz_])
                K.dma("sp", gzd[ti * 128:(ti + 1) * 128, :], tz_[:], r=[tz_])
                K.op("dve", lambda e: e.tensor_copy(out=tab_[:], in_=pE[:, 0:8]), r=[pE], w=[tab_])
                K.dma("sp", gabd[ti * 128:(ti + 1) * 128, :], tab_[:], r=[tab_])
                if CUT < 4:
                    continue
                K.op("act", lambda e: e.activation(out=xs_[:], in_=pB[:], func=AF.Copy), r=[pB], w=[xs_])
                xv = xs_[:].rearrange("p (g d) -> p g d", d=32)
                wv = xsw[:].rearrange("p (g d) -> p g d", d=32)
                K.op("pool", lambda e: e.tensor_copy(out=wv[:, :, 0:4], in_=xv[:, :, 4:8]), r=[xs_], w=[xsw])
                K.op("pool", lambda e: e.tensor_copy(out=wv[:, :, 4:8], in_=xv[:, :, 0:4]), r=[xs_], w=[xsw])
                K.op("dve", lambda e: e.tensor_tensor(out=xs_[:], in0=xs_[:], in1=cs_[:, 0, :], op=ALU.mult),
                     r=[xs_, cs_], w=[xs_])
                K.op("pool", lambda e: e.tensor_tensor(out=xsw2[:], in0=xsw[:], in1=cs_[:, 1, :], op=ALU.mult),
                     r=[xsw, cs_], w=[xsw2])
                K.op("dve", lambda e: e.tensor_tensor(out=tq_[:], in0=xs_[:], in1=xsw2[:], op=ALU.add),
                     r=[xs_, xsw2], w=[tq_])
                if CUT < 5:
                    continue
                tqf = tq_[:]
                for j in range(4):
                    K.op("pe", lambda e, j=j: e.transpose(out=pD[:, j * 128:(j + 1) * 128],
                                                          in_=tqf[:, j * 128:(j + 1) * 128], identity=identB[:]),
                         r=[tq_, identB], w=[pD])
                K.op("dve", lambda e: e.tensor_copy(
                    out=dTg[:, :, tt * 128:(tt + 1) * 128], in_=pD[:].rearrange("p (j t) -> p j t", t=128)),
                    r=[pD], w=[dTg])
            for j in range(4):
                dst = dqT if j < 2 else dkT
                K.dma("sp", dst[(j % 2) * 128:(j % 2 + 1) * 128, g * 512:(g + 1) * 512], dTg[:, j, :], r=[dTg])


def phase_sb(K, nc, I, S, sbqT, sbkT, sbv, mixT):
    NCH = S // 512
    NB = S // 128
    scale = 64 ** -0.5
    with contextlib.ExitStack() as es:
        msk = K.sb(es, "c_msk", [128, 4, 512])
        mskB = K.sb(es, "c_mskB", [128, 4, 512], BF)
        tri = K.sb(es, "c_tri", [128, 128])
        UsB = K.sb(es, "c_UsB", [128, 128], BF)
        onesB = K.sb(es, "c_onesB", [128, 128], BF)
        V = K.sb(es, "c_V", [128, NB, 256], BF)
        qT = [K.sb(es, f"c_qT{i}", [64, S], BF) for i in range(2)]
        kT = [K.sb(es, f"c_kT{i}", [64, S], BF) for i in range(2)]
        CS = K.sb(es, "c_CS", [128, 512])
        E1 = [K.sb(es, f"c_E1{i}", [128, 512]) for i in range(2)]
        SP = [K.sb(es, f"c_SP{i}", [128, 512]) for i in range(2)]
        SPb = [K.sb(es, f"c_SPb{i}", [128, 512], BF) for i in range(2)]
        t1 = [K.sb(es, f"c_t1{i}", [128, 512]) for i in range(2)]
        t2 = [K.sb(es, f"c_t2{i}", [128, 512]) for i in range(2)]
        Wt = [K.sb(es, f"c_W{i}", [128, 512], BF) for i in range(2)]
        osb = [K.sb(es, f"c_o{i}", [64, 512], BF) for i in range(2)]
        pZ = [K.ps(es, f"c_pZ{i}", [128, 512]) for i in range(2)]
        pBt = [K.ps(es, f"c_pB{i}", [128, 512]) for i in range(2)]
        pCt = [K.ps(es, f"c_pC{i}", [128, 512]) for i in range(2)]
        pO = K.ps(es, "c_pO", [64, 512])

        K.dma("sp", msk[:].rearrange("p r t -> p (r t)"), I["k_msb"][:, :], w=[msk])
        K.dma("sp", tri[:], I["k_tri"][:, 256:384], w=[tri])
        K.dma("sp", V[:], sbv.rearrange("(n p) d -> p n d", p=128), w=[V])
        K.op("dve", lambda e: e.tensor_copy(out=mskB[:], in_=msk[:]), r=[msk], w=[mskB])
        K.op("dve", lambda e: e.tensor_copy(out=UsB[:], in_=tri[:]), r=[tri], w=[UsB])
        K.op("pool", lambda e: e.memset(onesB[:], 1.0), w=[onesB])
        it = 0
        for h in range(4):
            q_, k_ = qT[h % 2], kT[h % 2]
            K.dma("sp", q_[:], sbqT[h * 64:(h + 1) * 64, :], w=[q_])
            K.dma("sp", k_[:], sbkT[h * 64:(h + 1) * 64, :], w=[k_])
            for c in range(NCH):
                K.op("pool", lambda e: e.memset(CS[:], 0.0), w=[CS])
                nblk = 4 * c + 4
                for bi, sbk in enumerate(range(nblk - 1, -1, -1)):
                    r_ = sbk - 4 * c
                    i2 = it % 2
                    it += 1
                    z, pb, pc = pZ[i2], pBt[i2], pCt[i2]
                    e1, sp, spb, a1, a2, w_ = E1[i2], SP[i2], SPb[i2], t1[i2], t2[i2], Wt[i2]
                    K.op("pe", lambda e: e.matmul(z[:], lhsT=k_[:, sbk * 128:(sbk + 1) * 128],
                                                  rhs=q_[:, c * 512:(c + 1) * 512], start=True, stop=True),
                         r=[k_, q_], w=[z])
                    K.op("act", lambda e: e.activation(out=e1[:], in_=z[:], func=AF.Exp, scale=scale), r=[z], w=[e1])
                    K.op("act", lambda e: e.activation(out=sp[:], in_=e1[:], func=AF.Ln, bias=1.0), r=[e1], w=[sp])
                    if r_ >= 0:
                        K.op("dve", lambda e: e.tensor_tensor(out=sp[:], in0=sp[:], in1=msk[:, r_, :], op=ALU.mult),
                             r=[sp, msk], w=[sp])
                    K.op("act", lambda e: e.activation(out=spb[:], in_=sp[:], func=AF.Copy), r=[sp], w=[spb])
                    K.op("pe", lambda e: e.matmul(pb[:], lhsT=UsB[:], rhs=spb[:], start=True, stop=True),
                         r=[UsB, spb], w=[pb])
                    K.op("pe", lambda e: e.matmul(pc[:], lhsT=onesB[:], rhs=spb[:], start=True, stop=True),
                         r=[onesB, spb], w=[pc])
                    K.op("dve", lambda e: e.scalar_tensor_tensor(out=a1[:], in0=z[:], scalar=scale, in1=sp[:],
                                                                 op0=ALU.mult, op1=ALU.subtract), r=[z, sp], w=[a1])
                    K.op("dve", lambda e: e.tensor_tensor(out=a2[:], in0=a1[:], in1=pb[:], op=ALU.subtract),
                         r=[a1, pb], w=[a2])
                    K.op("dve", lambda e: e.tensor_tensor(out=a2[:], in0=a2[:], in1=CS[:], op=ALU.subtract),
                         r=[a2, CS], w=[a2])
                    K.op("act", lambda e: e.activation(out=w_[:], in_=a2[:], func=AF.Exp), r=[a2], w=[w_])
                    if r_ >= 0:
                        K.op("dve", lambda e: e.tensor_tensor(out=w_[:], in0=w_[:], in1=mskB[:, r_, :], op=ALU.mult),
                             r=[w_, mskB], w=[w_])
                    K.op("pe", lambda e: e.matmul(pO[:], lhsT=V[:, sbk, h * 64:(h + 1) * 64], rhs=w_[:],
                                                  start=(bi == 0), stop=(bi == nblk - 1)), r=[V, w_], w=[pO])
                    if bi < nblk - 1:
                        K.op("dve", lambda e: e.tensor_tensor(out=CS[:], in0=CS[:], in1=pc[:], op=ALU.add),
                             r=[CS, pc], w=[CS])
                o_ = osb[(h * NCH + c) % 2]
                K.op("act", lambda e: e.activation(out=o_[:], in_=pO[:], func=AF.Copy), r=[pO], w=[o_])
                K.dma("sp", mixT[h * 64:(h + 1) * 64, c * 512:(c + 1) * 512], o_[:], r=[o_])


def phase_da(K, nc, I, l, S, lam_init, dqT, dkT, dav, mixT):
    NCH = S // 512
    NB = S // 128
    scale = 32 ** -0.5
    with contextlib.ExitStack() as es:
        mskB = K.sb(es, "d_mskB", [128, 4, 512], BF)
        msk = K.sb(es, "d_msk", [128, 4, 512])
        onesB = K.sb(es, "d_onesB", [128, 64], BF)
        onesF = K.sb(es, "d_onesF", [64, 64])
        V = K.sb(es, "d_V", [128, NB, 256], BF)
        qT = [K.sb(es, f"d_qT{i}", [64, S], BF) for i in range(2)]
        kT = [K.sb(es, f"d_kT{i}", [64, S], BF) for i in range(2)]
        Et = [K.sb(es, f"d_E{i}", [128, 512], BF) for i in range(4)]
        lp = K.sb(es, "d_lp", [128, 128])
        lpp = K.sb(es, "d_lpp", [128, 2, 32])
        ls = K.sb(es, "d_ls", [128, 2])
        le = K.sb(es, "d_le", [128, 2])
        nlam = K.sb(es, "d_nlam", [128, 1])
        wcol = K.sb(es, "d_wcol", [64, 1])
        rr = [K.sb(es, f"d_rr{i}", [64, 512]) for i in range(2)]
        oo = [K.sb(es, f"d_oo{i}", [64, 512]) for i in range(2)]
        od = K.sb(es, "d_od", [64, 512])
        sq = K.sb(es, "d_sq", [64, 512])
        rs = K.sb(es, "d_rs", [64, 512])
        ob = [K.sb(es, f"d_ob{i}", [64, 512], BF) for i in range(2)]
        pS = [K.ps(es, f"d_pS{i}", [128, 512]) for i in range(2)]
        pN = [K.ps(es, f"d_pN{i}", [64, 512]) for i in range(2)]
        pDn = [K.ps(es, f"d_pD{i}", [64, 512]) for i in range(2)]
        pQ = K.ps(es, "d_pQ", [64, 512])

        K.dma("sp", msk[:].rearrange("p r t -> p (r t)"), I["k_mda"][:, :], w=[msk])
        K.dma("sp", V[:], dav.rearrange("(n p) d -> p n d", p=128), w=[V])
        K.dma("sp", lp[:], _bcast_rows(I["da_lambda"][l:l + 1, :]), w=[lp])
        K.dma("sp", wcol[:], I["da_subln_w"][l:l + 1, :].rearrange("o d -> d o"), w=[wcol],
              allow_slow_non_contiguous=True)
        K.op("dve", lambda e: e.tensor_copy(out=mskB[:], in_=msk[:]), r=[msk], w=[mskB])
        K.op("pool", lambda e: e.memset(onesB[:], 1.0), w=[onesB])
        K.op("pool", lambda e: e.memset(onesF[:], 1.0), w=[onesF])
        K.op("dve", lambda e: e.tensor_tensor(out=lpp[:, 0, :], in0=lp[:, 0:32], in1=lp[:, 32:64], op=ALU.mult),
             r=[lp], w=[lpp])
        K.op("dve", lambda e: e.tensor_tensor(out=lpp[:, 1, :], in0=lp[:, 64:96], in1=lp[:, 96:128], op=ALU.mult),
             r=[lp], w=[lpp])
        K.op("dve", lambda e: e.tensor_reduce(out=ls[:], in_=lpp[:], axis=AX.X, op=ALU.add), r=[lpp], w=[ls])
        K.op("act", lambda e: e.activation(out=le[:], in_=ls[:], func=AF.Exp), r=[ls], w=[le])
        K.op("dve", lambda e: e.tensor_tensor(out=nlam[:], in0=le[:, 1:2], in1=le[:, 0:1], op=ALU.subtract),
             r=[le], w=[nlam])
        K.op("dve", lambda e: e.tensor_scalar(out=nlam[:], in0=nlam[:], scalar1=-lam_init, scalar2=None, op0=ALU.add),
             r=[nlam], w=[nlam])
        K.op("dve", lambda e: e.tensor_scalar(out=wcol[:], in0=wcol[:], scalar1=(1.0 - lam_init), scalar2=None,
                                              op0=ALU.mult), r=[wcol], w=[wcol])
        it = 0
        for h in range(4):
            q_, k_ = qT[h % 2], kT[h % 2]
            K.dma("sp", q_[:], dqT[h * 64:(h + 1) * 64, :], w=[q_])
            K.dma("sp", k_[:], dkT[h * 64:(h + 1) * 64, :], w=[k_])
            for c in range(NCH):
                nblk = 4 * c + 4
                for bi, sbk in enumerate(range(nblk - 1, -1, -1)):
                    r_ = sbk - 4 * c
                    for m in range(2):
                        ps_ = pS[it % 2]
                        e_ = Et[it % 4]
                        it += 1
                        K.op("pe", lambda e: e.matmul(ps_[:], lhsT=k_[m * 32:(m + 1) * 32, sbk * 128:(sbk + 1) * 128],
                                                      rhs=q_[m * 32:(m + 1) * 32, c * 512:(c + 1) * 512],
                                                      start=True, stop=True), r=[k_, q_], w=[ps_])
                        K.op("act", lambda e: e.activation(out=e_[:], in_=ps_[:], func=AF.Exp, scale=scale),
                             r=[ps_], w=[e_])
                        if r_ >= 0:
                            K.op("pool" if m else "dve",
                                 lambda e: e.tensor_tensor(out=e_[:], in0=e_[:], in1=mskB[:, r_, :], op=ALU.mult),
                                 r=[e_, mskB], w=[e_])
                        K.op("pe", lambda e: e.matmul(pN[m][:], lhsT=V[:, sbk, h * 64:(h + 1) * 64], rhs=e_[:],
                                                      start=(bi == 0), stop=(bi == nblk - 1)), r=[V, e_], w=[pN[m]])
                        K.op("pe", lambda e: e.matmul(pDn[m][:], lhsT=onesB[:], rhs=e_[:],
                                                      start=(bi == 0), stop=(bi == nblk - 1)), r=[onesB, e_], w=[pDn[m]])
                for m in range(2):
                    K.op("act", lambda e: e.activation(out=rr[m][:], in_=pDn[m][:], func=AF.Ln), r=[pDn[m]], w=[rr[m]])
                    K.op("act", lambda e: e.activation(out=rr[m][:], in_=rr[m][:], func=AF.Exp, scale=-1.0),
                         r=[rr[m]], w=[rr[m]])
                    K.op("dve", lambda e: e.tensor_tensor(out=oo[m][:], in0=pN[m][:], in1=rr[m][:], op=ALU.mult),
                         r=[pN[m], rr[m]], w=[oo[m]])
                K.op("dve", lambda e: e.scalar_tensor_tensor(out=od[:], in0=oo[1][:], scalar=nlam[0:64, :], in1=oo[0][:],
                                                             op0=ALU.mult, op1=ALU.add), r=[oo[0], oo[1], nlam], w=[od])
                K.op("pool", lambda e: e.tensor_tensor(out=sq[:], in0=od[:], in1=od[:], op=ALU.mult), r=[od], w=[sq])
                K.op("pe", lambda e: e.matmul(pQ[:], lhsT=onesF[:], rhs=sq[:], start=True, stop=True),
                     r=[onesF, sq], w=[pQ])
                K.op("act", lambda e: e.activation(out=rs[:], in_=pQ[:], func=AF.Ln, scale=1.0 / 64, bias=1e-6),
                     r=[pQ], w=[rs])
                K.op("act", lambda e: e.activation(out=rs[:], in_=rs[:], func=AF.Exp, scale=-0.5), r=[rs], w=[rs])
                o_ = ob[(h * NCH + c) % 2]
                K.op("dve", lambda e: e.scalar_tensor_tensor(out=o_[:], in0=od[:], scalar=wcol[:], in1=rs[:],
                                                             op0=ALU.mult, op1=ALU.mult), r=[od, wcol, rs], w=[o_])
                K.dma("sp", mixT[768 + h * 64:768 + (h + 1) * 64, c * 512:(c + 1) * 512], o_[:], r=[o_])


def phase_gdn(K, nc, I, l, S, identF, gpre, gzd, gabd, mixT):
    NU = S // 128
    with contextlib.ExitStack() as es:
        sbt = lambda n, shp, dt=F32: K.sb(es, "e_" + n, shp, dt)
        tri = sbt("tri", [128, 384])
        onesF = sbt("ones", [128, 128])
        c48 = sbt("c48", [48, 128])
        cw = sbt("cw", [128, 48])
        alog = sbt("alog", [128, 4])
        dtb = sbt("dtb", [128, 4])
        nea = sbt("nea", [128, 4])
        gnw = sbt("gnw", [128, 128])
        St = [sbt(f"St{h}", [128, 128]) for h in range(4)]
        win = [sbt(f"win{i}", [128, 12, 131]) for i in range(2)]
        gab = [sbt(f"gab{i}", [128, 8]) for i in range(2)]
        gz = [sbt(f"gz{i}", [128, 512]) for i in range(2)]
        sm = {n: [sbt(f"{n}{i}", [128, 4]) for i in range(2)] for n in
              ("xg", "ex", "sp", "a", "e2", "beta", "nbeta", "gcol", "eg", "egl", "ekd", "tmp4")}
        names = ("cq", "ck", "cv", "qn", "kn", "vt", "qnT", "knT", "abc", "dec", "decl", "decs", "X0", "X1",
                 "XT0", "XT1", "A", "AT", "wT", "kdec", "vnew", "o", "zs", "on", "junk")
        bt = {n: [sbt(f"{n}{i}", [128, 128]) for i in range(2)] for n in names}
        Yt = [sbt(f"Y{i}", [128, 256]) for i in range(2)]
        obf = [sbt(f"obf{i}", [128, 128], BF) for i in range(2)]
        ssq = [sbt(f"ssq{i}", [128, 4]) for i in range(2)]
        rn = [sbt(f"rn{i}", [128, 4]) for i in range(2)]
        banks = [K.ps(es, f"e_bank{i}", [128, 512]) for i in range(8)]
        playout = (("pq", "pk", "pv", "pc48"), ("pqT", "pkT", "pwT", "pg"), ("KK", "QK", "Grow", "pOT"),
                   ("pAT", "pXT", "p1", "p4"), ("pX2", "pXT2", "p2", "p3"))
        pt = {}
        for bi_, grp in enumerate(playout):
            for si_, n in enumerate(grp):
                pt[n] = TV(banks[bi_], banks[bi_][:, si_ * 128:(si_ + 1) * 128])
        pY = [TV(banks[5], banks[5][:, 0:256]), TV(banks[6], banks[6][:, 0:256])]
        Lf, mlow, mstr = tri[:, 0:128], tri[:, 128:256], tri[:, 256:384]

        K.dma("sp", tri[:], I["k_tri"][:, :], w=[tri])
        K.dma("sp", c48[:], I["conv_w"][l].rearrange("k (j p) -> (k j) p", p=128), w=[c48])
        K.dma("sp", alog[:], _bcast_rows(I["gdn_a_log"][l:l + 1, :]), w=[alog])
        K.dma("sp", dtb[:], _bcast_rows(I["gdn_dt_bias"][l:l + 1, :]), w=[dtb])
        K.dma("sp", gnw[:], _bcast_rows(I["gdn_norm_w"][l:l + 1, :]), w=[gnw])
        K.op("pool", lambda e: e.memset(onesF[:], 1.0), w=[onesF])
        for h in range(4):
            K.op("pool", lambda e, h=h: e.memset(St[h][:], 0.0), w=[St[h]])
        K.op("pe", lambda e: e.transpose(out=pt["pc48"][:, 0:48], in_=c48[:], identity=identF[0:48, 0:48]),
             r=[c48, identF], w=[pt["pc48"]])
        K.op("dve", lambda e: e.tensor_copy(out=cw[:], in_=pt["pc48"][:, 0:48]), r=[pt["pc48"]], w=[cw])
        K.op("act", lambda e: e.activation(out=nea[:], in_=alog[:], func=AF.Exp), r=[alog], w=[nea])
        K.op("dve", lambda e: e.tensor_scalar(out=nea[:], in0=nea[:], scalar1=-1.0, scalar2=None, op0=ALU.mult),
             r=[nea], w=[nea])

        def tr(dst_ps, src, srcT):
            K.op("pe", lambda e: e.transpose(out=dst_ps[:], in_=src, identity=identF[:]), r=[srcT, identF], w=[dst_ps])

        def mm(dst_ps, lhsT, rhs, deps):
            K.op("pe", lambda e: e.matmul(dst_ps[:], lhsT=lhsT, rhs=rhs, start=True, stop=True), r=deps, w=[dst_ps])

        cpy_i = [0]

        def cpy(dst, dstT, src_ps):
            cpy_i[0] += 1
            cm = os.environ.get("KCPY", "dve")
            if (cpy_i[0] % 2 and cm == "alt") or cm == "dve":
                K.op("dve", lambda e: e.tensor_copy(out=dst, in_=src_ps[:]), r=[src_ps], w=[dstT])
            else:
                K.op("act", lambda e: e.activation(out=dst, in_=src_ps[:], func=AF.Copy), r=[src_ps], w=[dstT])

        import os
        CUT3 = int(os.environ.get("KCUT3", "99"))
        for u in range(NU):
            if CUT3 < 1:
                break
            i2 = u % 2
            w_ = win[i2]
            g_ = gab[i2]
            z_ = gz[i2]
            if u == 0:
                K.op("pool", lambda e: e.memset(w_[:, :, 0:3], 0.0), w=[w_])
                K.dma("sp", w_[:, :, 3:131], gpre.rearrange("(j p) s -> p j s", p=128)[:, :, 0:128], w=[w_])
            else:
                K.dma("sp", w_[:], gpre.rearrange("(j p) s -> p j s", p=128)[:, :, u * 128 - 3:u * 128 + 128], w=[w_])
            K.dma("sp", g_[:], gabd[u * 128:(u + 1) * 128, :], w=[g_])
            K.dma("sp", z_[:], gzd[u * 128:(u + 1) * 128, :], w=[z_])
            s = {n: v[i2] for n, v in sm.items()}
            K.op("dve", lambda e: e.tensor_tensor(out=s["xg"][:], in0=g_[:, 0:4], in1=dtb[:], op=ALU.add),
                 r=[g_, dtb], w=[s["xg"]])
            K.op("act", lambda e: e.activation(out=s["ex"][:], in_=s["xg"][:], func=AF.Exp), r=[s["xg"]], w=[s["ex"]])
            K.op("act", lambda e: e.activation(out=s["sp"][:], in_=s["ex"][:], func=AF.Ln, bias=1.0),
                 r=[s["ex"]], w=[s["sp"]])
            K.op("dve", lambda e: e.tensor_tensor(out=s["a"][:], in0=s["sp"][:], in1=nea[:], op=ALU.mult),
                 r=[s["sp"], nea], w=[s["a"]])
            K.op("act", lambda e: e.activation(out=s["e2"][:], in_=g_[:, 4:8], func=AF.Exp, scale=-1.0),
                 r=[g_], w=[s["e2"]])
            K.op("dve", lambda e: e.tensor_scalar(out=s["e2"][:], in0=s["e2"][:], scalar1=1.0, scalar2=None,
                                                  op0=ALU.add), r=[s["e2"]], w=[s["e2"]])
            K.op("dve", lambda e: e.reciprocal(out=s["beta"][:], in_=s["e2"][:]), r=[s["e2"]], w=[s["beta"]])
            K.op("dve", lambda e: e.tensor_scalar(out=s["nbeta"][:], in0=s["beta"][:], scalar1=-1.0, scalar2=None,
                                                  op0=ALU.mult), r=[s["beta"]], w=[s["nbeta"]])
            pg = pt["pg"]
            K.op("pe", lambda e: e.matmul(pg[:, 0:4], lhsT=Lf, rhs=s["a"][:], start=True, stop=True),
                 r=[tri, s["a"]], w=[pg])
            K.op("pe", lambda e: e.matmul(pg[:, 4:8], lhsT=onesF[:], rhs=s["a"][:], start=True, stop=True),
                 r=[onesF, s["a"]], w=[pg])
            K.op("dve", lambda e: e.tensor_copy(out=s["gcol"][:], in_=pg[:, 0:4]), r=[pg], w=[s["gcol"]])
            K.op("act", lambda e: e.activation(out=s["eg"][:], in_=pg[:, 0:4], func=AF.Exp), r=[pg], w=[s["eg"]])
            K.op("act", lambda e: e.activation(out=s["egl"][:], in_=pg[:, 4:8], func=AF.Exp), r=[pg], w=[s["egl"]])
            K.op("dve", lambda e: e.tensor_tensor(out=s["tmp4"][:], in0=pg[:, 4:8], in1=s["gcol"][:], op=ALU.subtract),
                 r=[pg, s["gcol"]], w=[s["tmp4"]])
            K.op("act", lambda e: e.activation(out=s["ekd"][:], in_=s["tmp4"][:], func=AF.Exp),
                 r=[s["tmp4"]], w=[s["ekd"]])
            for h in range(4):
                if CUT3 < 2:
                    break
                b = {n: v[(u * 4 + h) % 2] for n, v in bt.items()}
                Y = Yt[(u * 4 + h) % 2]
                sq_, rn_ = ssq[(u * 4 + h) % 2], rn[(u * 4 + h) % 2]
                hc = slice(h, h + 1)
                for nm, j in (("cq", h), ("ck", 4 + h), ("cv", 8 + h)):
                    t_ = b[nm]
                    K.op("dve", lambda e: e.tensor_scalar(out=t_[:], in0=w_[:, j, 0:128], scalar1=cw[:, j:j + 1],
                                                          scalar2=None, op0=ALU.mult), r=[w_, cw], w=[t_])
                    for k in range(1, 4):
                        if int(os.environ.get("KCUT4", "99")) < 1:
                            continue
                        K.op("dve", lambda e, k=k: e.scalar_tensor_tensor(
                            out=t_[:], in0=w_[:, j, k:k + 128], scalar=cw[:, k * 12 + j:k * 12 + j + 1], in1=t_[:],
                            op0=ALU.mult, op1=ALU.add), r=[w_, cw, t_], w=[t_])
                    if int(os.environ.get("KCUT4", "99")) < 2:
                        continue
                    K.op("act", lambda e: e.activation(out=t_[:], in_=t_[:], func=AF.Silu), r=[t_], w=[t_])
                if CUT3 < 3:
                    continue
                C5 = int(os.environ.get("KCUT5", "99"))
                tr(pt["pq"], b["cq"][:], b["cq"])
                tr(pt["pk"], b["ck"][:], b["ck"])
                tr(pt["pv"], b["cv"][:], b["cv"])
                if C5 < 1:
                    continue
                K.op("act", lambda e: e.activation(out=b["junk"][:], in_=pt["pq"][:], func=AF.Square,
                                                   accum_out=sq_[:, 0:1]), r=[pt["pq"]], w=[b["junk"], sq_])
                K.op("act", lambda e: e.activation(out=b["junk"][:], in_=pt["pk"][:], func=AF.Square,
                                                   accum_out=sq_[:, 1:2]), r=[pt["pk"]], w=[b["junk"], sq_])
                if C5 < 2:
                    continue
                K.op("act", lambda e: e.activation(out=rn_[:, 0:2], in_=sq_[:, 0:2], func=AF.Ln, bias=1e-6),
                     r=[sq_], w=[rn_])
                K.op("act", lambda e: e.activation(out=rn_[:, 0:2], in_=rn_[:, 0:2], func=AF.Exp, scale=-0.5),
                     r=[rn_], w=[rn_])
                if C5 < 3:
                    continue
                K.op("dve", lambda e: e.tensor_scalar(out=b["qn"][:], in0=pt["pq"][:], scalar1=rn_[:, 0:1],
                                                      scalar2=128 ** -0.5, op0=ALU.mult, op1=ALU.mult),
                     r=[pt["pq"], rn_], w=[b["qn"]])
                if int(os.environ.get("KCUT6", "99")) < 1:
                    continue
                K.op("dve", lambda e: e.tensor_scalar(out=b["kn"][:], in0=pt["pk"][:], scalar1=rn_[:, 1:2],
                                                      scalar2=None, op0=ALU.mult), r=[pt["pk"], rn_], w=[b["kn"]])
                if int(os.environ.get("KCUT6", "99")) < 2:
                    continue
                cpy(b["vt"][:], b["vt"], pt["pv"])
                if C5 < 4:
                    continue
                tr(pt["pqT"], b["qn"][:], b["qn"])
                tr(pt["pkT"], b["kn"][:], b["kn"])
                cpy(b["qnT"][:], b["qnT"], pt["pqT"])
                cpy(b["knT"][:], b["knT"], pt["pkT"])
                if C5 < 5:
                    continue
                mm(pt["KK"], b["knT"][:], b["knT"][:], [b["knT"]])
                mm(pt["QK"], b["qnT"][:], b["knT"][:], [b["qnT"], b["knT"]])
                if CUT3 < 4:
                    continue
                K.op("pool", lambda e: e.tensor_scalar(out=b["abc"][:], in0=onesF[:], scalar1=s["a"][:, hc],
                                                       scalar2=None, op0=ALU.mult), r=[onesF, s["a"]], w=[b["abc"]])
                mm(pt["Grow"], b["abc"][:], Lf, [b["abc"], tri])
                K.op("dve", lambda e: e.tensor_scalar(out=b["dec"][:], in0=pt["Grow"][:], scalar1=s["gcol"][:, hc],
                                                      scalar2=0.0, op0=ALU.subtract, op1=ALU.max),
                     r=[pt["Grow"], s["gcol"]], w=[b["dec"]])
                K.op("act", lambda e: e.activation(out=b["dec"][:], in_=b["dec"][:], func=AF.Exp, scale=-1.0),
                     r=[b["dec"]], w=[b["dec"]])
                K.op("pool", lambda e: e.tensor_tensor(out=b["decl"][:], in0=b["dec"][:], in1=mlow, op=ALU.mult),
                     r=[b["dec"], tri], w=[b["decl"]])
                K.op("pool", lambda e: e.tensor_tensor(out=b["decs"][:], in0=b["dec"][:], in1=mstr, op=ALU.mult),
                     r=[b["dec"], tri], w=[b["decs"]])
                K.op("dve", lambda e: e.scalar_tensor_tensor(out=b["X0"][:], in0=pt["KK"][:], scalar=s["nbeta"][:, hc],
                                                             in1=b["decs"][:], op0=ALU.mult, op1=ALU.mult),
                     r=[pt["KK"], s["nbeta"], b["decs"]], w=[b["X0"]])
                K.op("dve", lambda e: e.tensor_tensor(out=b["A"][:], in0=pt["QK"][:], in1=b["decl"][:], op=ALU.mult),
                     r=[pt["QK"], b["decl"]], w=[b["A"]])
                tr(pt["pAT"], b["A"][:], b["A"])
                cpy(b["AT"][:], b["AT"], pt["pAT"])
                tr(pt["pXT"], b["X0"][:], b["X0"])
                cpy(b["XT0"][:], b["XT0"], pt["pXT"])
                K.op("pool", lambda e: e.tensor_scalar(out=Y[:, 0:128], in0=b["vt"][:], scalar1=s["beta"][:, hc],
                                                       scalar2=None, op0=ALU.mult), r=[b["vt"], s["beta"]], w=[Y])
                K.op("dve", lambda e: e.tensor_scalar(out=Y[:, 128:256], in0=b["kn"][:], scalar1=s["beta"][:, hc],
                                                      scalar2=s["eg"][:, hc], op0=ALU.mult, op1=ALU.mult),
                     r=[b["kn"], s["beta"], s["eg"]], w=[Y])
                if CUT3 < 5:
                    continue
                for k in range(7):
                    Xk, XTk = b[f"X{k % 2}"], b[f"XT{k % 2}"]
                    Xn, XTn = b[f"X{(k + 1) % 2}"], b[f"XT{(k + 1) % 2}"]
                    py = pY[k % 2]
                    K.op("pe", lambda e: e.matmul(py[:], lhsT=XTk[:], rhs=Y[:], start=True, stop=True),
                         r=[XTk, Y], w=[py])
                    if k < 6:
                        mm(pt["pX2"], XTk[:], Xk[:], [XTk, Xk])
                        mm(pt["pXT2"], Xk[:], XTk[:], [XTk, Xk])
                    K.op("dve", lambda e: e.tensor_tensor(out=Y[:], in0=Y[:], in1=py[:], op=ALU.add), r=[Y, py], w=[Y])
                    if k < 6:
                        K.op("act", lambda e: e.activation(out=Xn[:], in_=pt["pX2"][:], func=AF.Copy),
                             r=[pt["pX2"]], w=[Xn])
                        K.op("pool" if False else "act", lambda e: e.activation(out=XTn[:], in_=pt["pXT2"][:],
                                                                                 func=AF.Copy),
                             r=[pt["pXT2"]], w=[XTn])
                tr(pt["pwT"], Y[:, 128:256], Y)
                cpy(b["wT"][:], b["wT"], pt["pwT"])
                K.op("pool", lambda e: e.tensor_scalar(out=b["kdec"][:], in0=b["kn"][:], scalar1=s["ekd"][:, hc],
                                                       scalar2=None, op0=ALU.mult), r=[b["kn"], s["ekd"]], w=[b["kdec"]])
                if CUT3 < 6:
                    continue
                mm(pt["p1"], b["wT"][:], St[h][:], [b["wT"], St[h]])
                K.op("dve", lambda e: e.tensor_tensor(out=b["vnew"][:], in0=Y[:, 0:128], in1=pt["p1"][:],
                                                      op=ALU.subtract), r=[Y, pt["p1"]], w=[b["vnew"]])
                mm(pt["p2"], b["qnT"][:], St[h][:], [b["qnT"], St[h]])
                mm(pt["p3"], b["AT"][:], b["vnew"][:], [b["AT"], b["vnew"]])
                mm(pt["p4"], b["kdec"][:], b["vnew"][:], [b["kdec"], b["vnew"]])
                K.op("dve", lambda e: e.scalar_tensor_tensor(out=St[h][:], in0=St[h][:], scalar=s["egl"][:, hc],
                                                             in1=pt["p4"][:], op0=ALU.mult, op1=ALU.add),
                     r=[St[h], s["egl"], pt["p4"]], w=[St[h]])
                K.op("dve", lambda e: e.tensor_scalar(out=b["o"][:], in0=pt["p2"][:], scalar1=s["eg"][:, hc],
                                                      scalar2=None, op0=ALU.mult), r=[pt["p2"], s["eg"]], w=[b["o"]])
                K.op("dve", lambda e: e.tensor_tensor(out=b["o"][:], in0=b["o"][:], in1=pt["p3"][:], op=ALU.add),
                     r=[b["o"], pt["p3"]], w=[b["o"]])
                K.op("act", lambda e: e.activation(out=b["junk"][:], in_=b["o"][:], func=AF.Square,
                                                   accum_out=sq_[:, 2:3]), r=[b["o"]], w=[b["junk"], sq_])
                K.op("act", lambda e: e.activation(out=rn_[:, 2:3], in_=sq_[:, 2:3], func=AF.Ln, scale=1.0 / 128,
                                                   bias=1e-6), r=[sq_], w=[rn_])
                K.op("act", lambda e: e.activation(out=rn_[:, 2:3], in_=rn_[:, 2:3], func=AF.Exp, scale=-0.5),
                     r=[rn_], w=[rn_])
                K.op("act", lambda e: e.activation(out=b["zs"][:], in_=z_[:, h * 128:(h + 1) * 128], func=AF.Silu),
                     r=[z_], w=[b["zs"]])
                K.op("dve", lambda e: e.scalar_tensor_tensor(out=b["on"][:], in0=b["o"][:], scalar=rn_[:, 2:3],
                                                             in1=gnw[:], op0=ALU.mult, op1=ALU.mult),
                     r=[b["o"], rn_, gnw], w=[b["on"]])
                K.op("pool", lambda e: e.tensor_tensor(out=b["on"][:], in0=b["on"][:], in1=b["zs"][:], op=ALU.mult),
                     r=[b["on"], b["zs"]], w=[b["on"]])
                tr(pt["pOT"], b["on"][:], b["on"])
                ob_ = obf[(u * 4 + h) % 2]
                K.op("dve", lambda e: e.tensor_copy(out=ob_[:], in_=pt["pOT"][:]), r=[pt["pOT"]], w=[ob_])
                K.dma("sp", mixT[256 + h * 128:256 + (h + 1) * 128, u * 128:(u + 1) * 128], ob_[:], r=[ob_])


def _ln_tile(K, z, xo, PB, gi, bi, st, mv, rs, nmr, xn):
    for hh in range(2):
        K.op("dve", lambda e, hh=hh: e.bn_stats(out=st[:, hh, :], in_=z[:, hh * 512:(hh + 1) * 512]), r=[z], w=[st])
    K.op("dve", lambda e: e.bn_aggr(out=mv[:], in_=st[:].rearrange("p a b -> p (a b)")), r=[st], w=[mv])
    K.op("act", lambda e: e.activation(out=rs[:], in_=mv[:, 1:2], func=AF.Ln, bias=1e-5), r=[mv], w=[rs])
    K.op("act", lambda e: e.activation(out=rs[:], in_=rs[:], func=AF.Exp, scale=-0.5), r=[rs], w=[rs])
    K.op("dve", lambda e: e.scalar_tensor_tensor(out=nmr[:], in0=mv[:, 0:1], scalar=-1.0, in1=rs[:],
                                                 op0=ALU.mult, op1=ALU.mult), r=[mv, rs], w=[nmr])
    K.op("act", lambda e: e.activation(out=xn[:], in_=z[:], func=AF.Identity, bias=nmr[:], scale=rs[:]),
         r=[z, nmr, rs], w=[xn])
    K.op("pool", lambda e: e.tensor_tensor(out=xn[:], in0=xn[:], in1=PB[:, gi, :], op=ALU.mult), r=[xn, PB], w=[xn])
    K.op("pool", lambda e: e.tensor_tensor(out=xo[:], in0=xn[:], in1=PB[:, bi, :], op=ALU.add), r=[xn, PB], w=[xo])


def phase_outproj(K, nc, I, l, S, xin, x1, mixT, PB):
    NG = S // 512
    with contextlib.ExitStack() as es:
        W = K.sb(es, "f_W", [128, 8, D], BF)
        wst = [K.sb(es, f"f_wst{i}", [128, D]) for i in range(2)]
        mt = [K.sb(es, f"f_mt{i}", [128, 8, 512], BF) for i in range(2)]
        xt = [K.sb(es, f"f_xt{i}", [128, D]) for i in range(2)]
        z = [K.sb(es, f"f_z{i}", [128, D]) for i in range(2)]
        xn = [K.sb(es, f"f_xn{i}", [128, D]) for i in range(2)]
        xo = [K.sb(es, f"f_xo{i}", [128, D]) for i in range(2)]
        st = [K.sb(es, f"f_st{i}", [128, 2, 6]) for i in range(2)]
        mv = [K.sb(es, f"f_mv{i}", [128, 2]) for i in range(2)]
        rs = [K.sb(es, f"f_rs{i}", [128, 1]) for i in range(2)]
        nmr = [K.sb(es, f"f_nmr{i}", [128, 1]) for i in range(2)]
        ps = [K.ps(es, f"f_ps{i}", [128, 512]) for i in range(4)]
        for k in range(8):
            s_ = wst[k % 2]
            K.dma("sp", s_[:], I["w_out"][l, k * 128:(k + 1) * 128, :], w=[s_])
            K.op("act", lambda e, k=k, s_=s_: e.activation(out=W[:, k, :], in_=s_[:], func=AF.Copy), r=[s_], w=[W])
        for g in range(NG):
            m_ = mt[g % 2]
            K.dma("sp", m_[:], mixT.rearrange("(k p) s -> p k s", p=128)[:, :, g * 512:(g + 1) * 512], w=[m_])
            for tt in range(4):
                ti = g * 4 + tt
                i2 = ti % 2
                K.dma("sp", xt[i2][:], xin[ti * 128:(ti + 1) * 128, :], w=[xt[i2]])
                for hh in range(2):
                    p = ps[i2 * 2 + hh]
                    for k in range(8):
                        K.op("pe", lambda e, p=p, k=k, hh=hh: e.matmul(
                            p[:], lhsT=m_[:, k, tt * 128:(tt + 1) * 128], rhs=W[:, k, hh * 512:(hh + 1) * 512],
                            start=(k == 0), stop=(k == 7)), r=[m_, W], w=[p])
                    K.op("dve", lambda e, p=p, hh=hh: e.tensor_tensor(
                        out=z[i2][:, hh * 512:(hh + 1) * 512], in0=p[:], in1=PB[:, 0, hh * 512:(hh + 1) * 512],
                        op=ALU.mult), r=[p, PB], w=[z[i2]])
                K.op("dve", lambda e: e.scalar_tensor_tensor(out=z[i2][:], in0=xt[i2][:], scalar=DN_ALPHA, in1=z[i2][:],
                                                             op0=ALU.mult, op1=ALU.add), r=[xt[i2], z[i2]], w=[z[i2]])
                _ln_tile(K, z[i2], xo[i2], PB, 2, 3, st[i2], mv[i2], rs[i2], nmr[i2], xn[i2])
                K.dma("sp", x1[ti * 128:(ti + 1) * 128, :], xo[i2][:], r=[xo[i2]])


def phase_moe(K, nc, I, l, S, x1, xout, identF, PB, modT):
    TC = min(1024, S)
    NCH = S // TC
    NTT = TC // 128
    NH = TC // 512
    with contextlib.ExitStack() as es:
        sbt = lambda n, shp, dt=F32: K.sb(es, "g_" + n, shp, dt)
        Wg = sbt("Wg", [128, 8, D], BF)
        Wl = sbt("Wl", [128, 8, D], BF)
        Wd = sbt("Wd", [128, 8, D], BF)
        stg = [sbt(f"stg{i}", [128, 2 * D]) for i in range(2)]
        hT = sbt("hT", [128, 8, TC], BF)
        ysb = sbt("ysb", [128, NTT, D])
        act = sbt("act", [128, 8, TC], BF)
        xg = [sbt(f"xg{i}", [128, 512]) for i in range(2)]
        sg = [sbt(f"sg{i}", [128, 512]) for i in range(2)]
        xl = [sbt(f"xl{i}", [128, 512]) for i in range(2)]
        xt = [sbt(f"xt{i}", [128, D]) for i in range(2)]
        h32 = sbt("h32", [128, 8, 128])
        wr = sbt("wr", [128, 8, NE])
        brB = sbt("brB", [128, NE])
        bd = sbt("bd", [NE, D])
        bgR = sbt("bgR", [128, 2, 256])
        bgT = sbt("bgT", [128, 2, 256])
        lg = sbt("lg", [128, NE])
        t8 = sbt("t8", [128, 8])
        nmx = sbt("nmx", [128, 1])
        msk = sbt("msk", [128, NE])
        exs = sbt("exs", [128, NE])
        ssum = sbt("ssum", [128, 1])
        gates = sbt("gates", [128, NTT, NE])
        gT = sbt("gT", [NE, 128])
        ngates = sbt("ngates", [128, NTT, NE])
        bg7 = sbt("bg7", [128, 2, 256])
        z = sbt("z", [128, D])
        xn = sbt("xn", [128, D])
        xo = [sbt(f"xo{i}", [128, D]) for i in range(2)]
        st = sbt("st", [128, 2, 6])
        mv = sbt("mv", [128, 2])
        rs = sbt("rs", [128, 1])
        nmr = sbt("nmr", [128, 1])
        pG = [K.ps(es, f"g_pG{i}", [128, 512]) for i in range(2)]
        pL = [K.ps(es, f"g_pL{i}", [128, 512]) for i in range(2)]
        pD = [K.ps(es, f"g_pD{i}", [128, 512]) for i in range(2)]

        K.dma("sp", wr[:], I["w_router"][l].rearrange("(k p) e -> p k e", p=128), w=[wr])
        K.dma("sp", brB[:], _bcast_rows(I["b_router"][l:l + 1, :]), w=[brB])
        K.dma("sp", bd[:], I["b_down"][l, :, :], w=[bd])
        K.dma("sp", bgR[:], I["b_gu"][l].rearrange("(q e) (f c) -> (e f) q c", q=2, c=256), w=[bgR])
        for q in range(2):
            for two in range(2):
                K.op("pe", lambda e, q=q, two=two: e.transpose(
                    out=pG[0][:, (q * 2 + two) * 128:(q * 2 + two + 1) * 128],
                    in_=bgR[:, q, two::2], identity=identF[:]), r=[bgR, identF], w=[pG[0]])
        for q in range(2):
            for two in range(2):
                K.op("dve", lambda e, q=q, two=two: e.tensor_copy(
                    out=bgT[:, two, q * 128:(q + 1) * 128],
                    in_=pG[0][:, (q * 2 + two) * 128:(q * 2 + two + 1) * 128]), r=[pG[0]], w=[bgT])

        import os
        MODE = os.environ.get("KMOE", "")

        K.op("dve", lambda e: e.tensor_scalar(out=bg7[:], in0=bgT[:], scalar1=-1.0, scalar2=7.0, op0=ALU.mult,
                                              op1=ALU.add), r=[bgT], w=[bg7])

        def load_gu(e):
            if MODE in ("computeonly", "peonly"):
                return
            for k in range(8):
                s_ = stg[k % 2]
                K.dma("sp", s_[:], I["w_gu"][l, e, k * 128:(k + 1) * 128, :], w=[s_])
                K.op("act", lambda en, k=k, s_=s_: en.activation(out=Wg[:, k, :], in_=s_[:, 0::2], func=AF.Copy),
                     r=[s_], w=[Wg])
                K.op("act", lambda en, k=k, s_=s_: en.activation(out=Wl[:, k, :], in_=s_[:, 1::2], func=AF.Copy),
                     r=[s_], w=[Wl])

        def load_d(e):
            if MODE in ("computeonly", "peonly"):
                return
            for k2 in range(4):
                s_ = stg[k2 % 2]
                K.dma("sp", s_[:].rearrange("p (a d) -> p a d", a=2),
                      I["w_down"][l, e, k2 * 256:(k2 + 1) * 256, :].rearrange("(a p) d -> p a d", p=128), w=[s_])
                K.op("pool", lambda en, k2=k2, s_=s_: en.tensor_copy(
                    out=Wd[:, 2 * k2:2 * k2 + 2, :], in_=s_[:].rearrange("p (a d) -> p a d", a=2)), r=[s_], w=[Wd])

        for ch in range(NCH):
            t0 = ch * NTT
            for tt in range(NTT):
                ti = t0 + tt
                x_ = xt[ti % 2]
                K.dma("sp", x_[:], x1[ti * 128:(ti + 1) * 128, :], w=[x_])
                for hh in range(2):
                    p = pG[hh]
                    for kk in range(4):
                        k = hh * 4 + kk
                        K.op("pe", lambda e, p=p, kk=kk, k=k: e.transpose(
                            out=p[:, kk * 128:(kk + 1) * 128], in_=x_[:, k * 128:(k + 1) * 128], identity=identF[:]),
                            r=[x_, identF], w=[p])
                    for kk in range(4):
                        k = hh * 4 + kk
                        K.op("act", lambda e, p=p, kk=kk, k=k: e.activation(
                            out=h32[:, k, :], in_=p[:, kk * 128:(kk + 1) * 128], func=AF.Identity,
                            bias=modT[:, 2, k:k + 1], scale=modT[:, 3, k:k + 1]), r=[p, modT], w=[h32])
                K.op("pool", lambda e: e.tensor_copy(out=hT[:, :, tt * 128:(tt + 1) * 128], in_=h32[:]), r=[h32], w=[hT])
                for k in range(8):
                    K.op("pe", lambda e, k=k: e.matmul(pL[0][:, 0:NE], lhsT=h32[:, k, :], rhs=wr[:, k, :],
                                                       start=(k == 0), stop=(k == 7)), r=[h32, wr], w=[pL[0]])
                K.op("dve", lambda e: e.tensor_tensor(out=lg[:], in0=pL[0][:, 0:NE], in1=brB[:], op=ALU.add),
                     r=[pL[0], brB], w=[lg])
                K.op("dve", lambda e: e.max(out=t8[:], in_=lg[:]), r=[lg], w=[t8])
                K.op("dve", lambda e: e.tensor_scalar(out=msk[:], in0=lg[:], scalar1=t8[:, 3:4], scalar2=None,
                                                      op0=ALU.is_ge), r=[lg, t8], w=[msk])
                K.op("dve", lambda e: e.tensor_scalar(out=nmx[:], in0=t8[:, 0:1], scalar1=-1.0, scalar2=None,
                                                      op0=ALU.mult), r=[t8], w=[nmx])
                K.op("act", lambda e: e.activation(out=exs[:], in_=lg[:], func=AF.Exp, bias=nmx[:]), r=[lg, nmx], w=[exs])
                K.op("dve", lambda e: e.tensor_tensor(out=exs[:], in0=exs[:], in1=msk[:], op=ALU.mult),
                     r=[exs, msk], w=[exs])
                K.op("dve", lambda e: e.tensor_reduce(out=ssum[:], in_=exs[:], axis=AX.X, op=ALU.add), r=[exs], w=[ssum])
                K.op("dve", lambda e: e.reciprocal(out=ssum[:], in_=ssum[:]), r=[ssum], w=[ssum])
                K.op("dve", lambda e: e.tensor_scalar(out=gates[:, tt, :], in0=exs[:], scalar1=ssum[:], scalar2=None,
                                                      op0=ALU.mult), r=[exs, ssum], w=[gates])
                K.op("dve", lambda e: e.tensor_scalar(out=ngates[:, tt, :], in0=gates[:, tt, :], scalar1=-1.0,
                                                      scalar2=None, op0=ALU.mult), r=[gates], w=[ngates])
                K.op("pe", lambda e: e.transpose(out=pL[1][0:NE, 0:128], in_=gates[:, tt, :], identity=identF[:]),
                     r=[gates, identF], w=[pL[1]])
                K.op("dve", lambda e: e.tensor_copy(out=gT[:], in_=pL[1][0:NE, 0:128]), r=[pL[1]], w=[gT])
                for hh in range(2):
                    K.op("pe", lambda e, hh=hh: e.matmul(pD[hh][:], lhsT=gT[:], rhs=bd[:, hh * 512:(hh + 1) * 512],
                                                         start=True, stop=True), r=[gT, bd], w=[pD[hh]])
                    K.op("dve", lambda e, hh=hh: e.tensor_copy(out=ysb[:, tt, hh * 512:(hh + 1) * 512], in_=pD[hh][:]),
                         r=[pD[hh]], w=[ysb])
            load_gu(0)
            it = 0
            for ex in range(NE):
                load_d(ex)
                q, ei = ex // 16, (ex % 16) * 8
                for hf in range(NH if MODE != "loadonly" else 0):
                    for fc in range(8):
                        i2 = it % 2
                        it += 1
                        for (p, Wm) in ((pG[i2], Wg), (pL[i2], Wl)):
                            for k in range(8):
                                K.op("pe", lambda e, p=p, Wm=Wm, k=k: e.matmul(
                                    p[:], lhsT=Wm[:, k, fc * 128:(fc + 1) * 128], rhs=hT[:, k, hf * 512:(hf + 1) * 512],
                                    start=(k == 0), stop=(k == 7)), r=[Wm, hT], w=[p])
                        bcol = q * 128 + ei + fc
                        if MODE == "peonly":
                            continue
                        K.op("act", lambda e: e.activation(out=xg[i2][:], in_=pG[i2][:], func=AF.Relu,
                                                           bias=bg7[:, 0, bcol:bcol + 1], scale=-1.0),
                             r=[pG[i2], bg7], w=[xg[i2]])
                        K.op("act", lambda e: e.activation(out=sg[i2][:], in_=xg[i2][:], func=AF.Sigmoid,
                                                           bias=1.702 * 7.0, scale=-1.702),
                             r=[xg[i2]], w=[sg[i2]])
                        K.op("act", lambda e: e.activation(out=xl[i2][:], in_=pL[i2][:], func=AF.Relu,
                                                           bias=bg7[:, 1, bcol:bcol + 1], scale=-1.0),
                             r=[pL[i2], bg7], w=[xl[i2]])
                        K.op("act", lambda e: e.activation(out=xl[i2][:], in_=xl[i2][:], func=AF.Relu,
                                                           bias=14.0, scale=-1.0),
                             r=[xl[i2]], w=[xl[i2]])
                        K.op("dve", lambda e: e.scalar_tensor_tensor(out=xg[i2][:], in0=xg[i2][:], scalar=-7.0,
                                                                     in1=sg[i2][:], op0=ALU.add, op1=ALU.mult),
                             r=[xg[i2], sg[i2]], w=[xg[i2]])
                        K.op("dve", lambda e: e.scalar_tensor_tensor(out=act[:, fc, hf * 512:(hf + 1) * 512],
                                                                     in0=xl[i2][:], scalar=-6.0, in1=xg[i2][:],
                                                                     op0=ALU.add, op1=ALU.mult),
                             r=[xg[i2], xl[i2]], w=[act])
                if ex + 1 < NE:
                    load_gu(ex + 1)
                for tt in range(NTT if MODE != "loadonly" else 0):
                    for dh in range(2):
                        p = pD[(tt * 2 + dh) % 2]
                        for fc in range(8):
                            K.op("pe", lambda e, p=p, fc=fc: e.matmul(
                                p[:], lhsT=act[:, fc, tt * 128:(tt + 1) * 128], rhs=Wd[:, fc, dh * 512:(dh + 1) * 512],
                                start=(fc == 0), stop=(fc == 7)), r=[act, Wd], w=[p])
                        if MODE == "peonly":
                            continue
                        K.op("dve", lambda e, p=p: e.scalar_tensor_tensor(
                            out=ysb[:, tt, dh * 512:(dh + 1) * 512], in0=p[:], scalar=ngates[:, tt, ex:ex + 1],
                            in1=ysb[:, tt, dh * 512:(dh + 1) * 512], op0=ALU.mult, op1=ALU.add),
                            r=[p, ngates, ysb], w=[ysb])
            for tt in range(NTT):
                ti = t0 + tt
                x_ = xt[ti % 2]
                K.dma("sp", x_[:], x1[ti * 128:(ti + 1) * 128, :], w=[x_])
                K.op("dve", lambda e: e.tensor_tensor(out=z[:], in0=ysb[:, tt, :], in1=PB[:, 1, :], op=ALU.mult),
                     r=[ysb, PB], w=[z])
                K.op("dve", lambda e: e.scalar_tensor_tensor(out=z[:], in0=x_[:], scalar=DN_ALPHA, in1=z[:],
                                                             op0=ALU.mult, op1=ALU.add), r=[x_, z], w=[z])
                o_ = xo[ti % 2]
                _ln_tile(K, z, o_, PB, 4, 5, st, mv, rs, nmr, xn)
                K.dma("sp", xout[ti * 128:(ti + 1) * 128, :], o_[:], r=[o_])


def _consts(S):
    k = {}
    k["k_ident"] = np.eye(128, dtype=np.float32)
    r = np.arange(128)
    Lf = (r[:, None] <= r[None, :]).astype(np.float32)
    mlow = (r[:, None] >= r[None, :]).astype(np.float32)
    mstr = (r[:, None] > r[None, :]).astype(np.float32)
    k["k_tri"] = np.ascontiguousarray(np.concatenate([Lf, mlow, mstr], 1))
    t = np.arange(512)
    k["k_msb"] = np.ascontiguousarray(np.concatenate(
        [((128 * rr + r[:, None]) < t[None, :]).astype(np.float32) for rr in range(4)], 1))
    k["k_mda"] = np.ascontiguousarray(np.concatenate(
        [((128 * rr + r[:, None]) <= t[None, :]).astype(np.float32) for rr in range(4)], 1))
    pos = np.arange(S, dtype=np.float32)
    inv = (500000.0 ** (-np.arange(0, 8, 2, dtype=np.float32) / 8)).astype(np.float32)
    ang = pos[:, None] * inv[None, :]
    cf = np.ones((S, 16, 32), np.float32)
    sf = np.zeros((S, 16, 32), np.float32)
    co, si = np.cos(ang).astype(np.float32), np.sin(ang).astype(np.float32)
    cf[:, :, 0:4] = co[:, None, :]
    cf[:, :, 4:8] = co[:, None, :]
    sf[:, :, 0:4] = -si[:, None, :]
    sf[:, :, 4:8] = si[:, None, :]
    k["k_cos"] = np.ascontiguousarray(cf.reshape(S, 512))
    k["k_sin"] = np.ascontiguousarray(sf.reshape(S, 512))
    return k


_NC_CACHE = {}


def kernel(**inputs):
    x = np.asarray(inputs["x"], dtype=np.float32)
    B, S, _ = x.shape
    depth = int(np.asarray(inputs["w_in"]).shape[0])
    key = (S, depth)
    if key not in _NC_CACHE:
        _NC_CACHE[key] = build_program(S=S, depth=depth)
    nc = _NC_CACHE[key]
    shared = {}
    for k_, v in inputs.items():
        if k_ in ("x", "c"):
            continue
        v = np.ascontiguousarray(np.asarray(v, dtype=np.float32))
        if k_ == "da_lambda":
            v = np.ascontiguousarray(v.reshape(v.shape[0], 128))
        shared[k_] = v
    shared.update(_consts(S))
    c = np.asarray(inputs["c"], dtype=np.float32)
    in_maps = []
    for b in range(B):
        m = dict(shared)
        m["x"] = np.ascontiguousarray(x[b])
        m["c"] = np.ascontiguousarray(c[b].reshape(8, 128))
        in_maps.append(m)
    res = run_bass_kernel_spmd(nc, in_maps, core_ids=list(range(B)))
    return np.stack([np.asarray(r["out"], dtype=np.float32) for r in res.results], axis=0)
```

```python
import contextlib
import math
import numpy as np
import concourse.bass as bass
import concourse.mybir as mybir
from concourse.bass_utils import run_bass_kernel_spmd

F32 = mybir.dt.float32
BF = mybir.dt.bfloat16
AF = mybir.ActivationFunctionType
ALU = mybir.AluOpType
AX = mybir.AxisListType

D = 1024
DEPTH = 4
SEQ = 4096
NE = 32
IN_W = 3592
DN_ALPHA = (2 * DEPTH) ** 0.25
C_SBQ, C_SBK, C_SBV, C_GQ, C_GK, C_GV, C_GZ, C_GA, C_GB, C_DQ, C_DK, C_DV = (
    0, 256, 512, 768, 1280, 1792, 2304, 2816, 2820, 2824, 3080, 3336)


class T:
    __slots__ = ("h", "w", "r")

    def __init__(self, h):
        self.h = h
        self.w = None
        self.r = {}

    def __getitem__(self, k):
        return self.h[k]


class TV:
    __slots__ = ("h", "p")

    def __init__(self, parent, ap):
        self.h = ap
        self.p = parent

    def __getitem__(self, k):
        return self.h[k]


def _roots(bs):
    return [getattr(b, "p", b) for b in bs]


class KB:
    def __init__(self, nc, es):
        self.nc = nc
        self.es = es
        self.E = {}
        for name, e in (("pe", nc.tensor), ("dve", nc.vector), ("act", nc.scalar), ("pool", nc.gpsimd),
                        ("sp", nc.sync)):
            sem = es.enter_context(nc.semaphore("c_" + name))
            self.E[name] = {"eng": e, "sem": sem, "cnt": 0, "known": {}}
        self.rings = {}
        for q in ("sp", "pool", "act"):
            self.rings[q] = {"i": 0, "slots": [[es.enter_context(nc.semaphore(f"d_{q}{i}")), 0] for i in range(12)]}
        self.all_dma = {}
        self.ninst = 0

    def _waits(self, en, r, w, extra=()):
        E = self.E[en]
        deps = {}

        def add(t):
            k = id(t[0])
            if k not in deps or deps[k][1] < t[1]:
                deps[k] = t
        for b in r:
            if b.w:
                add(b.w)
        for b in w:
            if b.w:
                add(b.w)
            for d in b.r.values():
                add(d)
        for t in extra:
            add(t)
        for k, (sem, val) in deps.items():
            if en == "pe" and sem is E["sem"]:
                continue
            if E["known"].get(k, 0) < val:
                E["eng"].wait_ge(sem, val)
                E["known"][k] = val
                self.ninst += 1

    def op(self, en, fn, r=(), w=()):
        E = self.E[en]
        r, w = _roots(r), _roots(w)
        self._waits(en, r, w)
        inst = fn(E["eng"])
        E["cnt"] += 1
        inst.then_inc(E["sem"], 1)
        self.ninst += 1
        tok = (E["sem"], E["cnt"])
        k = id(E["sem"])
        for b in r:
            b.r[k] = tok
        for b in w:
            b.w = tok
            b.r = {}

    def dma(self, q, out, in_, r=(), w=(), **kw):
        E = self.E[q]
        r, w = _roots(r), _roots(w)
        ring = self.rings[q]
        slot = ring["slots"][ring["i"]]
        ring["i"] = (ring["i"] + 1) % len(ring["slots"])
        extra = [(slot[0], slot[1])] if slot[1] else []
        self._waits(q, r, w, extra)
        inst = E["eng"].dma_start(out=out, in_=in_, **kw)
        slot[1] += 16
        inst.then_inc(slot[0], 16)
        self.ninst += 1
        tok = (slot[0], slot[1])
        k = id(slot[0])
        for b in r:
            b.r[k] = tok
        for b in w:
            b.w = tok
            b.r = {}
        self.all_dma[k] = tok

    def barrier(self):
        toks = [(E["sem"], E["cnt"]) for E in self.E.values() if E["cnt"]]
        toks += list(self.all_dma.values())
        for en, E in self.E.items():
            for sem, val in toks:
                if E["known"].get(id(sem), 0) < val:
                    E["eng"].wait_ge(sem, val)
                    E["known"][id(sem)] = val
                    self.ninst += 1

    def sb(self, es, name, shape, dt=F32):
        self.uid = getattr(self, "uid", 0) + 1
        return T(es.enter_context(self.nc.sbuf_tensor(f"{name}_{self.uid}", list(shape), dt)))

    def ps(self, es, name, shape, dt=F32):
        self.uid = getattr(self, "uid", 0) + 1
        return T(es.enter_context(self.nc.psum_tensor(f"{name}_{self.uid}", list(shape), dt)))


def _bcast_rows(ap_row, n=128):
    return ap_row.partition_broadcast(n)


def build_program(S=SEQ, depth=DEPTH, debug=False, phases=None):
    nc = bass.Bass("TRN2", target_bir_lowering=False)
    NT = S // 128
    NG = S // 512
    dbg_kind = "ExternalOutput" if debug else "Internal"

    def din(name, shape, dt=F32):
        return nc.dram_tensor(name, list(shape), dt, kind="ExternalInput").ap()

    def dscr(name, shape, dt=F32):
        return nc.dram_tensor(name, list(shape), dt, kind=dbg_kind).ap()

    I = {}
    I["x"] = din("x", [S, D])
    I["c"] = din("c", [8, 128])
    I["w_ada"] = din("w_ada", [depth, D, 6 * D])
    I["b_ada"] = din("b_ada", [depth, 6 * D])
    I["w_in"] = din("w_in", [depth, D, IN_W])
    I["conv_w"] = din("conv_w", [depth, 4, 1536])
    I["gdn_a_log"] = din("gdn_a_log", [depth, 4])
    I["gdn_dt_bias"] = din("gdn_dt_bias", [depth, 4])
    I["gdn_norm_w"] = din("gdn_norm_w", [depth, 128])
    I["da_lambda"] = din("da_lambda", [depth, 128])
    I["da_subln_w"] = din("da_subln_w", [depth, 64])
    I["w_out"] = din("w_out", [depth, D, D])
    for n in ("ln1_g", "ln1_b", "ln2_g", "ln2_b"):
        I[n] = din(n, [depth, D])
    I["w_router"] = din("w_router", [depth, D, NE])
    I["b_router"] = din("b_router", [depth, NE])
    I["w_gu"] = din("w_gu", [depth, NE, D, 2 * D])
    I["b_gu"] = din("b_gu", [depth, NE, 2 * D])
    I["w_down"] = din("w_down", [depth, NE, D, D])
    I["b_down"] = din("b_down", [depth, NE, D])
    I["k_ident"] = din("k_ident", [128, 128])
    I["k_tri"] = din("k_tri", [128, 3 * 128])
    I["k_msb"] = din("k_msb", [128, 4 * 512])
    I["k_mda"] = din("k_mda", [128, 4 * 512])
    I["k_cos"] = din("k_cos", [S, 512])
    I["k_sin"] = din("k_sin", [S, 512])
    out = nc.dram_tensor("out", [S, D], F32, kind="ExternalOutput").ap()

    X = [I["x"]] + [dscr(f"xs{l}", [S, D]) for l in range(depth - 1)] + [out]
    x1 = dscr("x1", [S, D])
    sbqT = dscr("sbqT", [256, S], BF)
    sbkT = dscr("sbkT", [256, S], BF)
    sbv = dscr("sbv", [S, 256], BF)
    gpre = dscr("gpre", [1536, S])
    gzd = dscr("gzd", [S, 512])
    gabd = dscr("gabd", [S, 8])
    dqT = dscr("dqT", [256, S], BF)
    dkT = dscr("dkT", [256, S], BF)
    dav = dscr("dav", [S, 256], BF)
    mixT = dscr("mixT", [D, S], BF)

    with contextlib.ExitStack() as es:
        K = KB(nc, es)
        identF = K.sb(es, "identF", [128, 128])
        identB = K.sb(es, "identB", [128, 128], BF)
        PB = K.sb(es, "PB", [128, 6, D])
        modT = K.sb(es, "modT", [128, 4, 8])
        K.dma("sp", identF[:], I["k_ident"][:, :], w=[identF])
        K.op("dve", lambda e: e.tensor_copy(out=identB[:], in_=identF[:]), r=[identF], w=[identB])

        for l in range(depth):
            lam_init = 0.8 - 0.6 * math.exp(-0.3 * l)
            xin, xout = X[l], X[l + 1]
            if phases is None or "A" in phases:
                phase_adaln(K, nc, I, l, identF, PB, modT)
                K.barrier()
            if phases is None or "B" in phases:
                phase_inproj(K, nc, I, l, S, xin, identF, identB, modT,
                             sbqT, sbkT, sbv, gpre, gzd, gabd, dqT, dkT, dav)
                K.barrier()
            if phases is None or "C" in phases:
                phase_sb(K, nc, I, S, sbqT, sbkT, sbv, mixT)
                K.barrier()
            if phases is None or "D" in phases:
                phase_da(K, nc, I, l, S, lam_init, dqT, dkT, dav, mixT)
                K.barrier()
            if phases is None or "E" in phases:
                phase_gdn(K, nc, I, l, S, identF, gpre, gzd, gabd, mixT)
                K.barrier()
            if phases is None or "F" in phases:
                phase_outproj(K, nc, I, l, S, xin, x1, mixT, PB)
                K.barrier()
            if phases is None or "G" in phases:
                phase_moe(K, nc, I, l, S, x1, xout, identF, PB, modT)
                K.barrier()
        K.barrier()
    return nc


def phase_adaln(K, nc, I, l, identF, PB, modT):
    with contextlib.ExitStack() as es:
        c8 = K.sb(es, "a_c8", [8, 128])
        cT = K.sb(es, "a_cT", [128, 8])
        condB = K.sb(es, "a_condB", [128, 8, 128])
        onesF = K.sb(es, "a_ones", [128, 128])
        MODB = K.sb(es, "a_modb", [128, 6 * D])
        BAB = K.sb(es, "a_bab", [128, 6 * D])
        tmp = K.sb(es, "a_tmp", [128, 8, 128])
        wst = [K.sb(es, f"a_w{i}", [128, 3072]) for i in range(2)]
        pT = K.ps(es, "a_pT", [128, 8])
        acc = [K.ps(es, f"a_acc{i}", [128, 512]) for i in range(6)]

        K.dma("sp", c8[:], I["c"][:, :], w=[c8])
        K.dma("sp", BAB[:], _bcast_rows(I["b_ada"][l:l + 1, :]), w=[BAB])
        for j, n in enumerate(("ln1_g", "ln1_b", "ln2_g", "ln2_b")):
            K.dma("sp", PB[:, 2 + j, :], _bcast_rows(I[n][l:l + 1, :]), w=[PB])
        K.op("pool", lambda e: e.memset(onesF[:], 1.0), w=[onesF])
        K.op("pe", lambda e: e.transpose(out=pT[:], in_=c8[:], identity=identF[0:8, 0:8]), r=[c8, identF], w=[pT])
        K.op("act", lambda e: e.activation(out=cT[:], in_=pT[:], func=AF.Silu), r=[pT], w=[cT])
        for k in range(8):
            K.op("dve", lambda e, k=k: e.tensor_scalar(out=condB[:, k, :], in0=onesF[:], scalar1=cT[:, k:k + 1],
                                                        scalar2=None, op0=ALU.mult), r=[onesF, cT], w=[condB])
        for half in range(2):
            for k in range(8):
                wb = wst[k % 2]
                K.dma("sp", wb[:], I["w_ada"][l, k * 128:(k + 1) * 128, half * 3072:(half + 1) * 3072], w=[wb])
                for n in range(6):
                    K.op("pe", lambda e, n=n, k=k, wb=wb: e.matmul(acc[n][:], lhsT=condB[:, k, :],
                                                                     rhs=wb[:, n * 512:(n + 1) * 512],
                                                                     start=(k == 0), stop=(k == 7)),
                         r=[condB, wb], w=[acc[n]])
            for n in range(6):
                c0 = half * 3072 + n * 512
                K.op("dve", lambda e, n=n, c0=c0: e.tensor_tensor(out=MODB[:, c0:c0 + 512], in0=acc[n][:],
                                                                    in1=BAB[:, c0:c0 + 512], op=ALU.add),
                     r=[acc[n], BAB], w=[MODB])
        for j in (1, 2, 4, 5):
            K.op("dve", lambda e, j=j: e.tensor_scalar(out=MODB[:, j * D:(j + 1) * D], in0=MODB[:, j * D:(j + 1) * D],
                                                        scalar1=1.0, scalar2=None, op0=ALU.add), r=[MODB], w=[MODB])
        K.op("dve", lambda e: e.tensor_copy(out=PB[:, 0, :], in_=MODB[:, 2 * D:3 * D]), r=[MODB], w=[PB])
        K.op("dve", lambda e: e.tensor_copy(out=PB[:, 1, :], in_=MODB[:, 5 * D:6 * D]), r=[MODB], w=[PB])
        for i, j in enumerate((0, 1, 3, 4)):
            for k in range(8):
                K.op("dve", lambda e, j=j, k=k: e.tensor_tensor(out=tmp[:, k, :],
                                                                 in0=MODB[:, j * D + k * 128:j * D + (k + 1) * 128],
                                                                 in1=identF[:], op=ALU.mult),
                     r=[MODB, identF], w=[tmp])
            K.op("dve", lambda e, i=i: e.tensor_reduce(out=modT[:, i, :], in_=tmp[:], axis=AX.X, op=ALU.add),
                 r=[tmp], w=[modT])


def phase_inproj(K, nc, I, l, S, xin, identF, identB, modT, sbqT, sbkT, sbv, gpre, gzd, gabd, dqT, dkT, dav):
    import os
    CUT = int(os.environ.get("KCUT", "99"))
    NG = S // 512
    with contextlib.ExitStack() as es:
        W = K.sb(es, "b_W", [128, 8, IN_W], BF)
        wst = [K.sb(es, f"b_wst{i}", [128, IN_W]) for i in range(2)]
        xt = [K.sb(es, f"b_xt{i}", [128, D]) for i in range(2)]
        hT = [K.sb(es, f"b_hT{i}", [128, 8, 512], BF) for i in range(2)]
        ob = [K.sb(es, f"b_ob{i}", [128, 512], BF) for i in range(2)]
        of = [K.sb(es, f"b_of{i}", [128, 512]) for i in range(2)]
        tv = [K.sb(es, f"b_tv{i}", [128, 512], BF) for i in range(2)]
        tq = [K.sb(es, f"b_tq{i}", [128, 512], BF) for i in range(2)]
        xs = [K.sb(es, f"b_xs{i}", [128, 512]) for i in range(2)]
        xsw = K.sb(es, "b_xsw", [128, 512])
        xsw2 = K.sb(es, "b_xsw2", [128, 512])
        tz = [K.sb(es, f"b_tz{i}", [128, 512]) for i in range(2)]
        tab = [K.sb(es, f"b_tab{i}", [128, 8]) for i in range(2)]
        cs = [K.sb(es, f"b_cs{i}", [128, 2, 512]) for i in range(2)]
        dT = [K.sb(es, f"b_dT{i}", [128, 4, 512], BF) for i in range(2)]
        pX = [K.ps(es, f"b_pX{i}", [128, 512]) for i in range(2)]
        pF = [K.ps(es, f"b_pF{i}", [128, 512]) for i in range(2)]
        pA = K.ps(es, "b_pA", [128, 512])
        pB = K.ps(es, "b_pB", [128, 512])
        pC = K.ps(es, "b_pC", [128, 512])
        pD = K.ps(es, "b_pD", [128, 512], BF)
        pE = pX[0]

        K.op("pool", lambda e: e.memset(xsw[:], 0.0), w=[xsw])
        for k in range(8):
            st = wst[k % 2]
            K.dma("sp", st[:], I["w_in"][l, k * 128:(k + 1) * 128, :], w=[st])
            K.op("act" if k % 2 == 0 else "pool",
                 (lambda e, k=k, st=st: e.activation(out=W[:, k, :], in_=st[:], func=AF.Copy)) if k % 2 == 0 else
                 (lambda e, k=k, st=st: e.tensor_copy(out=W[:, k, :], in_=st[:])), r=[st], w=[W])

        if CUT < 1:
            return
        feat_chunks = ([(C_SBQ + j * 128, sbqT, j * 128, True) for j in range(2)] +
                       [(C_SBK + j * 128, sbkT, j * 128, True) for j in range(2)] +
                       [(C_GQ + j * 128, gpre, j * 128, False) for j in range(12)])
        ev = 0
        for g in range(NG):
            h = hT[g % 2]
            dTg = dT[g % 2]
            for tt in range(4):
                ti = g * 4 + tt
                x_ = xt[ti % 2]
                K.dma("sp", x_[:], xin[ti * 128:(ti + 1) * 128, :], w=[x_])
                for half in range(2):
                    p = pX[half]
                    for kk in range(4):
                        k = half * 4 + kk
                        K.op("pe", lambda e, p=p, kk=kk, k=k, x_=x_: e.transpose(
                            out=p[:, kk * 128:(kk + 1) * 128], in_=x_[:, k * 128:(k + 1) * 128], identity=identF[:]),
                            r=[x_, identF], w=[p])
                    for kk in range(4):
                        k = half * 4 + kk
                        K.op("act", lambda e, p=p, kk=kk, k=k, h=h, tt=tt: e.activation(
                            out=h[:, k, tt * 128:(tt + 1) * 128], in_=p[:, kk * 128:(kk + 1) * 128],
                            func=AF.Identity, bias=modT[:, 0, k:k + 1], scale=modT[:, 1, k:k + 1]),
                            r=[p, modT], w=[h])
            if CUT < 2:
                continue
            for ci, (c0, dst, r0, isbf) in enumerate(feat_chunks):
                p = pF[ci % 2]
                for k in range(8):
                    K.op("pe", lambda e, p=p, k=k, c0=c0, h=h: e.matmul(
                        p[:], lhsT=W[:, k, c0:c0 + 128], rhs=h[:, k, :], start=(k == 0), stop=(k == 7)),
                        r=[W, h], w=[p])
                o = (ob if isbf else of)[ev % 2]
                if ev % 2 == 0:
                    K.op("dve", lambda e, o=o, p=p: e.tensor_copy(out=o[:], in_=p[:]), r=[p], w=[o])
                else:
                    K.op("act", lambda e, o=o, p=p: e.activation(out=o[:], in_=p[:], func=AF.Copy), r=[p], w=[o])
                ev += 1
                K.dma("sp", dst[r0:r0 + 128, g * 512:(g + 1) * 512], o[:], r=[o], w=[])
            for tt in range(4):
                if CUT < 3:
                    continue
                ti = g * 4 + tt
                lt = lambda k: h[:, k, tt * 128:(tt + 1) * 128]
                tv_, tq_, tz_, tab_, cs_, xs_ = tv[ti % 2], tq[ti % 2], tz[ti % 2], tab[ti % 2], cs[ti % 2], xs[ti % 2]
                K.dma("sp", cs_[:, 0, :], I["k_cos"][ti * 128:(ti + 1) * 128, :], w=[cs_])
                K.dma("sp", cs_[:, 1, :], I["k_sin"][ti * 128:(ti + 1) * 128, :], w=[cs_])
                for (pp, o0, c0, n) in ((pA, 0, C_SBV, 256), (pA, 256, C_DV, 256), (pB, 0, C_DQ, 512),
                                        (pC, 0, C_GZ, 512), (pE, 0, C_GA, 8)):
                    for k in range(8):
                        K.op("pe", lambda e, pp=pp, o0=o0, c0=c0, n=n, k=k: e.matmul(
                            pp[:, o0:o0 + n], lhsT=lt(k), rhs=W[:, k, c0:c0 + n], start=(k == 0), stop=(k == 7)),
                            r=[W, h], w=[pp])
                K.op("act", lambda e: e.activation(out=tv_[:], in_=pA[:], func=AF.Copy), r=[pA], w=[tv_])
                K.dma("sp", sbv[ti * 128:(ti + 1) * 128, :], tv_[:, 0:256], r=[tv_])
                K.dma("sp", dav[ti * 128:(ti + 1) * 128, :], tv_[:, 256:512], r=[tv_])
                K.op("act", lambda e: e.activation(out=tz_[:], in_=pC[:], func=AF.Copy), r=[pC], w=[tz_])
                K.dma("sp", gzd[ti * 128:(ti + 1) * 128, :], tz_[:], r=[tz_])
                K.op("dve", lambda e: e.tensor_copy(out=tab_[:], in_=pE[:, 0:8]), r=[pE], w=[tab_])
                K.dma("sp", gabd[ti * 128:(ti + 1) * 128, :], tab_[:], r=[tab_])
                if CUT < 4:
                    continue
                K.op("act", lambda e: e.activation(out=xs_[:], in_=pB[:], func=AF.Copy), r=[pB], w=[xs_])
                xv = xs_[:].rearrange("p (g d) -> p g d", d=32)
                wv = xsw[:].rearrange("p (g d) -> p g d", d=32)
                K.op("pool", lambda e: e.tensor_copy(out=wv[:, :, 0:4], in_=xv[:, :, 4:8]), r=[xs_], w=[xsw])
                K.op("pool", lambda e: e.tensor_copy(out=wv[:, :, 4:8], in_=xv[:, :, 0:4]), r=[xs_], w=[xsw])
                K.op("dve", lambda e: e.tensor_tensor(out=xs_[:], in0=xs_[:], in1=cs_[:, 0, :], op=ALU.mult),
                     r=[xs_, cs_], w=[xs_])
                K.op("pool", lambda e: e.tensor_tensor(out=xsw2[:], in0=xsw[:], in1=cs_[:, 1, :], op=ALU.mult),
                     r=[xsw, cs_], w=[xsw2])
                K.op("dve", lambda e: e.tensor_tensor(out=tq_[:], in0=xs_[:], in1=xsw2[:], op=ALU.add),
                     r=[xs_, xsw2], w=[tq_])
                if CUT < 5:
                    continue
                tqf = tq_[:]
                for j in range(4):
                    K.op("pe", lambda e, j=j: e.transpose(out=pD[:, j * 128:(j + 1) * 128],
                                                          in_=tqf[:, j * 128:(j + 1) * 128], identity=identB[:]),
                         r=[tq_, identB], w=[pD])
                K.op("dve", lambda e: e.tensor_copy(
                    out=dTg[:, :, tt * 128:(tt + 1) * 128], in_=pD[:].rearrange("p (j t) -> p j t", t=128)),
                    r=[pD], w=[dTg])
            for j in range(4):
                dst = dqT if j < 2 else dkT
                K.dma("sp", dst[(j % 2) * 128:(j % 2 + 1) * 128, g * 512:(g + 1) * 512], dTg[:, j, :], r=[dTg])


def phase_sb(K, nc, I, S, sbqT, sbkT, sbv, mixT):
    NCH = S // 512
    NB = S // 128
    scale = 64 ** -0.5
    with contextlib.ExitStack() as es:
        msk = K.sb(es, "c_msk", [128, 4, 512])
        mskB = K.sb(es, "c_mskB", [128, 4, 512], BF)
        tri = K.sb(es, "c_tri", [128, 128])
        UsB = K.sb(es, "c_UsB", [128, 128], BF)
        onesB = K.sb(es, "c_onesB", [128, 128], BF)
        V = K.sb(es, "c_V", [128, NB, 256], BF)
        qT = [K.sb(es, f"c_qT{i}", [64, S], BF) for i in range(2)]
        kT = [K.sb(es, f"c_kT{i}", [64, S], BF) for i in range(2)]
        CS = K.sb(es, "c_CS", [128, 512])
        E1 = [K.sb(es, f"c_E1{i}", [128, 512]) for i in range(2)]
        SP = [K.sb(es, f"c_SP{i}", [128, 512]) for i in range(2)]
        SPb = [K.sb(es, f"c_SPb{i}", [128, 512], BF) for i in range(2)]
        t1 = [K.sb(es, f"c_t1{i}", [128, 512]) for i in range(2)]
        t2 = [K.sb(es, f"c_t2{i}", [128, 512]) for i in range(2)]
        Wt = [K.sb(es, f"c_W{i}", [128, 512], BF) for i in range(2)]
        osb = [K.sb(es, f"c_o{i}", [64, 512], BF) for i in range(2)]
        pZ = [K.ps(es, f"c_pZ{i}", [128, 512]) for i in range(2)]
        pBt = [K.ps(es, f"c_pB{i}", [128, 512]) for i in range(2)]
        pCt = [K.ps(es, f"c_pC{i}", [128, 512]) for i in range(2)]
        pO = K.ps(es, "c_pO", [64, 512])

        K.dma("sp", msk[:].rearrange("p r t -> p (r t)"), I["k_msb"][:, :], w=[msk])
        K.dma("sp", tri[:], I["k_tri"][:, 256:384], w=[tri])
        K.dma("sp", V[:], sbv.rearrange("(n p) d -> p n d", p=128), w=[V])
        K.op("dve", lambda e: e.tensor_copy(out=mskB[:], in_=msk[:]), r=[msk], w=[mskB])
        K.op("dve", lambda e: e.tensor_copy(out=UsB[:], in_=tri[:]), r=[tri], w=[UsB])
        K.op("pool", lambda e: e.memset(onesB[:], 1.0), w=[onesB])
        it = 0
        for h in range(4):
            q_, k_ = qT[h % 2], kT[h % 2]
            K.dma("sp", q_[:], sbqT[h * 64:(h + 1) * 64, :], w=[q_])
            K.dma("sp", k_[:], sbkT[h * 64:(h + 1) * 64, :], w=[k_])
            for c in range(NCH):
                K.op("pool", lambda e: e.memset(CS[:], 0.0), w=[CS])
                nblk = 4 * c + 4
                for bi, sbk in enumerate(range(nblk - 1, -1, -1)):
                    r_ = sbk - 4 * c
                    i2 = it % 2
                    it += 1
                    z, pb, pc = pZ[i2], pBt[i2], pCt[i2]
                    e1, sp, spb, a1, a2, w_ = E1[i2], SP[i2], SPb[i2], t1[i2], t2[i2], Wt[i2]
                    K.op("pe", lambda e: e.matmul(z[:], lhsT=k_[:, sbk * 128:(sbk + 1) * 128],
                                                  rhs=q_[:, c * 512:(c + 1) * 512], start=True, stop=True),
                         r=[k_, q_], w=[z])
                    K.op("act", lambda e: e.activation(out=e1[:], in_=z[:], func=AF.Exp, scale=scale), r=[z], w=[e1])
                    K.op("act", lambda e: e.activation(out=sp[:], in_=e1[:], func=AF.Ln, bias=1.0), r=[e1], w=[sp])
                    if r_ >= 0:
                        K.op("dve", lambda e: e.tensor_tensor(out=sp[:], in0=sp[:], in1=msk[:, r_, :], op=ALU.mult),
                             r=[sp, msk], w=[sp])
                    K.op("act", lambda e: e.activation(out=spb[:], in_=sp[:], func=AF.Copy), r=[sp], w=[spb])
                    K.op("pe", lambda e: e.matmul(pb[:], lhsT=UsB[:], rhs=spb[:], start=True, stop=True),
                         r=[UsB, spb], w=[pb])
                    K.op("pe", lambda e: e.matmul(pc[:], lhsT=onesB[:], rhs=spb[:], start=True, stop=True),
                         r=[onesB, spb], w=[pc])
                    K.op("dve", lambda e: e.scalar_tensor_tensor(out=a1[:], in0=z[:], scalar=scale, in1=sp[:],
                                                                 op0=ALU.mult, op1=ALU.subtract), r=[z, sp], w=[a1])
                    K.op("dve", lambda e: e.tensor_tensor(out=a2[:], in0=a1[:], in1=pb[:], op=ALU.subtract),
                         r=[a1, pb], w=[a2])
                    K.op("dve", lambda e: e.tensor_tensor(out=a2[:], in0=a2[:], in1=CS[:], op=ALU.subtract),
                         r=[a2, CS], w=[a2])
                    K.op("act", lambda e: e.activation(out=w_[:], in_=a2[:], func=AF.Exp), r=[a2], w=[w_])
                    if r_ >= 0:
                        K.op("dve", lambda e: e.tensor_tensor(out=w_[:], in0=w_[:], in1=mskB[:, r_, :], op=ALU.mult),
                             r=[w_, mskB], w=[w_])
                    K.op("pe", lambda e: e.matmul(pO[:], lhsT=V[:, sbk, h * 64:(h + 1) * 64], rhs=w_[:],
                                                  start=(bi == 0), stop=(bi == nblk - 1)), r=[V, w_], w=[pO])
                    if bi < nblk - 1:
                        K.op("dve", lambda e: e.tensor_tensor(out=CS[:], in0=CS[:], in1=pc[:], op=ALU.add),
                             r=[CS, pc], w=[CS])
                o_ = osb[(h * NCH + c) % 2]
                K.op("act", lambda e: e.activation(out=o_[:], in_=pO[:], func=AF.Copy), r=[pO], w=[o_])
                K.dma("sp", mixT[h * 64:(h + 1) * 64, c * 512:(c + 1) * 512], o_[:], r=[o_])


def phase_da(K, nc, I, l, S, lam_init, dqT, dkT, dav, mixT):
    NCH = S // 512
    NB = S // 128
    scale = 32 ** -0.5
    with contextlib.ExitStack() as es:
        mskB = K.sb(es, "d_mskB", [128, 4, 512], BF)
        msk = K.sb(es, "d_msk", [128, 4, 512])
        onesB = K.sb(es, "d_onesB", [128, 64], BF)
        onesF = K.sb(es, "d_onesF", [64, 64])
        V = K.sb(es, "d_V", [128, NB, 256], BF)
        qT = [K.sb(es, f"d_qT{i}", [64, S], BF) for i in range(2)]
        kT = [K.sb(es, f"d_kT{i}", [64, S], BF) for i in range(2)]
        Et = [K.sb(es, f"d_E{i}", [128, 1024], BF) for i in range(3)]
        lp = K.sb(es, "d_lp", [128, 128])
        lpp = K.sb(es, "d_lpp", [128, 2, 32])
        ls = K.sb(es, "d_ls", [128, 2])
        le = K.sb(es, "d_le", [128, 2])
        nlam = K.sb(es, "d_nlam", [128, 1])
        wcol = K.sb(es, "d_wcol", [64, 1])
        rr = [K.sb(es, f"d_rr{i}", [64, 512]) for i in range(2)]
        oo = [K.sb(es, f"d_oo{i}", [64, 512]) for i in range(2)]
        od = K.sb(es, "d_od", [64, 512])
        sq = K.sb(es, "d_sq", [64, 512])
        rs = K.sb(es, "d_rs", [64, 512])
        ob = [K.sb(es, f"d_ob{i}", [64, 512], BF) for i in range(2)]
        pS = [K.ps(es, f"d_pS{i}", [128, 1024]) for i in range(2)]
        pN = [K.ps(es, f"d_pN{i}", [64, 512]) for i in range(2)]
        pDn = [K.ps(es, f"d_pD{i}", [64, 512]) for i in range(2)]
        pQ = TV(pS[0], pS[0][0:64, 0:512])

        K.dma("sp", msk[:].rearrange("p r t -> p (r t)"), I["k_mda"][:, :], w=[msk])
        K.dma("sp", V[:], dav.rearrange("(n p) d -> p n d", p=128), w=[V])
        K.dma("sp", lp[:], _bcast_rows(I["da_lambda"][l:l + 1, :]), w=[lp])
        K.dma("sp", wcol[:], I["da_subln_w"][l:l + 1, :].rearrange("o d -> d o"), w=[wcol],
              allow_slow_non_contiguous=True)
        K.op("dve", lambda e: e.tensor_copy(out=mskB[:], in_=msk[:]), r=[msk], w=[mskB])
        K.op("pool", lambda e: e.memset(onesB[:], 1.0), w=[onesB])
        K.op("pool", lambda e: e.memset(onesF[:], 1.0), w=[onesF])
        K.op("dve", lambda e: e.tensor_tensor(out=lpp[:, 0, :], in0=lp[:, 0:32], in1=lp[:, 32:64], op=ALU.mult),
             r=[lp], w=[lpp])
        K.op("dve", lambda e: e.tensor_tensor(out=lpp[:, 1, :], in0=lp[:, 64:96], in1=lp[:, 96:128], op=ALU.mult),
             r=[lp], w=[lpp])
        K.op("dve", lambda e: e.tensor_reduce(out=ls[:], in_=lpp[:], axis=AX.X, op=ALU.add), r=[lpp], w=[ls])
        K.op("act", lambda e: e.activation(out=le[:], in_=ls[:], func=AF.Exp), r=[ls], w=[le])
        K.op("dve", lambda e: e.tensor_tensor(out=nlam[:], in0=le[:, 1:2], in1=le[:, 0:1], op=ALU.subtract),
             r=[le], w=[nlam])
        K.op("dve", lambda e: e.tensor_scalar(out=nlam[:], in0=nlam[:], scalar1=-lam_init, scalar2=None, op0=ALU.add),
             r=[nlam], w=[nlam])
        K.op("dve", lambda e: e.tensor_scalar(out=wcol[:], in0=wcol[:], scalar1=(1.0 - lam_init), scalar2=None,
                                              op0=ALU.mult), r=[wcol], w=[wcol])
        it = 0
        for h in range(4):
            q_, k_ = qT[h % 2], kT[h % 2]
            K.dma("sp", q_[:], dqT[h * 64:(h + 1) * 64, :], w=[q_])
            K.dma("sp", k_[:], dkT[h * 64:(h + 1) * 64, :], w=[k_])
            for c in range(NCH):
                nblk = 4 * c + 4
                for bi, sbk in enumerate(range(nblk - 1, -1, -1)):
                    r_ = sbk - 4 * c
                    ps_ = pS[it % 2]
                    e_ = Et[it % 3]
                    it += 1
                    for m in range(2):
                        K.op("pe", lambda e, m=m: e.matmul(
                            ps_[:, m * 512:(m + 1) * 512], lhsT=k_[m * 32:(m + 1) * 32, sbk * 128:(sbk + 1) * 128],
                            rhs=q_[m * 32:(m + 1) * 32, c * 512:(c + 1) * 512], start=True, stop=True),
                            r=[k_, q_], w=[ps_])
                    K.op("act", lambda e: e.activation(out=e_[:], in_=ps_[:], func=AF.Exp, scale=scale),
                         r=[ps_], w=[e_])
                    if r_ >= 0:
                        for m in range(2):
                            K.op("dve", lambda e, m=m: e.tensor_tensor(
                                out=e_[:, m * 512:(m + 1) * 512], in0=e_[:, m * 512:(m + 1) * 512],
                                in1=mskB[:, r_, :], op=ALU.mult), r=[e_, mskB], w=[e_])
                    for m in range(2):
                        K.op("pe", lambda e, m=m: e.matmul(pN[m][:], lhsT=V[:, sbk, h * 64:(h + 1) * 64],
                                                           rhs=e_[:, m * 512:(m + 1) * 512],
                                                           start=(bi == 0), stop=(bi == nblk - 1)),
                             r=[V, e_], w=[pN[m]])
                        K.op("pe", lambda e, m=m: e.matmul(pDn[m][:], lhsT=onesB[:], rhs=e_[:, m * 512:(m + 1) * 512],
                                                           start=(bi == 0), stop=(bi == nblk - 1)),
                             r=[onesB, e_], w=[pDn[m]])
                for m in range(2):
                    K.op("act", lambda e: e.activation(out=rr[m][:], in_=pDn[m][:], func=AF.Ln), r=[pDn[m]], w=[rr[m]])
                    K.op("act", lambda e: e.activation(out=rr[m][:], in_=rr[m][:], func=AF.Exp, scale=-1.0),
                         r=[rr[m]], w=[rr[m]])
                    K.op("dve", lambda e: e.tensor_tensor(out=oo[m][:], in0=pN[m][:], in1=rr[m][:], op=ALU.mult),
                         r=[pN[m], rr[m]], w=[oo[m]])
                K.op("dve", lambda e: e.scalar_tensor_tensor(out=od[:], in0=oo[1][:], scalar=nlam[0:64, :], in1=oo[0][:],
                                                             op0=ALU.mult, op1=ALU.add), r=[oo[0], oo[1], nlam], w=[od])
                K.op("pool", lambda e: e.tensor_tensor(out=sq[:], in0=od[:], in1=od[:], op=ALU.mult), r=[od], w=[sq])
                K.op("pe", lambda e: e.matmul(pQ[:], lhsT=onesF[:], rhs=sq[:], start=True, stop=True),
                     r=[onesF, sq], w=[pQ])
                K.op("act", lambda e: e.activation(out=rs[:], in_=pQ[:], func=AF.Ln, scale=1.0 / 64, bias=1e-6),
                     r=[pQ], w=[rs])
                K.op("act", lambda e: e.activation(out=rs[:], in_=rs[:], func=AF.Exp, scale=-0.5), r=[rs], w=[rs])
                o_ = ob[(h * NCH + c) % 2]
                K.op("dve", lambda e: e.scalar_tensor_tensor(out=o_[:], in0=od[:], scalar=wcol[:], in1=rs[:],
                                                             op0=ALU.mult, op1=ALU.mult), r=[od, wcol, rs], w=[o_])
                K.dma("sp", mixT[768 + h * 64:768 + (h + 1) * 64, c * 512:(c + 1) * 512], o_[:], r=[o_])


def phase_gdn(K, nc, I, l, S, identF, gpre, gzd, gabd, mixT):
    NU = S // 128
    with contextlib.ExitStack() as es:
        sbt = lambda n, shp, dt=F32: K.sb(es, "e_" + n, shp, dt)
        tri = sbt("tri", [128, 384])
        onesF = sbt("ones", [128, 128])
        c48 = sbt("c48", [48, 128])
        cw = sbt("cw", [128, 48])
        alog = sbt("alog", [128, 4])
        dtb = sbt("dtb", [128, 4])
        nea = sbt("nea", [128, 4])
        gnw = sbt("gnw", [128, 128])
        St = [sbt(f"St{h}", [128, 128]) for h in range(4)]
        win = [sbt(f"win{i}", [128, 12, 131]) for i in range(2)]
        gab = [sbt(f"gab{i}", [128, 8]) for i in range(2)]
        gz = [sbt(f"gz{i}", [128, 512]) for i in range(2)]
        sm = {n: [sbt(f"{n}{i}", [128, 4]) for i in range(2)] for n in
              ("xg", "ex", "sp", "a", "e2", "beta", "nbeta", "gcol", "eg", "egl", "ekd", "tmp4")}
        names = ("cq", "ck", "cv", "qn", "kn", "vt", "qnT", "knT", "abc", "dec", "decl", "decs", "X0", "X1",
                 "XT0", "XT1", "A", "AT", "wT", "kdec", "vnew", "o", "zs", "on", "junk")
        bt = {n: [sbt(f"{n}{i}", [128, 128]) for i in range(2)] for n in names}
        Yt = [sbt(f"Y{i}", [128, 256]) for i in range(2)]
        obf = [sbt(f"obf{i}", [128, 128], BF) for i in range(2)]
        ssq = [sbt(f"ssq{i}", [128, 4]) for i in range(2)]
        rn = [sbt(f"rn{i}", [128, 4]) for i in range(2)]
        banks = [K.ps(es, f"e_bank{i}", [128, 512]) for i in range(8)]
        playout = (("pq", "pk", "pv", "pc48"), ("pqT", "pkT", "pwT", "pg"), ("KK", "QK", "Grow", "pOT"),
                   ("pAT", "pXT", "p1", "p4"), ("pX2", "pXT2", "p2", "p3"))
        pt = {}
        for bi_, grp in enumerate(playout):
            for si_, n in enumerate(grp):
                pt[n] = TV(banks[bi_], banks[bi_][:, si_ * 128:(si_ + 1) * 128])
        pY = [TV(banks[5], banks[5][:, 0:256]), TV(banks[6], banks[6][:, 0:256])]
        Lf, mlow, mstr = tri[:, 0:128], tri[:, 128:256], tri[:, 256:384]

        K.dma("sp", tri[:], I["k_tri"][:, :], w=[tri])
        K.dma("sp", c48[:], I["conv_w"][l].rearrange("k (j p) -> (k j) p", p=128), w=[c48])
        K.dma("sp", alog[:], _bcast_rows(I["gdn_a_log"][l:l + 1, :]), w=[alog])
        K.dma("sp", dtb[:], _bcast_rows(I["gdn_dt_bias"][l:l + 1, :]), w=[dtb])
        K.dma("sp", gnw[:], _bcast_rows(I["gdn_norm_w"][l:l + 1, :]), w=[gnw])
        K.op("pool", lambda e: e.memset(onesF[:], 1.0), w=[onesF])
        for h in range(4):
            K.op("pool", lambda e, h=h: e.memset(St[h][:], 0.0), w=[St[h]])
        K.op("pe", lambda e: e.transpose(out=pt["pc48"][:, 0:48], in_=c48[:], identity=identF[0:48, 0:48]),
             r=[c48, identF], w=[pt["pc48"]])
        K.op("dve", lambda e: e.tensor_copy(out=cw[:], in_=pt["pc48"][:, 0:48]), r=[pt["pc48"]], w=[cw])
        K.op("act", lambda e: e.activation(out=nea[:], in_=alog[:], func=AF.Exp), r=[alog], w=[nea])
        K.op("dve", lambda e: e.tensor_scalar(out=nea[:], in0=nea[:], scalar1=-1.0, scalar2=None, op0=ALU.mult),
             r=[nea], w=[nea])

        def tr(dst_ps, src, srcT):
            K.op("pe", lambda e: e.transpose(out=dst_ps[:], in_=src, identity=identF[:]), r=[srcT, identF], w=[dst_ps])

        def mm(dst_ps, lhsT, rhs, deps):
            K.op("pe", lambda e: e.matmul(dst_ps[:], lhsT=lhsT, rhs=rhs, start=True, stop=True), r=deps, w=[dst_ps])

        cpy_i = [0]

        def cpy(dst, dstT, src_ps):
            cpy_i[0] += 1
            cm = os.environ.get("KCPY", "dve")
            if (cpy_i[0] % 2 and cm == "alt") or cm == "dve":
                K.op("dve", lambda e: e.tensor_copy(out=dst, in_=src_ps[:]), r=[src_ps], w=[dstT])
            else:
                K.op("act", lambda e: e.activation(out=dst, in_=src_ps[:], func=AF.Copy), r=[src_ps], w=[dstT])

        import os
        CUT3 = int(os.environ.get("KCUT3", "99"))
        for u in range(NU):
            if CUT3 < 1:
                break
            i2 = u % 2
            w_ = win[i2]
            g_ = gab[i2]
            z_ = gz[i2]
            if u == 0:
                K.op("pool", lambda e: e.memset(w_[:, :, 0:3], 0.0), w=[w_])
                K.dma("sp", w_[:, :, 3:131], gpre.rearrange("(j p) s -> p j s", p=128)[:, :, 0:128], w=[w_])
            else:
                K.dma("sp", w_[:], gpre.rearrange("(j p) s -> p j s", p=128)[:, :, u * 128 - 3:u * 128 + 128], w=[w_])
            K.dma("sp", g_[:], gabd[u * 128:(u + 1) * 128, :], w=[g_])
            K.dma("sp", z_[:], gzd[u * 128:(u + 1) * 128, :], w=[z_])
            s = {n: v[i2] for n, v in sm.items()}
            K.op("dve", lambda e: e.tensor_tensor(out=s["xg"][:], in0=g_[:, 0:4], in1=dtb[:], op=ALU.add),
                 r=[g_, dtb], w=[s["xg"]])
            K.op("act", lambda e: e.activation(out=s["ex"][:], in_=s["xg"][:], func=AF.Exp), r=[s["xg"]], w=[s["ex"]])
            K.op("act", lambda e: e.activation(out=s["sp"][:], in_=s["ex"][:], func=AF.Ln, bias=1.0),
                 r=[s["ex"]], w=[s["sp"]])
            K.op("dve", lambda e: e.tensor_tensor(out=s["a"][:], in0=s["sp"][:], in1=nea[:], op=ALU.mult),
                 r=[s["sp"], nea], w=[s["a"]])
            K.op("act", lambda e: e.activation(out=s["e2"][:], in_=g_[:, 4:8], func=AF.Exp, scale=-1.0),
                 r=[g_], w=[s["e2"]])
            K.op("dve", lambda e: e.tensor_scalar(out=s["e2"][:], in0=s["e2"][:], scalar1=1.0, scalar2=None,
                                                  op0=ALU.add), r=[s["e2"]], w=[s["e2"]])
            K.op("dve", lambda e: e.reciprocal(out=s["beta"][:], in_=s["e2"][:]), r=[s["e2"]], w=[s["beta"]])
            K.op("dve", lambda e: e.tensor_scalar(out=s["nbeta"][:], in0=s["beta"][:], scalar1=-1.0, scalar2=None,
                                                  op0=ALU.mult), r=[s["beta"]], w=[s["nbeta"]])
            pg = pt["pg"]
            K.op("pe", lambda e: e.matmul(pg[:, 0:4], lhsT=Lf, rhs=s["a"][:], start=True, stop=True),
                 r=[tri, s["a"]], w=[pg])
            K.op("pe", lambda e: e.matmul(pg[:, 4:8], lhsT=onesF[:], rhs=s["a"][:], start=True, stop=True),
                 r=[onesF, s["a"]], w=[pg])
            K.op("dve", lambda e: e.tensor_copy(out=s["gcol"][:], in_=pg[:, 0:4]), r=[pg], w=[s["gcol"]])
            K.op("act", lambda e: e.activation(out=s["eg"][:], in_=pg[:, 0:4], func=AF.Exp), r=[pg], w=[s["eg"]])
            K.op("act", lambda e: e.activation(out=s["egl"][:], in_=pg[:, 4:8], func=AF.Exp), r=[pg], w=[s["egl"]])
            K.op("dve", lambda e: e.tensor_tensor(out=s["tmp4"][:], in0=pg[:, 4:8], in1=s["gcol"][:], op=ALU.subtract),
                 r=[pg, s["gcol"]], w=[s["tmp4"]])
            K.op("act", lambda e: e.activation(out=s["ekd"][:], in_=s["tmp4"][:], func=AF.Exp),
                 r=[s["tmp4"]], w=[s["ekd"]])
            for h in range(4):
                if CUT3 < 2:
                    break
                b = {n: v[(u * 4 + h) % 2] for n, v in bt.items()}
                Y = Yt[(u * 4 + h) % 2]
                sq_, rn_ = ssq[(u * 4 + h) % 2], rn[(u * 4 + h) % 2]
                hc = slice(h, h + 1)
                for nm, j in (("cq", h), ("ck", 4 + h), ("cv", 8 + h)):
                    t_ = b[nm]
                    K.op("dve", lambda e: e.tensor_scalar(out=t_[:], in0=w_[:, j, 0:128], scalar1=cw[:, j:j + 1],
                                                          scalar2=None, op0=ALU.mult), r=[w_, cw], w=[t_])
                    for k in range(1, 4):
                        if int(os.environ.get("KCUT4", "99")) < 1:
                            continue
                        K.op("dve", lambda e, k=k: e.scalar_tensor_tensor(
                            out=t_[:], in0=w_[:, j, k:k + 128], scalar=cw[:, k * 12 + j:k * 12 + j + 1], in1=t_[:],
                            op0=ALU.mult, op1=ALU.add), r=[w_, cw, t_], w=[t_])
                    if int(os.environ.get("KCUT4", "99")) < 2:
                        continue
                    K.op("act", lambda e: e.activation(out=t_[:], in_=t_[:], func=AF.Silu), r=[t_], w=[t_])
                if CUT3 < 3:
                    continue
                C5 = int(os.environ.get("KCUT5", "99"))
                tr(pt["pq"], b["cq"][:], b["cq"])
                tr(pt["pk"], b["ck"][:], b["ck"])
                tr(pt["pv"], b["cv"][:], b["cv"])
                if C5 < 1:
                    continue
                K.op("act", lambda e: e.activation(out=b["junk"][:], in_=pt["pq"][:], func=AF.Square,
                                                   accum_out=sq_[:, 0:1]), r=[pt["pq"]], w=[b["junk"], sq_])
                K.op("act", lambda e: e.activation(out=b["junk"][:], in_=pt["pk"][:], func=AF.Square,
                                                   accum_out=sq_[:, 1:2]), r=[pt["pk"]], w=[b["junk"], sq_])
                if C5 < 2:
                    continue
                K.op("act", lambda e: e.activation(out=rn_[:, 0:2], in_=sq_[:, 0:2], func=AF.Ln, bias=1e-6),
                     r=[sq_], w=[rn_])
                K.op("act", lambda e: e.activation(out=rn_[:, 0:2], in_=rn_[:, 0:2], func=AF.Exp, scale=-0.5),
                     r=[rn_], w=[rn_])
                if C5 < 3:
                    continue
                K.op("dve", lambda e: e.tensor_scalar(out=b["qn"][:], in0=pt["pq"][:], scalar1=rn_[:, 0:1],
                                                      scalar2=128 ** -0.5, op0=ALU.mult, op1=ALU.mult),
                     r=[pt["pq"], rn_], w=[b["qn"]])
                if int(os.environ.get("KCUT6", "99")) < 1:
                    continue
                K.op("dve", lambda e: e.tensor_scalar(out=b["kn"][:], in0=pt["pk"][:], scalar1=rn_[:, 1:2],
                                                      scalar2=None, op0=ALU.mult), r=[pt["pk"], rn_], w=[b["kn"]])
                if int(os.environ.get("KCUT6", "99")) < 2:
                    continue
                cpy(b["vt"][:], b["vt"], pt["pv"])
                if C5 < 4:
                    continue
                tr(pt["pqT"], b["qn"][:], b["qn"])
                tr(pt["pkT"], b["kn"][:], b["kn"])
                cpy(b["qnT"][:], b["qnT"], pt["pqT"])
                cpy(b["knT"][:], b["knT"], pt["pkT"])
                if C5 < 5:
                    continue
                mm(pt["KK"], b["knT"][:], b["knT"][:], [b["knT"]])
                mm(pt["QK"], b["qnT"][:], b["knT"][:], [b["qnT"], b["knT"]])
                if CUT3 < 4:
                    continue
                K.op("pool", lambda e: e.tensor_scalar(out=b["abc"][:], in0=onesF[:], scalar1=s["a"][:, hc],
                                                       scalar2=None, op0=ALU.mult), r=[onesF, s["a"]], w=[b["abc"]])
                mm(pt["Grow"], b["abc"][:], Lf, [b["abc"], tri])
                K.op("dve", lambda e: e.tensor_scalar(out=b["dec"][:], in0=pt["Grow"][:], scalar1=s["gcol"][:, hc],
                                                      scalar2=0.0, op0=ALU.subtract, op1=ALU.max),
                     r=[pt["Grow"], s["gcol"]], w=[b["dec"]])
                K.op("act", lambda e: e.activation(out=b["dec"][:], in_=b["dec"][:], func=AF.Exp, scale=-1.0),
                     r=[b["dec"]], w=[b["dec"]])
                K.op("pool", lambda e: e.tensor_tensor(out=b["decl"][:], in0=b["dec"][:], in1=mlow, op=ALU.mult),
                     r=[b["dec"], tri], w=[b["decl"]])
                K.op("pool", lambda e: e.tensor_tensor(out=b["decs"][:], in0=b["dec"][:], in1=mstr, op=ALU.mult),
                     r=[b["dec"], tri], w=[b["decs"]])
                K.op("dve", lambda e: e.scalar_tensor_tensor(out=b["X0"][:], in0=pt["KK"][:], scalar=s["nbeta"][:, hc],
                                                             in1=b["decs"][:], op0=ALU.mult, op1=ALU.mult),
                     r=[pt["KK"], s["nbeta"], b["decs"]], w=[b["X0"]])
                K.op("dve", lambda e: e.tensor_tensor(out=b["A"][:], in0=pt["QK"][:], in1=b["decl"][:], op=ALU.mult),
                     r=[pt["QK"], b["decl"]], w=[b["A"]])
                tr(pt["pAT"], b["A"][:], b["A"])
                cpy(b["AT"][:], b["AT"], pt["pAT"])
                tr(pt["pXT"], b["X0"][:], b["X0"])
                cpy(b["XT0"][:], b["XT0"], pt["pXT"])
                K.op("pool", lambda e: e.tensor_scalar(out=Y[:, 0:128], in0=b["vt"][:], scalar1=s["beta"][:, hc],
                                                       scalar2=None, op0=ALU.mult), r=[b["vt"], s["beta"]], w=[Y])
                K.op("dve", lambda e: e.tensor_scalar(out=Y[:, 128:256], in0=b["kn"][:], scalar1=s["beta"][:, hc],
                                                      scalar2=s["eg"][:, hc], op0=ALU.mult, op1=ALU.mult),
                     r=[b["kn"], s["beta"], s["eg"]], w=[Y])
                if CUT3 < 5:
                    continue
                for k in range(7):
                    Xk, XTk = b[f"X{k % 2}"], b[f"XT{k % 2}"]
                    Xn, XTn = b[f"X{(k + 1) % 2}"], b[f"XT{(k + 1) % 2}"]
                    py = pY[k % 2]
                    K.op("pe", lambda e: e.matmul(py[:], lhsT=XTk[:], rhs=Y[:], start=True, stop=True),
                         r=[XTk, Y], w=[py])
                    if k < 6:
                        mm(pt["pX2"], XTk[:], Xk[:], [XTk, Xk])
                        mm(pt["pXT2"], Xk[:], XTk[:], [XTk, Xk])
                    K.op("dve", lambda e: e.tensor_tensor(out=Y[:], in0=Y[:], in1=py[:], op=ALU.add), r=[Y, py], w=[Y])
                    if k < 6:
                        K.op("act", lambda e: e.activation(out=Xn[:], in_=pt["pX2"][:], func=AF.Copy),
                             r=[pt["pX2"]], w=[Xn])
                        K.op("pool" if False else "act", lambda e: e.activation(out=XTn[:], in_=pt["pXT2"][:],
                                                                                 func=AF.Copy),
                             r=[pt["pXT2"]], w=[XTn])
                tr(pt["pwT"], Y[:, 128:256], Y)
                cpy(b["wT"][:], b["wT"], pt["pwT"])
                K.op("pool", lambda e: e.tensor_scalar(out=b["kdec"][:], in0=b["kn"][:], scalar1=s["ekd"][:, hc],
                                                       scalar2=None, op0=ALU.mult), r=[b["kn"], s["ekd"]], w=[b["kdec"]])
                if CUT3 < 6:
                    continue
                mm(pt["p1"], b["wT"][:], St[h][:], [b["wT"], St[h]])
                K.op("dve", lambda e: e.tensor_tensor(out=b["vnew"][:], in0=Y[:, 0:128], in1=pt["p1"][:],
                                                      op=ALU.subtract), r=[Y, pt["p1"]], w=[b["vnew"]])
                mm(pt["p2"], b["qnT"][:], St[h][:], [b["qnT"], St[h]])
                mm(pt["p3"], b["AT"][:], b["vnew"][:], [b["AT"], b["vnew"]])
                mm(pt["p4"], b["kdec"][:], b["vnew"][:], [b["kdec"], b["vnew"]])
                K.op("dve", lambda e: e.scalar_tensor_tensor(out=St[h][:], in0=St[h][:], scalar=s["egl"][:, hc],
                                                             in1=pt["p4"][:], op0=ALU.mult, op1=ALU.add),
                     r=[St[h], s["egl"], pt["p4"]], w=[St[h]])
                K.op("dve", lambda e: e.tensor_scalar(out=b["o"][:], in0=pt["p2"][:], scalar1=s["eg"][:, hc],
                                                      scalar2=None, op0=ALU.mult), r=[pt["p2"], s["eg"]], w=[b["o"]])
                K.op("dve", lambda e: e.tensor_tensor(out=b["o"][:], in0=b["o"][:], in1=pt["p3"][:], op=ALU.add),
                     r=[b["o"], pt["p3"]], w=[b["o"]])
                K.op("act", lambda e: e.activation(out=b["junk"][:], in_=b["o"][:], func=AF.Square,
                                                   accum_out=sq_[:, 2:3]), r=[b["o"]], w=[b["junk"], sq_])
                K.op("act", lambda e: e.activation(out=rn_[:, 2:3], in_=sq_[:, 2:3], func=AF.Ln, scale=1.0 / 128,
                                                   bias=1e-6), r=[sq_], w=[rn_])
                K.op("act", lambda e: e.activation(out=rn_[:, 2:3], in_=rn_[:, 2:3], func=AF.Exp, scale=-0.5),
                     r=[rn_], w=[rn_])
                K.op("act", lambda e: e.activation(out=b["zs"][:], in_=z_[:, h * 128:(h + 1) * 128], func=AF.Silu),
                     r=[z_], w=[b["zs"]])
                K.op("dve", lambda e: e.scalar_tensor_tensor(out=b["on"][:], in0=b["o"][:], scalar=rn_[:, 2:3],
                                                             in1=gnw[:], op0=ALU.mult, op1=ALU.mult),
                     r=[b["o"], rn_, gnw], w=[b["on"]])
                K.op("pool", lambda e: e.tensor_tensor(out=b["on"][:], in0=b["on"][:], in1=b["zs"][:], op=ALU.mult),
                     r=[b["on"], b["zs"]], w=[b["on"]])
                tr(pt["pOT"], b["on"][:], b["on"])
                ob_ = obf[(u * 4 + h) % 2]
                K.op("dve", lambda e: e.tensor_copy(out=ob_[:], in_=pt["pOT"][:]), r=[pt["pOT"]], w=[ob_])
                K.dma("sp", mixT[256 + h * 128:256 + (h + 1) * 128, u * 128:(u + 1) * 128], ob_[:], r=[ob_])


def _ln_tile(K, z, xo, PB, gi, bi, st, mv, rs, nmr, xn):
    for hh in range(2):
        K.op("dve", lambda e, hh=hh: e.bn_stats(out=st[:, hh, :], in_=z[:, hh * 512:(hh + 1) * 512]), r=[z], w=[st])
    K.op("dve", lambda e: e.bn_aggr(out=mv[:], in_=st[:].rearrange("p a b -> p (a b)")), r=[st], w=[mv])
    K.op("act", lambda e: e.activation(out=rs[:], in_=mv[:, 1:2], func=AF.Ln, bias=1e-5), r=[mv], w=[rs])
    K.op("act", lambda e: e.activation(out=rs[:], in_=rs[:], func=AF.Exp, scale=-0.5), r=[rs], w=[rs])
    K.op("dve", lambda e: e.scalar_tensor_tensor(out=nmr[:], in0=mv[:, 0:1], scalar=-1.0, in1=rs[:],
                                                 op0=ALU.mult, op1=ALU.mult), r=[mv, rs], w=[nmr])
    K.op("act", lambda e: e.activation(out=xn[:], in_=z[:], func=AF.Identity, bias=nmr[:], scale=rs[:]),
         r=[z, nmr, rs], w=[xn])
    K.op("pool", lambda e: e.tensor_tensor(out=xn[:], in0=xn[:], in1=PB[:, gi, :], op=ALU.mult), r=[xn, PB], w=[xn])
    K.op("pool", lambda e: e.tensor_tensor(out=xo[:], in0=xn[:], in1=PB[:, bi, :], op=ALU.add), r=[xn, PB], w=[xo])


def phase_outproj(K, nc, I, l, S, xin, x1, mixT, PB):
    NG = S // 512
    with contextlib.ExitStack() as es:
        W = K.sb(es, "f_W", [128, 8, D], BF)
        wst = [K.sb(es, f"f_wst{i}", [128, D]) for i in range(2)]
        mt = [K.sb(es, f"f_mt{i}", [128, 8, 512], BF) for i in range(2)]
        xt = [K.sb(es, f"f_xt{i}", [128, D]) for i in range(2)]
        z = [K.sb(es, f"f_z{i}", [128, D]) for i in range(2)]
        xn = [K.sb(es, f"f_xn{i}", [128, D]) for i in range(2)]
        xo = [K.sb(es, f"f_xo{i}", [128, D]) for i in range(2)]
        st = [K.sb(es, f"f_st{i}", [128, 2, 6]) for i in range(2)]
        mv = [K.sb(es, f"f_mv{i}", [128, 2]) for i in range(2)]
        rs = [K.sb(es, f"f_rs{i}", [128, 1]) for i in range(2)]
        nmr = [K.sb(es, f"f_nmr{i}", [128, 1]) for i in range(2)]
        ps = [K.ps(es, f"f_ps{i}", [128, 512]) for i in range(4)]
        for k in range(8):
            s_ = wst[k % 2]
            K.dma("sp", s_[:], I["w_out"][l, k * 128:(k + 1) * 128, :], w=[s_])
            K.op("act", lambda e, k=k, s_=s_: e.activation(out=W[:, k, :], in_=s_[:], func=AF.Copy), r=[s_], w=[W])
        for g in range(NG):
            m_ = mt[g % 2]
            K.dma("sp", m_[:], mixT.rearrange("(k p) s -> p k s", p=128)[:, :, g * 512:(g + 1) * 512], w=[m_])
            for tt in range(4):
                ti = g * 4 + tt
                i2 = ti % 2
                K.dma("sp", xt[i2][:], xin[ti * 128:(ti + 1) * 128, :], w=[xt[i2]])
                for hh in range(2):
                    p = ps[i2 * 2 + hh]
                    for k in range(8):
                        K.op("pe", lambda e, p=p, k=k, hh=hh: e.matmul(
                            p[:], lhsT=m_[:, k, tt * 128:(tt + 1) * 128], rhs=W[:, k, hh * 512:(hh + 1) * 512],
                            start=(k == 0), stop=(k == 7)), r=[m_, W], w=[p])
                    K.op("dve", lambda e, p=p, hh=hh: e.tensor_tensor(
                        out=z[i2][:, hh * 512:(hh + 1) * 512], in0=p[:], in1=PB[:, 0, hh * 512:(hh + 1) * 512],
                        op=ALU.mult), r=[p, PB], w=[z[i2]])
                K.op("dve", lambda e: e.scalar_tensor_tensor(out=z[i2][:], in0=xt[i2][:], scalar=DN_ALPHA, in1=z[i2][:],
                                                             op0=ALU.mult, op1=ALU.add), r=[xt[i2], z[i2]], w=[z[i2]])
                _ln_tile(K, z[i2], xo[i2], PB, 2, 3, st[i2], mv[i2], rs[i2], nmr[i2], xn[i2])
                K.dma("sp", x1[ti * 128:(ti + 1) * 128, :], xo[i2][:], r=[xo[i2]])


def phase_moe(K, nc, I, l, S, x1, xout, identF, PB, modT):
    TC = min(1024, S)
    NCH = S // TC
    NTT = TC // 128
    NH = TC // 512
    with contextlib.ExitStack() as es:
        sbt = lambda n, shp, dt=F32: K.sb(es, "g_" + n, shp, dt)
        Wg = sbt("Wg", [128, 8, D], BF)
        Wl = sbt("Wl", [128, 8, D], BF)
        Wd = sbt("Wd", [128, 8, D], BF)
        stg = [sbt(f"stg{i}", [128, 2 * D]) for i in range(2)]
        hT = sbt("hT", [128, 8, TC], BF)
        ysb = sbt("ysb", [128, NTT, D])
        act = sbt("act", [128, 8, TC], BF)
        xg = [sbt(f"xg{i}", [128, 512]) for i in range(2)]
        sg = [sbt(f"sg{i}", [128, 512]) for i in range(2)]
        xl = [sbt(f"xl{i}", [128, 512]) for i in range(2)]
        xt = [sbt(f"xt{i}", [128, D]) for i in range(2)]
        h32 = sbt("h32", [128, 8, 128])
        wr = sbt("wr", [128, 8, NE])
        brB = sbt("brB", [128, NE])
        bd = sbt("bd", [NE, D])
        bgR = sbt("bgR", [128, 2, 256])
        bgT = sbt("bgT", [128, 2, 256])
        lg = sbt("lg", [128, NE])
        t8 = sbt("t8", [128, 8])
        nmx = sbt("nmx", [128, 1])
        msk = sbt("msk", [128, NE])
        exs = sbt("exs", [128, NE])
        ssum = sbt("ssum", [128, 1])
        gates = sbt("gates", [128, NTT, NE])
        gT = sbt("gT", [NE, 128])
        ngates = sbt("ngates", [128, NTT, NE])
        bg7 = sbt("bg7", [128, 2, 256])
        z = sbt("z", [128, D])
        xn = sbt("xn", [128, D])
        xo = [sbt(f"xo{i}", [128, D]) for i in range(2)]
        st = sbt("st", [128, 2, 6])
        mv = sbt("mv", [128, 2])
        rs = sbt("rs", [128, 1])
        nmr = sbt("nmr", [128, 1])
        pG = [K.ps(es, f"g_pG{i}", [128, 512]) for i in range(2)]
        pL = [K.ps(es, f"g_pL{i}", [128, 512]) for i in range(2)]
        pD = [K.ps(es, f"g_pD{i}", [128, 512]) for i in range(2)]

        K.dma("sp", wr[:], I["w_router"][l].rearrange("(k p) e -> p k e", p=128), w=[wr])
        K.dma("sp", brB[:], _bcast_rows(I["b_router"][l:l + 1, :]), w=[brB])
        K.dma("sp", bd[:], I["b_down"][l, :, :], w=[bd])
        K.dma("sp", bgR[:], I["b_gu"][l].rearrange("(q e) (f c) -> (e f) q c", q=2, c=256), w=[bgR])
        for q in range(2):
            for two in range(2):
                K.op("pe", lambda e, q=q, two=two: e.transpose(
                    out=pG[0][:, (q * 2 + two) * 128:(q * 2 + two + 1) * 128],
                    in_=bgR[:, q, two::2], identity=identF[:]), r=[bgR, identF], w=[pG[0]])
        for q in range(2):
            for two in range(2):
                K.op("dve", lambda e, q=q, two=two: e.tensor_copy(
                    out=bgT[:, two, q * 128:(q + 1) * 128],
                    in_=pG[0][:, (q * 2 + two) * 128:(q * 2 + two + 1) * 128]), r=[pG[0]], w=[bgT])

        import os
        MODE = os.environ.get("KMOE", "")

        K.op("dve", lambda e: e.tensor_scalar(out=bg7[:], in0=bgT[:], scalar1=-1.0, scalar2=7.0, op0=ALU.mult,
                                              op1=ALU.add), r=[bgT], w=[bg7])

        def load_gu(e):
            if MODE in ("computeonly", "peonly"):
                return
            for k in range(8):
                s_ = stg[k % 2]
                K.dma("sp", s_[:], I["w_gu"][l, e, k * 128:(k + 1) * 128, :], w=[s_])
                K.op("act", lambda en, k=k, s_=s_: en.activation(out=Wg[:, k, :], in_=s_[:, 0::2], func=AF.Copy),
                     r=[s_], w=[Wg])
                K.op("act", lambda en, k=k, s_=s_: en.activation(out=Wl[:, k, :], in_=s_[:, 1::2], func=AF.Copy),
                     r=[s_], w=[Wl])

        def load_d(e):
            if MODE in ("computeonly", "peonly"):
                return
            for k2 in range(4):
                s_ = stg[k2 % 2]
                K.dma("sp", s_[:].rearrange("p (a d) -> p a d", a=2),
                      I["w_down"][l, e, k2 * 256:(k2 + 1) * 256, :].rearrange("(a p) d -> p a d", p=128), w=[s_])
                K.op("pool", lambda en, k2=k2, s_=s_: en.tensor_copy(
                    out=Wd[:, 2 * k2:2 * k2 + 2, :], in_=s_[:].rearrange("p (a d) -> p a d", a=2)), r=[s_], w=[Wd])

        for ch in range(NCH):
            t0 = ch * NTT
            for tt in range(NTT):
                ti = t0 + tt
                x_ = xt[ti % 2]
                K.dma("sp", x_[:], x1[ti * 128:(ti + 1) * 128, :], w=[x_])
                for hh in range(2):
                    p = pG[hh]
                    for kk in range(4):
                        k = hh * 4 + kk
                        K.op("pe", lambda e, p=p, kk=kk, k=k: e.transpose(
                            out=p[:, kk * 128:(kk + 1) * 128], in_=x_[:, k * 128:(k + 1) * 128], identity=identF[:]),
                            r=[x_, identF], w=[p])
                    for kk in range(4):
                        k = hh * 4 + kk
                        K.op("act", lambda e, p=p, kk=kk, k=k: e.activation(
                            out=h32[:, k, :], in_=p[:, kk * 128:(kk + 1) * 128], func=AF.Identity,
                            bias=modT[:, 2, k:k + 1], scale=modT[:, 3, k:k + 1]), r=[p, modT], w=[h32])
                K.op("pool", lambda e: e.tensor_copy(out=hT[:, :, tt * 128:(tt + 1) * 128], in_=h32[:]), r=[h32], w=[hT])
                for k in range(8):
                    K.op("pe", lambda e, k=k: e.matmul(pL[0][:, 0:NE], lhsT=h32[:, k, :], rhs=wr[:, k, :],
                                                       start=(k == 0), stop=(k == 7)), r=[h32, wr], w=[pL[0]])
                K.op("dve", lambda e: e.tensor_tensor(out=lg[:], in0=pL[0][:, 0:NE], in1=brB[:], op=ALU.add),
                     r=[pL[0], brB], w=[lg])
                K.op("dve", lambda e: e.max(out=t8[:], in_=lg[:]), r=[lg], w=[t8])
                K.op("dve", lambda e: e.tensor_scalar(out=msk[:], in0=lg[:], scalar1=t8[:, 3:4], scalar2=None,
                                                      op0=ALU.is_ge), r=[lg, t8], w=[msk])
                K.op("dve", lambda e: e.tensor_scalar(out=nmx[:], in0=t8[:, 0:1], scalar1=-1.0, scalar2=None,
                                                      op0=ALU.mult), r=[t8], w=[nmx])
                K.op("act", lambda e: e.activation(out=exs[:], in_=lg[:], func=AF.Exp, bias=nmx[:]), r=[lg, nmx], w=[exs])
                K.op("dve", lambda e: e.tensor_tensor(out=exs[:], in0=exs[:], in1=msk[:], op=ALU.mult),
                     r=[exs, msk], w=[exs])
                K.op("dve", lambda e: e.tensor_reduce(out=ssum[:], in_=exs[:], axis=AX.X, op=ALU.add), r=[exs], w=[ssum])
                K.op("dve", lambda e: e.reciprocal(out=ssum[:], in_=ssum[:]), r=[ssum], w=[ssum])
                K.op("dve", lambda e: e.tensor_scalar(out=gates[:, tt, :], in0=exs[:], scalar1=ssum[:], scalar2=None,
                                                      op0=ALU.mult), r=[exs, ssum], w=[gates])
                K.op("dve", lambda e: e.tensor_scalar(out=ngates[:, tt, :], in0=gates[:, tt, :], scalar1=-1.0,
                                                      scalar2=None, op0=ALU.mult), r=[gates], w=[ngates])
                K.op("pe", lambda e: e.transpose(out=pL[1][0:NE, 0:128], in_=gates[:, tt, :], identity=identF[:]),
                     r=[gates, identF], w=[pL[1]])
                K.op("dve", lambda e: e.tensor_copy(out=gT[:], in_=pL[1][0:NE, 0:128]), r=[pL[1]], w=[gT])
                for hh in range(2):
                    K.op("pe", lambda e, hh=hh: e.matmul(pD[hh][:], lhsT=gT[:], rhs=bd[:, hh * 512:(hh + 1) * 512],
                                                         start=True, stop=True), r=[gT, bd], w=[pD[hh]])
                    K.op("dve", lambda e, hh=hh: e.tensor_copy(out=ysb[:, tt, hh * 512:(hh + 1) * 512], in_=pD[hh][:]),
                         r=[pD[hh]], w=[ysb])
            load_gu(0)
            it = 0
            for ex in range(NE):
                load_d(ex)
                q, ei = ex // 16, (ex % 16) * 8
                for hf in range(NH if MODE != "loadonly" else 0):
                    for fc in range(8):
                        i2 = it % 2
                        it += 1
                        for (p, Wm) in ((pG[i2], Wg), (pL[i2], Wl)):
                            for k in range(8):
                                K.op("pe", lambda e, p=p, Wm=Wm, k=k: e.matmul(
                                    p[:], lhsT=Wm[:, k, fc * 128:(fc + 1) * 128], rhs=hT[:, k, hf * 512:(hf + 1) * 512],
                                    start=(k == 0), stop=(k == 7)), r=[Wm, hT], w=[p])
                        bcol = q * 128 + ei + fc
                        if MODE == "peonly":
                            continue
                        K.op("act", lambda e: e.activation(out=xg[i2][:], in_=pG[i2][:], func=AF.Relu,
                                                           bias=bg7[:, 0, bcol:bcol + 1], scale=-1.0),
                             r=[pG[i2], bg7], w=[xg[i2]])
                        K.op("act", lambda e: e.activation(out=sg[i2][:], in_=xg[i2][:], func=AF.Sigmoid,
                                                           bias=1.702 * 7.0, scale=-1.702),
                             r=[xg[i2]], w=[sg[i2]])
                        K.op("act", lambda e: e.activation(out=xl[i2][:], in_=pL[i2][:], func=AF.Relu,
                                                           bias=bg7[:, 1, bcol:bcol + 1], scale=-1.0),
                             r=[pL[i2], bg7], w=[xl[i2]])
                        K.op("act", lambda e: e.activation(out=xl[i2][:], in_=xl[i2][:], func=AF.Relu,
                                                           bias=14.0, scale=-1.0),
                             r=[xl[i2]], w=[xl[i2]])
                        K.op("dve", lambda e: e.scalar_tensor_tensor(out=xg[i2][:], in0=xg[i2][:], scalar=-7.0,
                                                                     in1=sg[i2][:], op0=ALU.add, op1=ALU.mult),
                             r=[xg[i2], sg[i2]], w=[xg[i2]])
                        K.op("dve", lambda e: e.scalar_tensor_tensor(out=act[:, fc, hf * 512:(hf + 1) * 512],
                                                                     in0=xl[i2][:], scalar=-6.0, in1=xg[i2][:],
                                                                     op0=ALU.add, op1=ALU.mult),
                             r=[xg[i2], xl[i2]], w=[act])
                if ex + 1 < NE:
                    load_gu(ex + 1)
                for tt in range(NTT if MODE != "loadonly" else 0):
                    for dh in range(2):
                        p = pD[(tt * 2 + dh) % 2]
                        for fc in range(8):
                            K.op("pe", lambda e, p=p, fc=fc: e.matmul(
                                p[:], lhsT=act[:, fc, tt * 128:(tt + 1) * 128], rhs=Wd[:, fc, dh * 512:(dh + 1) * 512],
                                start=(fc == 0), stop=(fc == 7)), r=[act, Wd], w=[p])
                        if MODE == "peonly":
                            continue
                        K.op("dve", lambda e, p=p: e.scalar_tensor_tensor(
                            out=ysb[:, tt, dh * 512:(dh + 1) * 512], in0=p[:], scalar=ngates[:, tt, ex:ex + 1],
                            in1=ysb[:, tt, dh * 512:(dh + 1) * 512], op0=ALU.mult, op1=ALU.add),
                            r=[p, ngates, ysb], w=[ysb])
            for tt in range(NTT):
                ti = t0 + tt
                x_ = xt[ti % 2]
                K.dma("sp", x_[:], x1[ti * 128:(ti + 1) * 128, :], w=[x_])
                K.op("dve", lambda e: e.tensor_tensor(out=z[:], in0=ysb[:, tt, :], in1=PB[:, 1, :], op=ALU.mult),
                     r=[ysb, PB], w=[z])
                K.op("dve", lambda e: e.scalar_tensor_tensor(out=z[:], in0=x_[:], scalar=DN_ALPHA, in1=z[:],
                                                             op0=ALU.mult, op1=ALU.add), r=[x_, z], w=[z])
                o_ = xo[ti % 2]
                _ln_tile(K, z, o_, PB, 4, 5, st, mv, rs, nmr, xn)
                K.dma("sp", xout[ti * 128:(ti + 1) * 128, :], o_[:], r=[o_])


def _consts(S):
    k = {}
    k["k_ident"] = np.eye(128, dtype=np.float32)
    r = np.arange(128)
    Lf = (r[:, None] <= r[None, :]).astype(np.float32)
    mlow = (r[:, None] >= r[None, :]).astype(np.float32)
    mstr = (r[:, None] > r[None, :]).astype(np.float32)
    k["k_tri"] = np.ascontiguousarray(np.concatenate([Lf, mlow, mstr], 1))
    t = np.arange(512)
    k["k_msb"] = np.ascontiguousarray(np.concatenate(
        [((128 * rr + r[:, None]) < t[None, :]).astype(np.float32) for rr in range(4)], 1))
    k["k_mda"] = np.ascontiguousarray(np.concatenate(
        [((128 * rr + r[:, None]) <= t[None, :]).astype(np.float32) for rr in range(4)], 1))
    pos = np.arange(S, dtype=np.float32)
    inv = (500000.0 ** (-np.arange(0, 8, 2, dtype=np.float32) / 8)).astype(np.float32)
    ang = pos[:, None] * inv[None, :]
    cf = np.ones((S, 16, 32), np.float32)
    sf = np.zeros((S, 16, 32), np.float32)
    co, si = np.cos(ang).astype(np.float32), np.sin(ang).astype(np.float32)
    cf[:, :, 0:4] = co[:, None, :]
    cf[:, :, 4:8] = co[:, None, :]
    sf[:, :, 0:4] = -si[:, None, :]
    sf[:, :, 4:8] = si[:, None, :]
    k["k_cos"] = np.ascontiguousarray(cf.reshape(S, 512))
    k["k_sin"] = np.ascontiguousarray(sf.reshape(S, 512))
    return k


_NC_CACHE = {}


def kernel(**inputs):
    x = np.asarray(inputs["x"], dtype=np.float32)
    B, S, _ = x.shape
    depth = int(np.asarray(inputs["w_in"]).shape[0])
    key = (S, depth)
    if key not in _NC_CACHE:
        _NC_CACHE[key] = build_program(S=S, depth=depth)
    nc = _NC_CACHE[key]
    shared = {}
    for k_, v in inputs.items():
        if k_ in ("x", "c"):
            continue
        v = np.ascontiguousarray(np.asarray(v, dtype=np.float32))
        if k_ == "da_lambda":
            v = np.ascontiguousarray(v.reshape(v.shape[0], 128))
        shared[k_] = v
    shared.update(_consts(S))
    c = np.asarray(inputs["c"], dtype=np.float32)
    in_maps = []
    for b in range(B):
        m = dict(shared)
        m["x"] = np.ascontiguousarray(x[b])
        m["c"] = np.ascontiguousarray(c[b].reshape(8, 128))
        in_maps.append(m)
    res = run_bass_kernel_spmd(nc, in_maps, core_ids=list(range(B)))
    return np.stack([np.asarray(r["out"], dtype=np.float32) for r in res.results], axis=0)
```
